# Optimizing a Trainium2 kernel written in Bass

```python
import jax, jax.numpy as jnp
from jax import lax
import numpy as np

D_MODEL = 1024
BATCH = 16
SEQ = 2048
DEPTH = 4

LRU_WIDTH = 512
LRU_BLOCKS = 8
LRU_BLOCK_DIM = LRU_WIDTH // LRU_BLOCKS
LRU_C = 8.0
CONV_WIDTH = 4
GDN_HEADS = 4
GDN_DK = 128
GDN_DV = 128
GDN_CHUNK = 64
SB_HEADS = 8
SB_DH = 64
SB_BLOCK = 128
GLA_HEADS = 4
GLA_DK = 128
GLA_DV = 128
GLA_GATE_RANK = 16
GLA_TAU = 16.0
GLA_CHUNK = 16
N_BRANCHES = 4
BRANCH_WIDTH = 512
N_EXPERTS = 16
N_GROUPS = 4
EXPERTS_PER_GROUP = N_EXPERTS // N_GROUPS
TOP_K = 2
D_EXPERT = 512
LN_EPS = 1e-5
NORM_EPS = 1e-6
DN_ALPHA = (2 * DEPTH) ** 0.25
DN_BETA = (8 * DEPTH) ** -0.25

IN_SPLITS = (
    LRU_WIDTH,
    3 * GDN_HEADS * GDN_DK,
    GDN_HEADS,
    GDN_HEADS,
    GDN_HEADS * GDN_DV,
    3 * SB_HEADS * SB_DH,
    GLA_HEADS * (2 * GLA_DK + GLA_DV),
    GLA_GATE_RANK,
    GLA_HEADS * GLA_DV,
    N_BRANCHES * D_MODEL,
)
D_IN = sum(IN_SPLITS)

kernel_name = "hybrid_rglru_gdn_stickbreak_gla_groupmoe"


def layer_norm(x, g, b):
    xf = x.astype(jnp.float32)
    mu = jnp.mean(xf, -1, keepdims=True)
    var = jnp.mean(jnp.square(xf - mu), -1, keepdims=True)
    y = (xf - mu) * lax.rsqrt(var + LN_EPS) * g.astype(jnp.float32) + b.astype(jnp.float32)
    return y.astype(x.dtype)


def rms_norm(x, g):
    xf = x.astype(jnp.float32)
    return xf * lax.rsqrt(jnp.mean(jnp.square(xf), -1, keepdims=True) + NORM_EPS) * g.astype(jnp.float32)


def l2_normalize(x):
    return x * lax.rsqrt(jnp.sum(jnp.square(x), -1, keepdims=True) + NORM_EPS)


def causal_depthwise_conv(x, w):
    return lax.conv_general_dilated(
        x, w[:, None, :].astype(x.dtype), window_strides=(1,),
        padding=((w.shape[0] - 1, 0),), dimension_numbers=("NWC", "WIO", "NWC"),
        feature_group_count=x.shape[-1])


def _to_chunks(t, c):
    b, s, h = t.shape[:3]
    return jnp.moveaxis(t.reshape((b, s // c, c, h) + t.shape[3:]), 3, 1)


def _from_chunks(t):
    b, h, n, c, d = t.shape
    return jnp.moveaxis(t, 1, 3).reshape(b, n * c, h, d)


def rg_lru(u, conv_w, conv_b, w_a, b_a, w_x, b_x, lam):
    b, s, _ = u.shape
    xc = (causal_depthwise_conv(u, conv_w) + conv_b).astype(jnp.float32)
    xblk = xc.reshape(b, s, LRU_BLOCKS, LRU_BLOCK_DIM)
    r = jax.nn.sigmoid(jnp.einsum("bsgi,gij->bsgj", xblk, w_a.astype(jnp.float32)).reshape(b, s, LRU_WIDTH) + b_a)
    i = jax.nn.sigmoid(jnp.einsum("bsgi,gij->bsgj", xblk, w_x.astype(jnp.float32)).reshape(b, s, LRU_WIDTH) + b_x)
    log_a = -LRU_C * r * jax.nn.softplus(-lam.astype(jnp.float32))
    a = jnp.exp(log_a)
    mult = jnp.sqrt(-jnp.expm1(2.0 * log_a))
    mult = jnp.where(jnp.arange(s)[None, :, None] == 0, 1.0, mult)
    inp = mult * i * xc

    def combine(lhs, rhs):
        return (lhs[0] * rhs[0], rhs[0] * lhs[1] + rhs[1])

    _, h = lax.associative_scan(combine, (a, inp), axis=1)
    return h


def gated_delta_rule_chunked(q, k, v, g, beta):
    b, s, h, dk = q.shape
    dv = v.shape[-1]
    c = GDN_CHUNK
    q, k, v = _to_chunks(q, c), _to_chunks(k, c), _to_chunks(v, c)
    g, beta = _to_chunks(g, c), _to_chunks(beta, c)
    gc = jnp.cumsum(g, axis=-1)
    causal = jnp.tril(jnp.ones((c, c), dtype=bool))
    strict = jnp.tril(jnp.ones((c, c), dtype=bool), k=-1)
    decay = jnp.exp(jnp.where(causal, gc[..., :, None] - gc[..., None, :], -jnp.inf))
    k_beta = k * beta[..., None]
    a_mat = jnp.where(strict, jnp.einsum("bhnik,bhnjk->bhnij", k_beta, k) * decay, 0.0)
    eye = jnp.broadcast_to(jnp.eye(c, dtype=a_mat.dtype), a_mat.shape)
    t_inv = lax.linalg.triangular_solve(a_mat, eye, left_side=True, lower=True, unit_diagonal=True)
    u = t_inv @ (v * beta[..., None])
    w = t_inv @ (k_beta * jnp.exp(gc)[..., None])
    q_dec = q * jnp.exp(gc)[..., None]
    attn = jnp.einsum("bhnik,bhnjk->bhnij", q, k) * decay
    k_dec = k * jnp.exp(gc[..., -1:] - gc)[..., None]
    g_last = jnp.exp(gc[..., -1])

    def step(state, xs):
        u_i, w_i, q_i, attn_i, kd_i, gl_i = xs
        v_new = u_i - w_i @ state
        o_i = q_i @ state + attn_i @ v_new
        state = state * gl_i[..., None, None] + jnp.swapaxes(kd_i, -1, -2) @ v_new
        return state, o_i

    xs = tuple(jnp.moveaxis(t, 2, 0) for t in (u, w, q_dec, attn, k_dec, g_last))
    state0 = jnp.zeros((b, h, dk, dv), q.dtype)
    _, o = lax.scan(step, state0, xs)
    return _from_chunks(jnp.moveaxis(o, 0, 2))


def gla_chunked(q, k, v, gk):
    b, s, h, dk = q.shape
    dv = v.shape[-1]
    c = GLA_CHUNK
    q, k, v, gk = (_to_chunks(t, c) for t in (q, k, v, gk))
    bcum = jnp.cumsum(gk, axis=-2)
    q_dec = q * jnp.exp(bcum)
    k_inv = k * jnp.exp(-bcum)
    causal = jnp.tril(jnp.ones((c, c), dtype=bool))
    attn = jnp.where(causal, jnp.einsum("bhnik,bhnjk->bhnij", q_dec, k_inv), 0.0)
    o_intra = attn @ v
    k_dec = k * jnp.exp(bcum[..., -1:, :] - bcum)
    g_last = jnp.exp(bcum[..., -1, :])

    def step(state, xs):
        q_i, kd_i, v_i, gl_i = xs
        o_i = q_i @ state
        state = state * gl_i[..., :, None] + jnp.swapaxes(kd_i, -1, -2) @ v_i
        return state, o_i

    xs = tuple(jnp.moveaxis(t, 2, 0) for t in (q_dec, k_dec, v, g_last))
    state0 = jnp.zeros((b, h, dk, dv), q.dtype)
    _, o_inter = lax.scan(step, state0, xs)
    return _from_chunks(o_intra + jnp.moveaxis(o_inter, 0, 2))


def stick_breaking_attention(q, k, v):
    b, s, h, dh = q.shape
    q = jnp.swapaxes(q, 1, 2) * dh ** -0.5
    k = jnp.swapaxes(k, 1, 2)
    v = jnp.swapaxes(v, 1, 2)
    outs = []
    for blk in range(s // SB_BLOCK):
        start = blk * SB_BLOCK
        end = start + SB_BLOCK
        z = jnp.einsum("bhqd,bhkd->bhqk", q[:, :, start:end], k[:, :, :end])
        strict = jnp.arange(end)[None, :] < (start + jnp.arange(SB_BLOCK))[:, None]
        log_keep = jnp.where(strict, jax.nn.log_sigmoid(-z), 0.0)
        between = lax.cumsum(log_keep, axis=3, reverse=True) - log_keep
        weights = jnp.where(strict, jnp.exp(jax.nn.log_sigmoid(z) + between), 0.0)
        outs.append(jnp.einsum("bhqk,bhkd->bhqd", weights, v[:, :, :end]))
    o = jnp.concatenate(outs, axis=2)
    return jnp.swapaxes(o, 1, 2).reshape(b, s, h * dh)


def mixer(x, w_in, conv_a_w, conv_a_b, rg_w_a, rg_b_a, rg_w_x, rg_b_x, rg_lambda,
          gdn_conv_w, gdn_a_log, gdn_dt_bias, gdn_norm_w,
          gla_w_gate_up, gla_b_gate, gla_norm_w, w_branch, w_out):
    b, s, _ = x.shape
    f32 = jnp.float32
    proj = jnp.einsum("bsd,de->bse", x, w_in)
    offsets = np.cumsum(IN_SPLITS)[:-1].tolist()
    (a_in, gdn_qkv, gdn_beta_in, gdn_decay_in, gdn_gate_in,
     sb_qkv, gla_qkv, gla_lr, gla_gate_in, merge_in) = jnp.split(proj, offsets, axis=-1)

    y_a = rg_lru(a_in, conv_a_w, conv_a_b, rg_w_a, rg_b_a, rg_w_x, rg_b_x, rg_lambda)

    qkv = jax.nn.silu(causal_depthwise_conv(gdn_qkv, gdn_conv_w)).astype(f32)
    q, k, v = jnp.split(qkv, [GDN_HEADS * GDN_DK, 2 * GDN_HEADS * GDN_DK], axis=-1)
    q = l2_normalize(q.reshape(b, s, GDN_HEADS, GDN_DK)) * GDN_DK ** -0.5
    k = l2_normalize(k.reshape(b, s, GDN_HEADS, GDN_DK))
    v = v.reshape(b, s, GDN_HEADS, GDN_DV)
    beta = jax.nn.sigmoid(gdn_beta_in.astype(f32))
    g = -jnp.exp(gdn_a_log.astype(f32)) * jax.nn.softplus(gdn_decay_in.astype(f32) + gdn_dt_bias)
    o_b = gated_delta_rule_chunked(q, k, v, g, beta)
    y_b = (rms_norm(o_b, gdn_norm_w) * jax.nn.silu(gdn_gate_in.reshape(b, s, GDN_HEADS, GDN_DV).astype(f32))).reshape(b, s, -1)

    q, k, v = jnp.split(sb_qkv.astype(f32), 3, axis=-1)
    y_c = stick_breaking_attention(q.reshape(b, s, SB_HEADS, SB_DH), k.reshape(b, s, SB_HEADS, SB_DH),
                                   v.reshape(b, s, SB_HEADS, SB_DH))

    q, k, v = jnp.split(gla_qkv.astype(f32), [GLA_HEADS * GLA_DK, 2 * GLA_HEADS * GLA_DK], axis=-1)
    gk = jax.nn.log_sigmoid(jnp.einsum("bsr,rc->bsc", gla_lr.astype(f32), gla_w_gate_up.astype(f32)) + gla_b_gate) / GLA_TAU
    o_d = gla_chunked(q.reshape(b, s, GLA_HEADS, GLA_DK) * GLA_DK ** -0.5, k.reshape(b, s, GLA_HEADS, GLA_DK),
                      v.reshape(b, s, GLA_HEADS, GLA_DV), gk.reshape(b, s, GLA_HEADS, GLA_DK))
    y_d = (rms_norm(o_d, gla_norm_w) * jax.nn.silu(gla_gate_in.reshape(b, s, GLA_HEADS, GLA_DV).astype(f32))).reshape(b, s, -1)

    gates = jax.nn.sigmoid(merge_in.reshape(b, s, N_BRANCHES, D_MODEL))
    merged = jnp.zeros_like(x)
    for n, y_n in enumerate((y_a, y_b, y_c, y_d)):
        merged = merged + gates[:, :, n] * jnp.einsum("bsc,cd->bsd", y_n.astype(x.dtype), w_branch[n])
    return jnp.einsum("bsd,de->bse", merged, w_out)


def moe(x, w_router, router_bias, w_gate, w_up, w_down):
    b, s, _ = x.shape
    scores = jax.nn.sigmoid(jnp.einsum("bsd,de->bse", x.astype(jnp.float32), w_router.astype(jnp.float32)))
    biased = scores + router_bias.astype(jnp.float32)
    grouped = biased.reshape(b, s, N_GROUPS, EXPERTS_PER_GROUP)
    group_score = jnp.sum(lax.top_k(grouped, TOP_K)[0], axis=-1)
    group_mask = jnp.argmax(group_score, axis=-1)[..., None] == jnp.arange(N_GROUPS)
    masked = jnp.where(group_mask[..., None], grouped, -jnp.inf).reshape(b, s, N_EXPERTS)
    _, expert_idx = lax.top_k(masked, TOP_K)
    w = jnp.take_along_axis(scores, expert_idx, axis=-1)
    w = w / jnp.sum(w, axis=-1, keepdims=True)
    combine = jnp.sum(jax.nn.one_hot(expert_idx, N_EXPERTS, dtype=jnp.float32) * w[..., None], axis=-2)
    combine = combine.astype(x.dtype)
    y = jnp.zeros_like(x)
    for e in range(N_EXPERTS):
        h = jax.nn.silu(jnp.einsum("bsd,df->bsf", x, w_gate[e])) * jnp.einsum("bsd,df->bsf", x, w_up[e])
        y = y + combine[..., e:e + 1] * jnp.einsum("bsf,fd->bsd", h, w_down[e])
    return y


def setup_inputs(seed: int = 0) -> dict:
    key = jax.random.key(seed)
    ks = jax.random.split(key, 32)
    f32 = jnp.float32
    L = DEPTH

    def nrm(i, shape, scale):
        return jax.random.normal(ks[i], shape, f32) * scale

    x = nrm(0, (BATCH, SEQ, D_MODEL), 1.0)
    w_in = nrm(1, (L, D_MODEL, D_IN), D_MODEL ** -0.5)
    conv_a_w = nrm(2, (L, CONV_WIDTH, LRU_WIDTH), CONV_WIDTH ** -0.5)
    conv_a_b = nrm(3, (L, LRU_WIDTH), 0.01)
    rg_w_a = nrm(4, (L, LRU_BLOCKS, LRU_BLOCK_DIM, LRU_BLOCK_DIM), LRU_BLOCK_DIM ** -0.5)
    rg_b_a = nrm(5, (L, LRU_WIDTH), 0.01)
    rg_w_x = nrm(6, (L, LRU_BLOCKS, LRU_BLOCK_DIM, LRU_BLOCK_DIM), LRU_BLOCK_DIM ** -0.5)
    rg_b_x = nrm(7, (L, LRU_WIDTH), 0.01)
    a_pow = jax.random.uniform(ks[8], (L, LRU_WIDTH), f32, 0.9, 0.999)
    a0 = a_pow ** (1.0 / LRU_C)
    rg_lambda = jnp.log(a0) - jnp.log1p(-a0)
    gdn_conv_w = nrm(9, (L, CONV_WIDTH, 3 * GDN_HEADS * GDN_DK), CONV_WIDTH ** -0.5)
    gdn_a_log = jnp.log(jax.random.uniform(ks[10], (L, GDN_HEADS), f32, 1.0, 16.0))
    dt = jnp.exp(jax.random.uniform(ks[11], (L, GDN_HEADS), f32, float(np.log(1e-3)), float(np.log(1e-1))))
    gdn_dt_bias = dt + jnp.log(-jnp.expm1(-dt))
    gdn_norm_w = 1.0 + nrm(12, (L, GDN_DV), 0.01)
    gla_w_gate_up = nrm(13, (L, GLA_GATE_RANK, GLA_HEADS * GLA_DK), GLA_GATE_RANK ** -0.5)
    gla_b_gate = nrm(14, (L, GLA_HEADS * GLA_DK), 0.01)
    gla_norm_w = 1.0 + nrm(15, (L, GLA_DV), 0.01)
    w_branch = nrm(16, (L, N_BRANCHES, BRANCH_WIDTH, D_MODEL), BRANCH_WIDTH ** -0.5)
    w_out = nrm(17, (L, D_MODEL, D_MODEL), D_MODEL ** -0.5 * DN_BETA)
    ln1_g = 1.0 + nrm(18, (L, D_MODEL), 0.01)
    ln1_b = nrm(19, (L, D_MODEL), 0.01)
    w_router = nrm(20, (D_MODEL, N_EXPERTS), D_MODEL ** -0.5)
    router_bias = nrm(21, (N_EXPERTS,), 0.01)
    w_gate = nrm(22, (L, N_EXPERTS, D_MODEL, D_EXPERT), D_MODEL ** -0.5)
    w_up = nrm(23, (L, N_EXPERTS, D_MODEL, D_EXPERT), D_MODEL ** -0.5)
    w_down = nrm(24, (L, N_EXPERTS, D_EXPERT, D_MODEL), D_EXPERT ** -0.5 * DN_BETA)
    ln2_g = 1.0 + nrm(25, (L, D_MODEL), 0.01)
    ln2_b = nrm(26, (L, D_MODEL), 0.01)
    return {"x": x, "w_in": w_in, "conv_a_w": conv_a_w, "conv_a_b": conv_a_b,
            "rg_w_a": rg_w_a, "rg_b_a": rg_b_a, "rg_w_x": rg_w_x, "rg_b_x": rg_b_x,
            "rg_lambda": rg_lambda, "gdn_conv_w": gdn_conv_w, "gdn_a_log": gdn_a_log,
            "gdn_dt_bias": gdn_dt_bias, "gdn_norm_w": gdn_norm_w, "gla_w_gate_up": gla_w_gate_up,
            "gla_b_gate": gla_b_gate, "gla_norm_w": gla_norm_w, "w_branch": w_branch,
            "w_out": w_out, "ln1_g": ln1_g, "ln1_b": ln1_b, "w_router": w_router,
            "router_bias": router_bias, "w_gate": w_gate, "w_up": w_up, "w_down": w_down,
            "ln2_g": ln2_g, "ln2_b": ln2_b}


def reference(x, w_in, conv_a_w, conv_a_b, rg_w_a, rg_b_a, rg_w_x, rg_b_x, rg_lambda,
              gdn_conv_w, gdn_a_log, gdn_dt_bias, gdn_norm_w, gla_w_gate_up, gla_b_gate,
              gla_norm_w, w_branch, w_out, ln1_g, ln1_b, w_router, router_bias,
              w_gate, w_up, w_down, ln2_g, ln2_b):
    for l in range(DEPTH):
        h = mixer(x, w_in[l], conv_a_w[l], conv_a_b[l], rg_w_a[l], rg_b_a[l], rg_w_x[l], rg_b_x[l],
                  rg_lambda[l], gdn_conv_w[l], gdn_a_log[l], gdn_dt_bias[l], gdn_norm_w[l],
                  gla_w_gate_up[l], gla_b_gate[l], gla_norm_w[l], w_branch[l], w_out[l])
        x = layer_norm(DN_ALPHA * x + h, ln1_g[l], ln1_b[l])
        h = moe(x, w_router, router_bias, w_gate[l], w_up[l], w_down[l])
        x = layer_norm(DN_ALPHA * x + h, ln2_g[l], ln2_b[l])
    return x
```

```python
from contextlib import ExitStack
import numpy as np
import concourse.bass as bass
import concourse.mybir as mybir
from concourse.bass_utils import run_bass_kernel_spmd

F32 = mybir.dt.float32
F32R = mybir.dt.float32r
BF16 = mybir.dt.bfloat16
AF = mybir.ActivationFunctionType
ALU = mybir.AluOpType
AX = mybir.AxisListType

STRICT = True
DBG = {}
ENGS = ("pe", "act", "dve", "pool", "sp")
CENG = ("pe", "act", "dve", "pool")


class Res:
    __slots__ = ("name", "w", "rs")

    def __init__(self, name):
        self.name = name
        self.w = None
        self.rs = {}


class Tok:
    __slots__ = ("kind", "eng", "idx", "clock")

    def __init__(self, kind, eng, idx, clock):
        self.kind = kind
        self.eng = eng
        self.idx = idx
        self.clock = clock


class T:
    __slots__ = ("t", "res", "name")

    def __init__(self, t, name):
        self.t = t
        self.res = Res(name)
        self.name = name

    def __getitem__(self, k):
        return self.t[k]


class Prog:
    def __init__(self, nc, stack):
        self.nc = nc
        self.stack = stack
        self.ops = {e: [] for e in ENGS}
        self.known = {e: {} for e in ENGS}
        self.n = {e: 0 for e in ENGS}
        self.last = {}
        self.dcount = {}
        self.needed = {e: set() for e in CENG}

    def sb(self, name, shape, dt=F32):
        t = self.stack.enter_context(self.nc.sbuf_tensor(name, list(shape), dt))
        return T(t, name)

    def ps(self, name, shape, dt=F32):
        t = self.stack.enter_context(self.nc.psum_tensor(name, list(shape), dt))
        return T(t, name)

    def _need(self, eng, known, waits, tok, raw, is_dma):
        if tok is None:
            return
        if tok.kind == "c":
            if tok.eng == eng and not raw and not is_dma and (not STRICT or eng == 'pe'):
                return
            if known.get(tok.eng, 0) >= tok.idx:
                return
            waits.append(("c", tok.eng, tok.idx))
            self.needed[tok.eng].add(tok.idx)
        else:
            if known.get(tok.eng, 0) >= tok.idx:
                return
            waits.append(("d", tok.eng, tok.idx))
        for k, v in tok.clock.items():
            if known.get(k, 0) < v:
                known[k] = v
        known[tok.eng] = tok.idx

    def _wait_list(self, eng, reads, writes, is_dma):
        known = self.known[eng]
        waits = []
        for r in reads:
            self._need(eng, known, waits, r.w, True, is_dma)
        for r in writes:
            self._need(eng, known, waits, r.w, False, is_dma)
            for tk in r.rs.values():
                self._need(eng, known, waits, tk, False, is_dma)
        return waits

    @staticmethod
    def _res(x):
        return x.res if isinstance(x, T) else x

    def op(self, eng, fn, reads=(), writes=()):
        reads = [self._res(r) for r in reads]
        writes = [self._res(r) for r in writes]
        waits = self._wait_list(eng, reads, writes, False)
        self.n[eng] += 1
        idx = self.n[eng]
        clock = {k: v for k, v in self.known[eng].items() if k in CENG}
        tok = Tok("c", eng, idx, clock)
        self.ops[eng].append((waits, fn, ("c", idx)))
        self.last[eng] = tok
        for r in reads:
            r.rs[eng] = tok
        for r in writes:
            r.w = tok
            r.rs = {}
        return tok

    def dma(self, eng, out_ap, in_ap, reads=(), writes=(), key=None):
        reads = [self._res(r) for r in reads]
        writes = [self._res(r) for r in writes]
        dkey = ("d", key)
        waits = self._wait_list(eng, reads, writes, True)
        cnt = self.dcount.get(dkey, 0) + 1
        self.dcount[dkey] = cnt
        clock = {k: v for k, v in self.known[eng].items() if k in CENG}
        tok = Tok("d", dkey, cnt, clock)

        nc = self.nc

        def fn(e, out_ap=out_ap, in_ap=in_ap):
            if out_ap.dtype == F32R:
                nc.dge_precook = False
                r = e.dma_start(out=out_ap, in_=in_ap)
                nc.dge_precook = True
                return r
            return e.dma_start(out=out_ap, in_=in_ap)

        self.ops[eng].append((waits, fn, ("d", dkey)))
        self.last[dkey] = tok
        for r in reads:
            r.rs[dkey] = tok
        for r in writes:
            r.w = tok
            r.rs = {}
        return tok

    def barrier(self, skip=(), skip_keys=()):
        toks = []
        for k, tok in self.last.items():
            if tok.kind == "d" and isinstance(tok.eng[1], tuple) and tok.eng[1][0] in skip_keys:
                continue
            toks.append(tok)
        for eng in ENGS:
            if eng in skip:
                continue
            known = self.known[eng]
            waits = []
            for tok in toks:
                self._need(eng, known, waits, tok, True, True)
            if waits:
                self.ops[eng].append((waits, None, None))

    def final_wait(self, eng, toks):
        known = self.known[eng]
        waits = []
        for tok in toks:
            self._need(eng, known, waits, tok, True, True)
        self.ops[eng].append((waits, None, None))

    def emit(self):
        nc = self.nc
        rank = {}
        for e in CENG:
            s = sorted(self.needed[e])
            rank[e] = {idx: i + 1 for i, idx in enumerate(s)}
            assert len(s) < 60000, (e, len(s))
        sems = {e: self.stack.enter_context(nc.semaphore("sem_" + e)) for e in CENG}
        dsems = {}
        for dkey in self.dcount:
            dsems[dkey] = self.stack.enter_context(nc.semaphore("dsem%d" % len(dsems)))
        block = self.stack.enter_context(nc.Block())

        def run(engname, eng):
            for waits, fn, info in self.ops[engname]:
                for kind, k, idx in waits:
                    if kind == "c":
                        eng.wait_ge(sems[k], rank[k][idx])
                    else:
                        eng.wait_ge(dsems[k], 16 * idx)
                if fn is None:
                    continue
                ins = fn(eng)
                if info[0] == "c":
                    if info[1] in rank[engname]:
                        ins.then_inc(sems[engname], 1)
                else:
                    ins.then_inc(dsems[info[1]], 16)

        @block.tensor
        def _(e):
            run("pe", e)

        @block.scalar
        def _(e):
            run("act", e)

        @block.vector
        def _(e):
            run("dve", e)

        @block.gpsimd
        def _(e):
            run("pool", e)

        @block.sync
        def _(e):
            run("sp", e)


D = 1024
S = 2048
TT = 512
NSUB = 4
D_IN = 10264
C_A = 0
C_BQKV = 512
C_BBETA = 2048
C_BGATE = 2056
C_CQKV = 2568
C_DQKV = 4104
C_DLR = 5640
C_DGATE = 5656
C_MERGE = 6168
ALPHA = 8.0 ** 0.25
LN_EPS = 1e-5
NORM_EPS = 1e-6

PP_CAW = 0
PP_CAB = 16
PP_RBA = 20
PP_RBX = 24
PP_LAM = 28
PP_GCW = 32
PP_GNW = 80
PP_LNW = 81
PP_DTB = 82
PP_ALOG = 86
PP_RB = 90
PP_GLB = 106
PP_LN1G = 618
PP_LN1B = 1642
PP_LN2G = 2666
PP_LN2B = 3690
PP_ONE = 4714
PP_EPS = 4715
PP_LNEPS = 4716
PP_N = 4717

K_ID = 0
K_LE = 128
K_GE = 256
K_GT = 384
K_ONE = 512
K_LE16 = 640
K_GT16 = 768
K_LT = 896
K_SBM = 1024
K_N = 1024 + 2048


def host_consts():
    p = np.arange(128)[:, None]
    f = np.arange(128)[None, :]
    c = np.zeros((128, K_N), np.float32)
    c[:, K_ID:K_ID + 128] = (p == f)
    c[:, K_LE:K_LE + 128] = (p <= f)
    c[:, K_GE:K_GE + 128] = (p >= f)
    c[:, K_GT:K_GT + 128] = (p > f)
    c[:, K_ONE:K_ONE + 128] = 1.0
    c[:, K_LE16:K_LE16 + 128] = (p <= f) * (-1.0 / 16.0)
    c[:, K_GT16:K_GT16 + 128] = (p > f) * (-1.0 / 16.0)
    c[:, K_LT:K_LT + 128] = (p < f)
    f5 = np.arange(512)[None, :]
    for jo in range(4):
        c[:, K_SBM + jo * 512:K_SBM + (jo + 1) * 512] = (jo * 128 + p < f5)
    return c


def build(nc, L=4, NSEQ=2, taps=(), en="ABCD", moe=True):
    NTOK = NSEQ * S
    st = ExitStack()
    P = Prog(nc, st)
    dr = lambda name, shape, kind="ExternalInput": nc.dram_tensor(name, list(shape), F32, kind=kind).ap()
    x_d = dr("x", [NTOK, D])
    w_in_d = dr("w_in", [4, D, D_IN])
    w_br_d = dr("w_branch", [4, 4, 512, D])
    w_out_d = dr("w_out", [4, D, D])
    w_g_d = dr("w_gate", [4, 16, D, 512])
    w_u_d = dr("w_up", [4, 16, D, 512])
    w_d_d = dr("w_down", [4, 16, 512, D])
    w_r_d = dr("w_router", [D, 16])
    pp_d = dr("pp", [4, 128, PP_N])
    bd_d = dr("bd", [4, 2, 4, 128, 128])
    gup_d = dr("gla_up", [4, 16, 512])
    k_d = dr("consts", [128, K_N])
    out_d = dr("out", [NTOK, D], "ExternalOutput")
    xs_d = dr("xs_scr", [2, NTOK, D], "Internal")
    tap_d = {}
    for name, shape in taps:
        tap_d[name] = dr("tap_" + name, shape, "ExternalOutput")
    dres = {}

    def DR(key):
        if key not in dres:
            dres[key] = Res("dram:" + str(key))
        return dres[key]

    out_toks = []

    kc = P.sb("kc", [128, 1024])
    kb = P.sb("kb", [128, 2048], BF16)
    pp = P.sb("pp_sb", [128, PP_N])
    NB = 4
    wring = [P.sb("wr%d" % i, [128, 2048]) for i in range(NB)]
    wri = [0]
    bdw = P.sb("bdw", [128, 8, 128])
    gup = P.sb("gup", [16, 512])
    wrt = P.sb("wrt", [128, 8, 16])
    lam8 = P.sb("lam8", [128, 4])
    aexp = P.sb("aexp", [128, 4])
    nge = P.sb("nge", [128, 128], BF16)
    none_ = P.sb("none", [128, 128], BF16)
    ARENA = 36000 - 12288 - 2048
    arena = P.sb("arena", [128, ARENA])
    arenaR = P.sb("arenaR", [128, 12288])
    xst = [P.sb("xst0", [128, 1024]), P.sb("xst1", [128, 1024])]
    banks = [P.ps("bank%d" % i, [128, 512]) for i in range(8)]

    aoff = [0]

    def carve(name, ncols, shape=None, dt=F32):
        DBG[name] = (aoff[0], ncols)
        ap = arena[:, aoff[0]:aoff[0] + ncols]
        aoff[0] += ncols
        assert aoff[0] <= ARENA, (name, aoff[0])
        if dt == BF16:
            ap = ap.bitcast(BF16)
        if shape is not None and len(shape) == 3:
            ap = ap.rearrange("p (a b) -> p a b", a=shape[1])
        return T(ap, name)

    roff = [0]

    def carveR(name, ncols, shape=None):
        ap = arenaR[:, roff[0]:roff[0] + ncols]
        roff[0] += ncols
        assert roff[0] <= 12288
        if shape is not None and len(shape) == 3:
            ap = ap.rearrange("p (a b) -> p a b", a=shape[1])
        return T(ap, name)

    def r32(ap):
        return ap.bitcast(F32R)

    def v3(ap, a):
        return ap.rearrange("p (a b) -> p a b", a=a)

    def mm(out_t, out_ap, l_t, l_ap, r_t, r_ap, start, stop):
        P.op("pe", lambda e: e.matmul(out_ap, l_ap, r_ap, start=start, stop=stop), reads=[l_t, r_t], writes=[out_t])

    def tr(out_t, out_ap, in_t, in_ap):
        P.op("pe", lambda e: e.transpose(out_ap, in_ap, kc[:, K_ID:K_ID + 128]), reads=[in_t, kc], writes=[out_t])

    def act(out_t, out_ap, in_t, in_ap, func, bias=None, scale=None, extra=()):
        kw = {}
        if bias is not None:
            kw["bias"] = bias
        if scale is not None:
            kw["scale"] = scale
        P.op("act", lambda e: e.activation(out_ap, in_ap, func, **kw), reads=[in_t] + list(extra), writes=[out_t])

    def ts(eng, out_t, out_ap, in_t, in_ap, s1, s2, op0, op1=None, extra=()):
        if op1 is None:
            P.op(eng, lambda e: e.tensor_scalar(out_ap, in_ap, s1, None, op0), reads=[in_t] + list(extra), writes=[out_t])
        else:
            P.op(eng, lambda e: e.tensor_scalar(out_ap, in_ap, s1, s2, op0, op1), reads=[in_t] + list(extra), writes=[out_t])

    def tt(eng, out_t, out_ap, a_t, a_ap, b_t, b_ap, op):
        P.op(eng, lambda e: e.tensor_tensor(out_ap, a_ap, b_ap, op), reads=[a_t, b_t], writes=[out_t])

    def stt(out_t, out_ap, a_t, a_ap, sc, b_t, b_ap, op0, op1, extra=()):
        P.op("dve", lambda e: e.scalar_tensor_tensor(out_ap, a_ap, sc, b_ap, op0, op1),
             reads=[a_t, b_t] + list(extra), writes=[out_t])

    def cp(eng, out_t, out_ap, in_t, in_ap):
        if eng == "act":
            P.op("act", lambda e: e.copy(out_ap, in_ap), reads=[in_t], writes=[out_t])
        else:
            P.op(eng, lambda e: e.tensor_copy(out_ap, in_ap), reads=[in_t], writes=[out_t])

    def memset(eng, out_t, out_ap, val):
        P.op(eng, lambda e: e.memset(out_ap, val), writes=[out_t])

    def wload(src_ap):
        buf = wring[wri[0] % NB]
        wri[0] += 1
        n = 1
        for s_ in src_ap.shape[1:]:
            n *= s_
        dst = buf[:, 0:n]
        if len(src_ap.shape) == 3:
            dst = dst.rearrange("p (a b) -> p a b", a=src_ap.shape[1])
        P.dma("sp", dst.bitcast(F32R), src_ap.bitcast(F32R), writes=[buf], key=("wr", buf.name))
        return buf, dst

    def tap(name, src_t, src_ap, dst_ap_fn):
        if name in tap_d:
            tk = P.dma("pool", dst_ap_fn(tap_d[name]), src_ap, reads=[src_t], key=("tap", name))
            out_toks.append(tk)

    def barrier():
        P.barrier(skip=("sp",), skip_keys=("wr",))

    P.dma("sp", kc[:], k_d[:, 0:1024], writes=[kc], key="kc")
    P.dma("sp", arena[:, 0:2048], k_d[:, 1024:3072], writes=[arena], key="kcm")
    cp("dve", kb, kb[:], arena, arena[:, 0:2048])
    P.barrier()
    P.dma("sp", wrt[:], w_r_d.rearrange("(k p) e -> p k e", p=128), writes=[wrt], key="wrt")
    ts("dve", nge, nge[:], kc, kc[:, K_GE:K_GE + 128], -1.0, None, ALU.mult)
    ts("dve", none_, none_[:], kc, kc[:, K_ONE:K_ONE + 128], -1.0, None, ALU.mult)
    ident = kc[:, K_ID:K_ID + 128]
    ones = kc[:, K_ONE:K_ONE + 128]

    def win(l, c0, n):
        return wload(w_in_d[l].rearrange("(k p) n -> p k n", p=128)[:, :, c0:c0 + n])

    for l in range(L):
        barrier()
        P.dma("sp", pp[:], pp_d[l], writes=[pp], key="pp")
        P.dma("sp", bdw[:], bd_d[l].rearrange("a c p j -> p (a c) j"), writes=[bdw], key="bdw")
        P.dma("sp", gup[:], gup_d[l], writes=[gup], key="gup")
        act(lam8, lam8[:], pp, pp[:, PP_LAM:PP_LAM + 4], AF.Exp, scale=-1.0)
        act(lam8, lam8[:], lam8, lam8[:], AF.Ln, bias=pp[:, PP_ONE:PP_ONE + 1], extra=[pp])
        ts("dve", lam8, lam8[:], lam8, lam8[:], -8.0, None, ALU.mult)
        act(aexp, aexp[:], pp, pp[:, PP_ALOG:PP_ALOG + 4], AF.Exp)
        ts("dve", aexp, aexp[:], aexp, aexp[:], -1.0, None, ALU.mult)

        for s in range(NSEQ):
            barrier()
            aoff[0] = 0
            roff[0] = 0
            xT = carveR("xT", 4096, [128, 8, 512])
            merged = carveR("merged", 4096, [128, 8, 512])
            yn = carveR("yn", 2048, [128, 4, 512])
            KT = carve("KT", 4096, [128, 4, 2048], BF16)
            Vc = carve("Vc", 4096, [128, 16, 512], BF16)
            utail = carve("utail", 64, [128, 16, 4])
            hst = carve("hst", 4)
            S_d = carve("S_d", 512, [128, 4, 128])
            S_b = carve("S_b", 512, [128, 4, 128])
            S_db = carve("S_db", 256, [128, 4, 128], BF16)
            gsig = carve("gsig", 512)
            tmpm = carve("tmpm", 512)
            OV = aoff[0]
            memset("pool", utail, utail[:], 0.0)
            memset("pool", hst, hst[:], 0.0)
            memset("pool", S_d, S_d[:], 0.0)
            memset("pool", S_b, S_b[:], 0.0)
            memset("pool", S_db, S_db[:], 0.0)

            for c in range(NSUB):
                row0 = s * S + c * TT

                def proj_fm(bk, wb, wap, j0, ncols):
                    for k in range(8):
                        mm(bk, bk[0:ncols, :], wb, r32(wap[:, k, j0:j0 + ncols]), xT, r32(xT[:, k, :]), k == 0, k == 7)

                def proj_tm(bk, sub, wb, wap, j0, ncols):
                    for k in range(8):
                        mm(bk, bk[:, 0:ncols], xT, r32(xT[:, k, sub * 128:(sub + 1) * 128]), wb, r32(wap[:, k, j0:j0 + ncols]),
                           k == 0, k == 7)

                def branch_merge(n, first):
                    for dq in range(4):
                        wbb, wbap = wload(w_br_d[l, n].rearrange("(k p) n -> p k n", p=128)[:, :, dq * 256:(dq + 1) * 256])
                        wgb, wgap = win(l, C_MERGE + n * 1024 + dq * 256, 256)
                        for h_ in range(2):
                            dch = dq * 2 + h_
                            bp = banks[4 + (dch % 2)]
                            bg = banks[6 + (dch % 2)]
                            for k in range(4):
                                mm(bp, bp[:, :], wbb, r32(wbap[:, k, h_ * 128:(h_ + 1) * 128]), yn, r32(yn[:, k, :]), k == 0, k == 3)
                            proj_fm(bg, wgb, wgap, h_ * 128, 128)
                            act(gsig, gsig[:], bg, bg[:], AF.Sigmoid)
                            if first:
                                tt("dve", merged, r32(merged[:, dch, :]), bp, bp[:], gsig, gsig[:], ALU.mult)
                            else:
                                tt("dve", tmpm, tmpm[:], bp, bp[:], gsig, gsig[:], ALU.mult)
                                tt("pool", merged, r32(merged[:, dch, :]), merged, merged[:, dch, :], tmpm, tmpm[:], ALU.add)

                def gated_norm(o_t, o_ap, h, gate_c0, nw_col, t1, t2, t3):
                    act(t1, t1[:], o_t, o_ap, AF.Square)
                    bs = banks[6]
                    mm(bs, bs[:], kc, ones, t1, t1[:], True, True)
                    act(t2, t2[:], bs, bs[:], AF.Ln, bias=pp[:, PP_EPS:PP_EPS + 1], scale=1.0 / 128.0, extra=[pp])
                    act(t2, t2[:], t2, t2[:], AF.Exp, scale=-0.5)
                    wgb, wgap = win(l, gate_c0 + h * 128, 128)
                    bg = banks[7]
                    proj_fm(bg, wgb, wgap, 0, 128)
                    act(t3, t3[:], bg, bg[:], AF.Silu)
                    stt(t1, t1[:], o_t, o_ap, pp[:, nw_col:nw_col + 1], t2, t2[:], ALU.mult, ALU.mult, extra=[pp])
                    tt("dve", yn, r32(yn[:, h, :]), t1, t1[:], t3, t3[:], ALU.mult)

                barrier()
                for a in range(4):
                    xs_ = xst[a % 2]
                    r0 = row0 + a * 128
                    if l == 0:
                        P.dma("sp", r32(xs_[:]), r32(x_d[r0:r0 + 128, :]), writes=[xs_], key=("xst", a % 2))
                    else:
                        P.dma("sp", r32(xs_[:]), r32(xs_d[0, r0:r0 + 128, :]), reads=[DR(("x0", s, c))], writes=[xs_], key=("xst", a % 2))
                    for hf in range(2):
                        bk = banks[hf]
                        for k in range(4):
                            kk = hf * 4 + k
                            tr(bk, bk[:, k * 128:(k + 1) * 128], xs_, xs_[:, kk * 128:(kk + 1) * 128])
                        cp("act" if hf else "dve", xT, r32(xT[:, hf * 4:hf * 4 + 4, a * 128:(a + 1) * 128]), bk, v3(bk[:], 4))

                first = True
                if "A" in en:
                    barrier()
                    aoff[0] = OV
                    ubuf = carve("ubuf", 516)
                    t1 = carve("t1", 512)
                    t2 = carve("t2", 512)
                    t3 = carve("t3", 512)
                    t4 = carve("t4", 512)
                    wa = [win(l, C_A, 256), win(l, C_A + 256, 256)]
                    for ch in range(4):
                        bk = banks[ch % 2]
                        proj_fm(bk, wa[ch // 2][0], wa[ch // 2][1], (ch % 2) * 128, 128)
                        cp("dve", ubuf, ubuf[:, 0:3], utail, utail[:, ch, 0:3])
                        cp("act", ubuf, ubuf[:, 3:515], bk, bk[:])
                        cp("pool", utail, utail[:, ch, 0:3], ubuf, ubuf[:, 512:515])
                        cw = lambda k: pp[:, PP_CAW + ch * 4 + k:PP_CAW + ch * 4 + k + 1]
                        ts("dve", t1, t1[:], ubuf, ubuf[:, 3:515], cw(3), pp[:, PP_CAB + ch:PP_CAB + ch + 1], ALU.mult, ALU.add, extra=[pp])
                        for k in range(3):
                            stt(t1, t1[:], ubuf, ubuf[:, k:k + 512], cw(k), t1, t1[:], ALU.mult, ALU.add, extra=[pp])
                        ba = banks[2]
                        bx = banks[3]
                        mm(ba, ba[:], bdw, bdw[:, ch, :], t1, t1[:], True, True)
                        mm(bx, bx[:], bdw, bdw[:, 4 + ch, :], t1, t1[:], True, True)
                        act(t2, t2[:], ba, ba[:], AF.Sigmoid, bias=pp[:, PP_RBA + ch:PP_RBA + ch + 1], extra=[pp])
                        act(t3, t3[:], bx, bx[:], AF.Sigmoid, bias=pp[:, PP_RBX + ch:PP_RBX + ch + 1], extra=[pp])
                        act(t2, t2[:], t2, t2[:], AF.Exp, scale=lam8[:, ch:ch + 1], extra=[lam8])
                        tt("dve", t4, t4[:], t2, t2[:], t2, t2[:], ALU.mult)
                        act(t4, t4[:], t4, t4[:], AF.Sqrt, bias=pp[:, PP_ONE:PP_ONE + 1], scale=-1.0, extra=[pp])
                        if c == 0:
                            memset("dve", t4, t4[:, 0:1], 1.0)
                        tt("dve", t3, t3[:], t3, t3[:], t1, t1[:], ALU.mult)
                        tt("dve", t3, t3[:], t3, t3[:], t4, t4[:], ALU.mult)
                        P.op("dve", lambda e, o_=r32(yn[:, ch, :]), a_=t2[:], b_=t3[:], i_=hst[:, ch:ch + 1]: e.tensor_tensor_scan(o_, a_, b_, i_, ALU.mult, ALU.add),
                             reads=[t2, t3, hst], writes=[yn])
                        cp("pool", hst, hst[:, ch:ch + 1], yn, yn[:, ch, 511:512])
                    tap("y_a", yn, yn[:], lambda d_: d_[:, :, c * TT:(c + 1) * TT])
                    branch_merge(0, first)
                    first = False

                if "B" in en:
                    barrier()
                    aoff[0] = OV
                    qnT = carve("qnT", 2048, [128, 4, 512])
                    knT = carve("knT", 2048, [128, 4, 512])
                    vT = carve("vT", 2048, [128, 4, 512])
                    ubuf = carve("ubuf", 516)
                    t1 = carve("t1", 512)
                    t2 = carve("t2", 512)
                    t3 = carve("t3", 512)
                    beta_t = carve("beta_t", 16)
                    g_t = carve("g_t", 16)
                    gcc = carve("gcc", 16)
                    bexp = carve("bexp", 16)
                    kdsc = carve("kdsc", 16)
                    gl = carve("gl", 16)
                    tsm = carve("tsm", 16)
                    tg = carve("tg", 128)
                    tmp1 = carve("tmp1", 128)
                    tmp2 = carve("tmp2", 128)
                    e1m = carve("e1m", 128)
                    e2m = carve("e2m", 128)
                    qdec = carve("qdec", 128)
                    AAT = [carve("AAT0", 256), carve("AAT1", 256)]
                    PT = carve("PT", 128)
                    attn_s = carve("attn_s", 128)
                    kbg = carve("kbg", 128)
                    kdec = carve("kdec", 128)
                    vb = carve("vb", 128)
                    u_s = carve("u_s", 128)
                    wT_s = carve("wT_s", 128)
                    vn_s = carve("vn_s", 128)
                    dests = [qnT, knT, vT]
                    for un in range(6):
                        wq = win(l, C_BQKV + un * 256, 256)
                        for h_ in range(2):
                            ci = un * 2 + h_
                            bk = banks[ci % 2]
                            proj_fm(bk, wq[0], wq[1], h_ * 128, 128)
                            cp("dve", ubuf, ubuf[:, 0:3], utail, utail[:, 4 + ci, 0:3])
                            cp("act", ubuf, ubuf[:, 3:515], bk, bk[:])
                            cp("pool", utail, utail[:, 4 + ci, 0:3], ubuf, ubuf[:, 512:515])
                            cw = lambda k: pp[:, PP_GCW + ci * 4 + k:PP_GCW + ci * 4 + k + 1]
                            ts("dve", t1, t1[:], ubuf, ubuf[:, 3:515], cw(3), None, ALU.mult, extra=[pp])
                            for k in range(3):
                                stt(t1, t1[:], ubuf, ubuf[:, k:k + 512], cw(k), t1, t1[:], ALU.mult, ALU.add, extra=[pp])
                            dst = dests[ci // 4]
                            act(dst, dst[:, ci % 4, :], t1, t1[:], AF.Silu)
                    for qi, dst in enumerate((qnT, knT)):
                        for h in range(4):
                            act(t1, t1[:], dst, dst[:, h, :], AF.Square)
                            bs = banks[2 + h % 2]
                            mm(bs, bs[:], kc, ones, t1, t1[:], True, True)
                            act(t2, t2[:], bs, bs[:], AF.Ln, bias=pp[:, PP_EPS:PP_EPS + 1], extra=[pp])
                            act(t2, t2[:], t2, t2[:], AF.Exp, scale=-0.5)
                            if qi == 0:
                                stt(dst, dst[:, h, :], dst, dst[:, h, :], 128.0 ** -0.5, t2, t2[:], ALU.mult, ALU.mult)
                            else:
                                tt("dve", dst, dst[:, h, :], dst, dst[:, h, :], t2, t2[:], ALU.mult)
                    wbg = win(l, C_BBETA, 8)
                    bsm = banks[4]
                    for sub in range(4):
                        for k in range(8):
                            mm(bsm, bsm[:, sub * 8:(sub + 1) * 8], xT, r32(xT[:, k, sub * 128:(sub + 1) * 128]), wbg[0], r32(wbg[1][:, k, 0:8]), k == 0, k == 7)
                    bsm3 = v3(bsm[:, 0:32], 4)
                    b3 = lambda t_: v3(t_[:], 4)
                    act(beta_t, b3(beta_t), bsm, bsm3[:, :, 0:4], AF.Sigmoid)
                    cp("act", tsm, b3(tsm), bsm, bsm3[:, :, 4:8])
                    tt("dve", tsm, b3(tsm), tsm, b3(tsm), pp, pp[:, PP_DTB:PP_DTB + 4].unsqueeze(1).to_broadcast([128, 4, 4]), ALU.add)
                    act(tsm, tsm[:], tsm, tsm[:], AF.Exp)
                    act(tsm, tsm[:], tsm, tsm[:], AF.Ln, bias=pp[:, PP_ONE:PP_ONE + 1], extra=[pp])
                    tt("dve", g_t, b3(g_t), tsm, b3(tsm), aexp, aexp[:].unsqueeze(1).to_broadcast([128, 4, 4]), ALU.mult)
                    bsm2 = banks[5]
                    for sub in range(4):
                        mm(bsm2, bsm2[:, sub * 4:(sub + 1) * 4], kc, kc[:, K_LE:K_LE + 128], g_t, g_t[:, sub * 4:(sub + 1) * 4], True, True)
                    for sub in range(4):
                        mm(bsm2, bsm2[:, 16 + sub * 4:16 + (sub + 1) * 4], kc, ones, g_t, g_t[:, sub * 4:(sub + 1) * 4], True, True)
                    cp("act", gcc, gcc[:], bsm2, bsm2[:, 0:16])
                    act(bexp, bexp[:], bsm2, bsm2[:, 0:16], AF.Exp)
                    tt("dve", bexp, bexp[:], bexp, bexp[:], beta_t, beta_t[:], ALU.mult)
                    act(gl, gl[:], bsm2, bsm2[:, 16:32], AF.Exp)
                    cp("act", kdsc, kdsc[:], bsm2, bsm2[:, 16:32])
                    tt("dve", kdsc, kdsc[:], kdsc, kdsc[:], gcc, gcc[:], ALU.subtract)
                    act(kdsc, kdsc[:], kdsc, kdsc[:], AF.Exp)
                    import os
                    GS = int(os.environ.get("GDN_STOP", "99"))
                    for h in range(4 if GS > 0 else 0):
                        bo = banks[5]
                        for sub in range(4):
                            cs = slice(sub * 128, (sub + 1) * 128)
                            si = sub * 4 + h
                            b1 = banks[0]
                            b2 = banks[1]
                            ts("dve", tg, tg[:], kc, kc[:, K_LE:K_LE + 128], g_t[:, si:si + 1], None, ALU.mult, extra=[g_t])
                            mm(b1, b1[:, 0:128], kc, ones, tg, tg[:], True, True)
                            mm(b1, b1[:, 128:256], knT, knT[:, h, cs], knT, knT[:, h, cs], True, True)
                            mm(b1, b1[:, 256:384], knT, knT[:, h, cs], qnT, qnT[:, h, cs], True, True)
                            mm(b1, b1[:, 384:512], knT, knT[:, h, cs], kc, ident, True, True)
                            mm(b2, b2[:, 0:128], vT, vT[:, h, cs], kc, ident, True, True)
                            ts("dve", tmp1, tmp1[:], b1, b1[:, 0:128], gcc[:, si:si + 1], 0.0, ALU.subtract, ALU.min, extra=[gcc])
                            ts("dve", tmp2, tmp2[:], b1, b1[:, 0:128], gcc[:, si:si + 1], 0.0, ALU.subtract, ALU.max, extra=[gcc])
                            act(tmp1, tmp1[:], tmp1, tmp1[:], AF.Exp)
                            act(tmp2, tmp2[:], tmp2, tmp2[:], AF.Exp, scale=-1.0)
                            tt("pool", e1m, e1m[:], tmp1, tmp1[:], kc, kc[:, K_LE:K_LE + 128], ALU.mult)
                            tt("pool", e2m, e2m[:], tmp2, tmp2[:], kc, kc[:, K_GT:K_GT + 128], ALU.mult)
                            act(tg, tg[:], b1, b1[:, 0:128], AF.Exp)
                            tt("dve", qdec, qdec[:], qnT, qnT[:, h, cs], tg, tg[:], ALU.mult)
                            cur = 0
                            stt(AAT[0], AAT[0][:, 0:128], b1, b1[:, 128:256], beta_t[:, si:si + 1], e2m, e2m[:], ALU.mult, ALU.mult, extra=[beta_t])
                            tt("dve", attn_s, attn_s[:], b1, b1[:, 256:384], e1m, e1m[:], ALU.mult)
                            ts("dve", kbg, kbg[:], b1, b1[:, 384:512], bexp[:, si:si + 1], None, ALU.mult, extra=[bexp])
                            ts("dve", kdec, kdec[:], b1, b1[:, 384:512], kdsc[:, si:si + 1], None, ALU.mult, extra=[kdsc])
                            ts("dve", vb, vb[:], b2, b2[:, 0:128], beta_t[:, si:si + 1], None, ALU.mult, extra=[beta_t])
                            if GS < 2:
                                continue
                            GV = int(os.environ.get("GDN_V", "9"))
                            mm(b2, b2[:, 128:256], AAT[0], AAT[0][:, 0:128], kc, ident, True, True)
                            if GV >= 2:
                                cp("act", AAT[0], AAT[0][:, 128:256], b2, b2[:, 128:256])
                            if GV >= 3:
                                act(PT, PT[:], b2, b2[:, 128:256], AF.Copy, scale=-1.0)
                                tt("pool", PT, PT[:], PT, PT[:], kc, ident, ALU.add)
                            for m in range(1, 1 + int(os.environ.get('GDN_LV', '6'))):
                                b3_ = banks[2 + m % 2]
                                A_c = AAT[cur]
                                A_n = AAT[1 - cur]
                                mm(b3_, b3_[:, 0:128], A_c, A_c[:, 128:256], A_c, A_c[:, 0:128], True, True)
                                if m < 6:
                                    mm(b3_, b3_[:, 128:256], A_c, A_c[:, 0:128], A_c, A_c[:, 128:256], True, True)
                                    cp("act", A_n, A_n[:, 0:256], b3_, b3_[:, 0:256])
                                else:
                                    cp("act", A_n, A_n[:, 0:128], b3_, b3_[:, 0:128])
                                mm(b3_, b3_[:, 256:384], A_n, A_n[:, 0:128], PT, PT[:], True, True)
                                tt("dve", PT, PT[:], PT, PT[:], b3_, b3_[:, 256:384], ALU.add)
                                cur = 1 - cur
                            if GS < 3:
                                continue
                            b5 = banks[4]
                            mm(b5, b5[:, 0:128], PT, PT[:], vb, vb[:], True, True)
                            mm(b5, b5[:, 128:256], kbg, kbg[:], PT, PT[:], True, True)
                            cp("act", u_s, u_s[:], b5, b5[:, 0:128])
                            cp("act", wT_s, wT_s[:], b5, b5[:, 128:256])
                            mm(b5, b5[:, 256:384], wT_s, wT_s[:], S_b, S_b[:, h, :], True, True)
                            tt("dve", vn_s, vn_s[:], u_s, u_s[:], b5, b5[:, 256:384], ALU.subtract)
                            mm(bo, bo[:, cs], S_b, S_b[:, h, :], qdec, qdec[:], True, False)
                            mm(bo, bo[:, cs], vn_s, vn_s[:], attn_s, attn_s[:], False, True)
                            mm(b5, b5[:, 384:512], kdec, kdec[:], vn_s, vn_s[:], True, True)
                            stt(S_b, S_b[:, h, :], S_b, S_b[:, h, :], gl[:, si:si + 1], b5, b5[:, 384:512], ALU.mult, ALU.add, extra=[gl])
                        if GS >= 3:
                            gated_norm(bo, bo[:], h, C_BGATE, PP_GNW, t1, t2, t3)
                    tap("y_b", yn, yn[:], lambda d_: d_[:, :, c * TT:(c + 1) * TT])
                    branch_merge(1, first)
                    first = False

                if "C" in en:
                    barrier()
                    aoff[0] = OV
                    qTb = carve("qTb", 2048, [128, 8, 512], BF16)
                    memset("pool", qTb, qTb[:], 0.0)
                    e_sb = carve("e_sb", 512)
                    sp_sb = carve("sp_sb", 256, None, BF16)
                    spsum = carve("spsum", 512)
                    spsum_b = carve("spsum_b", 256, None, BF16)
                    w_sb = carve("w_sb", 256, None, BF16)
                    qb0 = c * 4
                    for u in range(2):
                        wq = win(l, C_CQKV + u * 256, 256)
                        wk = win(l, C_CQKV + 512 + u * 256, 256)
                        for h_ in range(2):
                            pr = u * 2 + h_
                            bq = banks[0 + h_]
                            proj_fm(bq, wq[0], wq[1], h_ * 128, 128)
                            ts("dve", qTb, qTb[0:64, 2 * pr, :], bq, bq[0:64, :], 0.125, None, ALU.mult)
                            ts("dve", qTb, qTb[64:128, 2 * pr + 1, :], bq, bq[64:128, :], 0.125, None, ALU.mult)
                            bk_ = banks[2 + h_]
                            proj_fm(bk_, wk[0], wk[1], h_ * 128, 128)
                            cp("act", KT, KT[:, pr, c * TT:(c + 1) * TT], bk_, bk_[:])
                    wv0 = win(l, C_CQKV + 1024, 256)
                    wv1 = win(l, C_CQKV + 1280, 256)
                    for sub in range(4):
                        bv = banks[sub % 2]
                        proj_tm(bv, sub, wv0[0], wv0[1], 0, 256)
                        cp("act", Vc, Vc[:, qb0 + sub, 0:256], bv, bv[:, 0:256])
                        bv2 = banks[2 + sub % 2]
                        proj_tm(bv2, sub, wv1[0], wv1[1], 0, 256)
                        cp("dve", Vc, Vc[:, qb0 + sub, 256:512], bv2, bv2[:, 0:256])
                    for pr in range(4):
                        for hh in range(2):
                            bo = banks[6 + hh]
                            h = pr * 2 + hh
                            ps_ = slice(hh * 64, hh * 64 + 64)
                            nkb = qb0 + 4
                            for step, J in enumerate(range(nkb - 1, -1, -1)):
                                jo = J - qb0
                                bz = banks[step % 2]
                                bd_ = banks[2 + step % 2]
                                mm(bz, bz[:], KT, KT[:, pr, J * 128:(J + 1) * 128], qTb, qTb[:, h, :], True, True)
                                act(e_sb, e_sb[:], bz, bz[:], AF.Exp)
                                act(sp_sb, sp_sb[:], e_sb, e_sb[:], AF.Ln, bias=pp[:, PP_ONE:PP_ONE + 1], extra=[pp])
                                if jo >= 0:
                                    tt("pool", sp_sb, sp_sb[:], sp_sb, sp_sb[:], kb, kb[:, jo * 512:(jo + 1) * 512], ALU.mult)
                                mm(bd_, bd_[:], KT, KT[:, pr, J * 128:(J + 1) * 128], qTb, qTb[:, h, :], True, False)
                                mm(bd_, bd_[:], nge, nge[:], sp_sb, sp_sb[:], False, step == 0)
                                if step > 0:
                                    mm(bd_, bd_[:], none_, none_[:], spsum_b, spsum_b[:], False, True)
                                act(w_sb, w_sb[:], bd_, bd_[:], AF.Exp)
                                if jo >= 0:
                                    tt("pool", w_sb, w_sb[:], w_sb, w_sb[:], kb, kb[:, jo * 512:(jo + 1) * 512], ALU.mult)
                                mm(bo, bo[:], Vc, Vc[:, J, pr * 128:(pr + 1) * 128], w_sb, w_sb[:], step == 0, J == 0)
                                if J > 0:
                                    if step == 0:
                                        cp("dve", spsum, spsum[:], sp_sb, sp_sb[:])
                                    else:
                                        tt("dve", spsum, spsum[:], spsum, spsum[:], sp_sb, sp_sb[:], ALU.add)
                                    cp("pool", spsum_b, spsum_b[:], spsum, spsum[:])
                            cp("act", yn, r32(yn[ps_, pr, :]), bo, bo[ps_, :])
                    tap("y_c", yn, yn[:], lambda d_: d_[:, :, c * TT:(c + 1) * TT])
                    branch_merge(2, first)
                    first = False

                if "D" in en:
                    barrier()
                    aoff[0] = OV
                    t1 = carve("t1", 512)
                    t2 = carve("t2", 512)
                    t3 = carve("t3", 512)
                    sp_tok = carve("sp_tok", 2048, [128, 4, 512])
                    qd = carve("qd", 1024, [128, 4, 512], BF16)
                    ki = carve("ki", 1024, [128, 4, 512], BF16)
                    kdt = carve("kdt", 1024, [128, 4, 512], BF16)
                    vtk = carve("vtk", 1024, [128, 4, 512], BF16)
                    lrT = carve("lrT", 512)
                    glast = carve("glast", 16)
                    attn = carve("attn", 64, None, BF16)
                    wl = win(l, C_DLR, 16)
                    b0 = banks[0]
                    proj_fm(b0, wl[0], wl[1], 0, 16)
                    cp("act", lrT, lrT[0:16, :], b0, b0[0:16, :])
                    for sub in range(4):
                        bg_ = banks[1 + sub % 2]
                        mm(bg_, bg_[:], lrT, lrT[0:16, sub * 128:(sub + 1) * 128], gup, gup[:], True, True)
                        tt("dve", t1, t1[:], bg_, bg_[:], pp, pp[:, PP_GLB:PP_GLB + 512], ALU.add)
                        act(t1, t1[:], t1, t1[:], AF.Exp, scale=-1.0)
                        act(sp_tok, sp_tok[:, sub, :], t1, t1[:], AF.Ln, bias=pp[:, PP_ONE:PP_ONE + 1], extra=[pp])
                    wk0 = win(l, C_DQKV + 512, 256)
                    wk1 = win(l, C_DQKV + 768, 256)
                    for sub in range(4):
                        br = banks[3]
                        mm(br, br[:], kc, kc[:, K_GT16:K_GT16 + 128], sp_tok, sp_tok[:, sub, :], True, True)
                        act(t2, t2[:], br, br[:], AF.Exp)
                        for hf, wk_ in enumerate((wk0, wk1)):
                            bkk = banks[4 + hf]
                            proj_tm(bkk, sub, wk_[0], wk_[1], 0, 256)
                            tt("dve", kdt, kdt[:, sub, hf * 256:(hf + 1) * 256], bkk, bkk[:, 0:256], t2, t2[:, hf * 256:(hf + 1) * 256], ALU.mult)
                    wv0 = win(l, C_DQKV + 1024, 256)
                    wv1 = win(l, C_DQKV + 1280, 256)
                    for sub in range(4):
                        for hf, wvv in enumerate((wv0, wv1)):
                            bvv = banks[6 + hf]
                            proj_tm(bvv, sub, wvv[0], wvv[1], 0, 256)
                            cp("act", vtk, vtk[:, sub, hf * 256:(hf + 1) * 256], bvv, bvv[:, 0:256])
                    for h in range(4):
                        bb = banks[0]
                        for sub in range(4):
                            mm(bb, bb[:, sub * 128:(sub + 1) * 128], sp_tok, sp_tok[:, sub, h * 128:(h + 1) * 128],
                               kc, kc[:, K_LE16:K_LE16 + 128], True, True)
                        act(t1, t1[:], bb, bb[:], AF.Exp)
                        act(t2, t2[:], bb, bb[:], AF.Exp, scale=-1.0)
                        for sub in range(4):
                            cp("pool", glast, glast[:, h * 4 + sub:h * 4 + sub + 1], t1, t1[:, sub * 128 + 127:sub * 128 + 128])
                        wq = win(l, C_DQKV + h * 128, 128)
                        bq = banks[1]
                        proj_fm(bq, wq[0], wq[1], 0, 128)
                        stt(qd, qd[:, h, :], bq, bq[:], 128.0 ** -0.5, t1, t1[:], ALU.mult, ALU.mult)
                        wkf = win(l, C_DQKV + 512 + h * 128, 128)
                        bk2 = banks[2]
                        proj_fm(bk2, wkf[0], wkf[1], 0, 128)
                        tt("dve", ki, ki[:, h, :], bk2, bk2[:], t2, t2[:], ALU.mult)
                    for h in range(4):
                        bo = banks[3 + h % 2]
                        for sub in range(4):
                            cs = slice(sub * 128, (sub + 1) * 128)
                            hs = slice(h * 128, (h + 1) * 128)
                            ba_ = banks[5]
                            mm(ba_, ba_[:, 0:128], ki, ki[:, h, cs], qd, qd[:, h, cs], True, True)
                            tt("dve", attn, attn[:], ba_, ba_[:, 0:128], kc, kc[:, K_LE:K_LE + 128], ALU.mult)
                            mm(bo, bo[:, cs], vtk, vtk[:, sub, hs], attn, attn[:], True, False)
                            mm(bo, bo[:, cs], S_db, S_db[:, h, :], qd, qd[:, h, cs], False, True)
                            bs_ = banks[6]
                            mm(bs_, bs_[:, 0:128], kdt, kdt[:, sub, hs], vtk, vtk[:, sub, hs], True, True)
                            stt(S_d, S_d[:, h, :], S_d, S_d[:, h, :], glast[:, h * 4 + sub:h * 4 + sub + 1], bs_, bs_[:, 0:128],
                                ALU.mult, ALU.add, extra=[glast])
                            cp("pool", S_db, S_db[:, h, :], S_d, S_d[:, h, :])
                        gated_norm(bo, bo[:], h, C_DGATE, PP_LNW, t1, t2, t3)
                    tap("y_d", yn, yn[:], lambda d_: d_[:, :, c * TT:(c + 1) * TT])
                    branch_merge(3, first)
                    first = False

                barrier()
                aoff[0] = OV
                x_tok = carve("x_tok", 4096, [128, 4, 1024])
                lnt = carve("lnt", 64)
                src = (x_d if l == 0 else xs_d[0])[row0:row0 + TT, :]
                P.dma("pool", x_tok[:], src.rearrange("(a p) d -> p a d", p=128),
                      reads=([DR(("x0", s, c))] if l > 0 else []), writes=[x_tok], key="xtok")
                for dq in range(4):
                    wo = wload(w_out_d[l].rearrange("(k p) n -> p k n", p=128)[:, :, dq * 256:(dq + 1) * 256])
                    for sub in range(4):
                        bo = banks[sub % 4]
                        for k in range(8):
                            mm(bo, bo[:, 0:256], merged, r32(merged[:, k, sub * 128:(sub + 1) * 128]), wo[0], r32(wo[1][:, k, :]), k == 0, k == 7)
                        stt(x_tok, x_tok[:, sub, dq * 256:(dq + 1) * 256], x_tok, x_tok[:, sub, dq * 256:(dq + 1) * 256], ALPHA,
                            bo, bo[:, 0:256], ALU.mult, ALU.add)
                for sub in range(4):
                    layer_norm(P, x_tok, x_tok[:, sub, :], lnt, pp, PP_LN1G, PP_LN1B)
                tap("x1", x_tok, x_tok[:], lambda d_: d_[c * TT:(c + 1) * TT, :].rearrange("(a p) d -> p a d", p=128))
                dst_d = xs_d[1] if moe else (out_d if l == L - 1 else xs_d[0])
                tk = P.dma("pool", dst_d[row0:row0 + TT, :].rearrange("(a p) d -> p a d", p=128), x_tok[:],
                           reads=[x_tok], writes=[DR((("x1" if moe else "x0"), s, c))], key="xst_out")
                if not moe and l == L - 1:
                    out_toks.append(tk)

            if not moe:
                continue
            for g in range(2):
                barrier()
                aoff[0] = 0
                roff[0] = 0
                x1T = carveR("x1T", 8192, [128, 8, 1024])
                hT = [carveR("hT0", 2048, [128, 2, 1024]), carveR("hT1", 2048, [128, 2, 1024])]
                yacc = carve("yacc", 8192, [128, 8, 1024])
                sg = [carve("sg0", 512), carve("sg1", 512)]
                sc = carve("sc", 128, [128, 8, 16])
                bi = carve("bi", 128, [128, 8, 16])
                mb = carve("mb", 128, [128, 8, 16])
                eq = carve("eq", 128, [128, 8, 16])
                sel = carve("sel", 128, [128, 8, 16])
                comb = carve("comb", 128, [128, 8, 16])
                m1 = carve("m1", 32)
                m2 = carve("m2", 32)
                gsel = carve("gsel", 32)
                gm = carve("gm", 8)
                lnt = carve("lnt", 64)
                grow0 = s * S + g * 1024
                P.dma("pool", yacc[:], xs_d[1, grow0:grow0 + 1024, :].rearrange("(a p) d -> p a d", p=128),
                      reads=[DR(("x1", s, 2 * g)), DR(("x1", s, 2 * g + 1))], writes=[yacc], key="yacc")
                for a in range(8):
                    xs_ = xst[a % 2]
                    P.dma("sp", r32(xs_[:]), r32(xs_d[1, grow0 + a * 128:grow0 + (a + 1) * 128, :]),
                          reads=[DR(("x1", s, 2 * g)), DR(("x1", s, 2 * g + 1))], writes=[xs_], key=("xst", a % 2))
                    for hf in range(2):
                        bk = banks[hf]
                        for k in range(4):
                            kk = hf * 4 + k
                            tr(bk, bk[:, k * 128:(k + 1) * 128], xs_, xs_[:, kk * 128:(kk + 1) * 128])
                        cp("act" if hf else "dve", x1T, r32(x1T[:, hf * 4:hf * 4 + 4, a * 128:(a + 1) * 128]), bk, v3(bk[:], 4))
                brt = banks[2]
                for a in range(8):
                    for k in range(8):
                        mm(brt, brt[:, a * 16:(a + 1) * 16], x1T, x1T[:, k, a * 128:(a + 1) * 128], wrt, wrt[:, k, :], k == 0, k == 7)
                act(sc, sc[:], brt, v3(brt[:, 0:128], 8), AF.Sigmoid)
                rb_b = pp[:, PP_RB:PP_RB + 16].unsqueeze(1).to_broadcast([128, 8, 16])
                tt("dve", bi, bi[:], sc, sc[:], pp, rb_b, ALU.add)
                bi4 = bi[:].rearrange("p a (g e) -> p (a g) e", g=4)
                mb4 = mb[:].rearrange("p a (g e) -> p (a g) e", g=4)
                eq4 = eq[:].rearrange("p a (g e) -> p (a g) e", g=4)
                P.op("dve", lambda e, o_=m1[:], i_=bi4: e.tensor_reduce(o_, i_, AX.X, ALU.max), reads=[bi], writes=[m1])
                tt("dve", eq, eq4, bi, bi4, m1, m1[:].unsqueeze(2).to_broadcast([128, 32, 4]), ALU.is_equal)
                stt(mb, mb4, eq, eq4, -1e30, bi, bi4, ALU.mult, ALU.add)
                P.op("dve", lambda e, o_=m2[:], i_=mb4: e.tensor_reduce(o_, i_, AX.X, ALU.max), reads=[mb], writes=[m2])
                tt("dve", m1, m1[:], m1, m1[:], m2, m2[:], ALU.add)
                m1g = m1[:].rearrange("p (a g) -> p a g", g=4)
                P.op("dve", lambda e, o_=gm[:], i_=m1g: e.tensor_reduce(o_, i_, AX.X, ALU.max), reads=[m1], writes=[gm])
                tt("dve", gsel, gsel[:].rearrange("p (a g) -> p a g", g=4), m1, m1g, gm, gm[:].unsqueeze(2).to_broadcast([128, 8, 4]), ALU.is_equal)
                ts("dve", gsel, gsel[:], gsel, gsel[:], -1.0, 1e30, ALU.add, ALU.mult)
                tt("dve", mb, mb4, bi, bi4, gsel, gsel[:].unsqueeze(2).to_broadcast([128, 32, 4]), ALU.add)
                P.op("dve", lambda e, o_=gm[:], i_=mb[:]: e.tensor_reduce(o_, i_, AX.X, ALU.max), reads=[mb], writes=[gm])
                tt("dve", sel, sel[:], mb, mb[:], gm, gm[:].unsqueeze(2).to_broadcast([128, 8, 16]), ALU.is_equal)
                stt(mb, mb[:], sel, sel[:], -1e30, mb, mb[:], ALU.mult, ALU.add)
                P.op("dve", lambda e, o_=gm[:], i_=mb[:]: e.tensor_reduce(o_, i_, AX.X, ALU.max), reads=[mb], writes=[gm])
                tt("dve", eq, eq[:], mb, mb[:], gm, gm[:].unsqueeze(2).to_broadcast([128, 8, 16]), ALU.is_equal)
                tt("dve", sel, sel[:], sel, sel[:], eq, eq[:], ALU.add)
                tt("dve", comb, comb[:], sel, sel[:], sc, sc[:], ALU.mult)
                P.op("dve", lambda e, o_=gm[:], i_=comb[:]: e.tensor_reduce(o_, i_, AX.X, ALU.add), reads=[comb], writes=[gm])
                P.op("dve", lambda e, o_=gm[:]: e.reciprocal(o_, o_), reads=[gm], writes=[gm])
                tt("dve", comb, comb[:], comb, comb[:], gm, gm[:].unsqueeze(2).to_broadcast([128, 8, 16]), ALU.mult)
                tap("comb", comb, comb[:], lambda d_: d_[g])
                for a in range(8):
                    ts("pool", yacc, yacc[:, a, :], yacc, yacc[:, a, :], ALPHA, None, ALU.mult)
                for ex in range(16):
                    for fh in range(2):
                        u_i = ex * 2 + fh
                        wg = wload(w_g_d[l, ex].rearrange("(k p) f -> p k f", p=128)[:, :, fh * 256:(fh + 1) * 256])
                        wu = wload(w_u_d[l, ex].rearrange("(k p) f -> p k f", p=128)[:, :, fh * 256:(fh + 1) * 256])
                        wd = wload(w_d_d[l, ex, fh * 256:(fh + 1) * 256, :].rearrange("(a p) d -> p a d", p=128))
                        hb = hT[u_i % 2]
                        for fc in range(2):
                            for th in range(2):
                                bg_ = banks[(fc * 2 + th) % 2]
                                bu_ = banks[2 + (fc * 2 + th) % 2]
                                for k in range(8):
                                    mm(bg_, bg_[:], wg[0], r32(wg[1][:, k, fc * 128:(fc + 1) * 128]), x1T, r32(x1T[:, k, th * 512:(th + 1) * 512]), k == 0, k == 7)
                                for k in range(8):
                                    mm(bu_, bu_[:], wu[0], r32(wu[1][:, k, fc * 128:(fc + 1) * 128]), x1T, r32(x1T[:, k, th * 512:(th + 1) * 512]), k == 0, k == 7)
                                sgt = sg[(fc * 2 + th) % 2]
                                act(sgt, sgt[:], bg_, bg_[:], AF.Silu)
                                tt("dve", hb, r32(hb[:, fc, th * 512:(th + 1) * 512]), bu_, bu_[:], sgt, sgt[:], ALU.mult)
                        for a in range(8):
                            for dh in range(2):
                                by = banks[4 + (a * 2 + dh) % 4]
                                for fc in range(2):
                                    mm(by, by[:], hb, r32(hb[:, fc, a * 128:(a + 1) * 128]), wd[0], r32(wd[1][:, fc, dh * 512:(dh + 1) * 512]), fc == 0, fc == 1)
                                stt(yacc, yacc[:, a, dh * 512:(dh + 1) * 512], by, by[:], comb[:, a, ex:ex + 1],
                                    yacc, yacc[:, a, dh * 512:(dh + 1) * 512], ALU.mult, ALU.add, extra=[comb])
                for a in range(8):
                    layer_norm(P, yacc, yacc[:, a, :], lnt, pp, PP_LN2G, PP_LN2B)
                last = (l == L - 1)
                dst_d = out_d if last else xs_d[0]
                tk = P.dma("pool", dst_d[grow0:grow0 + 1024, :].rearrange("(a p) d -> p a d", p=128), yacc[:],
                           reads=[yacc], writes=[DR(("x0", s, 2 * g)), DR(("x0", s, 2 * g + 1))], key="yst_out")
                if last:
                    out_toks.append(tk)
    P.final_wait("sp", out_toks)
    P.emit()
    return st


def layer_norm(P, x_t, x_ap, lnt, pp, gcol, bcol):
    stats = lnt[:, 0:12].rearrange("p (a b) -> p a b", a=2)
    mv = lnt[:, 12:14]
    rstd = lnt[:, 14:15]
    for hf in range(2):
        P.op("dve", lambda e, hf=hf: e.bn_stats(stats[:, hf, :], x_ap[:, hf * 512:(hf + 1) * 512]), reads=[x_t], writes=[lnt])
    P.op("dve", lambda e: e.bn_aggr(mv, lnt[:, 0:12]), reads=[lnt], writes=[lnt])
    P.op("act", lambda e: e.activation(rstd, lnt[:, 13:14], AF.Ln, bias=pp[:, PP_LNEPS:PP_LNEPS + 1]), reads=[lnt, pp], writes=[lnt])
    P.op("act", lambda e: e.activation(rstd, rstd, AF.Exp, scale=-0.5), reads=[lnt], writes=[lnt])
    P.op("dve", lambda e: e.tensor_scalar(x_ap, x_ap, lnt[:, 12:13], rstd, ALU.subtract, ALU.mult), reads=[x_t, lnt], writes=[x_t])
    P.op("pool", lambda e: e.tensor_tensor(x_ap, x_ap, pp[:, gcol:gcol + 1024], ALU.mult), reads=[x_t, pp], writes=[x_t])
    P.op("pool", lambda e: e.tensor_tensor(x_ap, x_ap, pp[:, bcol:bcol + 1024], ALU.add), reads=[x_t, pp], writes=[x_t])


def _pack_inputs(inp):
    L = 4
    pp = np.zeros((L, 128, PP_N), np.float32)
    bd = np.zeros((L, 2, 4, 128, 128), np.float32)
    for l in range(L):
        pp[l, :, PP_CAW:PP_CAW + 16] = inp["conv_a_w"][l].reshape(4, 4, 128).transpose(2, 1, 0).reshape(128, 16)
        pp[l, :, PP_CAB:PP_CAB + 4] = inp["conv_a_b"][l].reshape(4, 128).T
        pp[l, :, PP_RBA:PP_RBA + 4] = inp["rg_b_a"][l].reshape(4, 128).T
        pp[l, :, PP_RBX:PP_RBX + 4] = inp["rg_b_x"][l].reshape(4, 128).T
        pp[l, :, PP_LAM:PP_LAM + 4] = inp["rg_lambda"][l].reshape(4, 128).T
        pp[l, :, PP_GCW:PP_GCW + 48] = inp["gdn_conv_w"][l].reshape(4, 12, 128).transpose(2, 1, 0).reshape(128, 48)
        pp[l, :, PP_GNW] = inp["gdn_norm_w"][l]
        pp[l, :, PP_LNW] = inp["gla_norm_w"][l]
        pp[l, :, PP_DTB:PP_DTB + 4] = inp["gdn_dt_bias"][l][None, :]
        pp[l, :, PP_ALOG:PP_ALOG + 4] = inp["gdn_a_log"][l][None, :]
        pp[l, :, PP_RB:PP_RB + 16] = inp["router_bias"][None, :]
        pp[l, :, PP_GLB:PP_GLB + 512] = inp["gla_b_gate"][l][None, :]
        pp[l, :, PP_LN1G:PP_LN1G + 1024] = inp["ln1_g"][l][None, :]
        pp[l, :, PP_LN1B:PP_LN1B + 1024] = inp["ln1_b"][l][None, :]
        pp[l, :, PP_LN2G:PP_LN2G + 1024] = inp["ln2_g"][l][None, :]
        pp[l, :, PP_LN2B:PP_LN2B + 1024] = inp["ln2_b"][l][None, :]
        pp[l, :, PP_ONE] = 1.0
        pp[l, :, PP_EPS] = NORM_EPS
        pp[l, :, PP_LNEPS] = LN_EPS
        for a, nm in enumerate(("rg_w_a", "rg_w_x")):
            w = inp[nm][l]
            for ch in range(4):
                for gb in range(2):
                    bd[l, a, ch, gb * 64:(gb + 1) * 64, gb * 64:(gb + 1) * 64] = w[ch * 2 + gb]
    return pp, bd


_NC_CACHE = {}


def kernel(**inp):
    inp = {k: np.ascontiguousarray(np.asarray(v)) for k, v in inp.items()}
    n = 8
    nseq = 2
    pp, bd = _pack_inputs(inp)
    consts = host_consts()
    if "nc" not in _NC_CACHE:
        nc = bass.Bass("TRN2", target_bir_lowering=False)
        st = build(nc, L=4, NSEQ=nseq)
        _NC_CACHE["nc"] = (nc, st)
    nc = _NC_CACHE["nc"][0]
    x = inp["x"].reshape(n, nseq * S, D)
    shared = {"w_in": inp["w_in"], "w_branch": inp["w_branch"], "w_out": inp["w_out"], "w_gate": inp["w_gate"],
              "w_up": inp["w_up"], "w_down": inp["w_down"], "w_router": inp["w_router"], "pp": pp, "bd": bd,
              "gla_up": inp["gla_w_gate_up"], "consts": consts}
    in_maps = [dict(shared, x=x[i]) for i in range(n)]
    res = run_bass_kernel_spmd(nc, in_maps, core_ids=list(range(n)))
    out = np.stack([np.asarray(r["out"]) for r in res.results], 0)
    return out.reshape(16, S, D).astype(np.float32)
```

```python
from contextlib import ExitStack
import numpy as np
import concourse.bass as bass
import concourse.mybir as mybir
from concourse.bass_utils import run_bass_kernel_spmd

F32 = mybir.dt.float32
F32R = mybir.dt.float32r
BF16 = mybir.dt.bfloat16
AF = mybir.ActivationFunctionType
ALU = mybir.AluOpType
AX = mybir.AxisListType

STRICT = False
DBG = {}
ENGS = ("pe", "act", "dve", "pool", "sp")
CENG = ("pe", "act", "dve", "pool")


class Res:
    __slots__ = ("name", "w", "rs")

    def __init__(self, name):
        self.name = name
        self.w = None
        self.rs = {}


class Tok:
    __slots__ = ("kind", "eng", "idx", "clock")

    def __init__(self, kind, eng, idx, clock):
        self.kind = kind
        self.eng = eng
        self.idx = idx
        self.clock = clock


class T:
    __slots__ = ("t", "res", "name")

    def __init__(self, t, name):
        self.t = t
        self.res = Res(name)
        self.name = name

    def __getitem__(self, k):
        return self.t[k]


class Prog:
    def __init__(self, nc, stack):
        self.nc = nc
        self.stack = stack
        self.ops = {e: [] for e in ENGS}
        self.known = {e: {} for e in ENGS}
        self.n = {e: 0 for e in ENGS}
        self.last = {}
        self.dcount = {}
        self.needed = {e: set() for e in CENG}

    def sb(self, name, shape, dt=F32):
        t = self.stack.enter_context(self.nc.sbuf_tensor(name, list(shape), dt))
        return T(t, name)

    def ps(self, name, shape, dt=F32):
        t = self.stack.enter_context(self.nc.psum_tensor(name, list(shape), dt))
        return T(t, name)

    def _need(self, eng, known, waits, tok, raw, is_dma):
        if tok is None:
            return
        if tok.kind == "c":
            if tok.eng == eng and not raw and not is_dma and (not STRICT or eng == 'pe'):
                return
            if known.get(tok.eng, 0) >= tok.idx:
                return
            waits.append(("c", tok.eng, tok.idx))
            self.needed[tok.eng].add(tok.idx)
        else:
            if known.get(tok.eng, 0) >= tok.idx:
                return
            waits.append(("d", tok.eng, tok.idx))
        for k, v in tok.clock.items():
            if known.get(k, 0) < v:
                known[k] = v
        known[tok.eng] = tok.idx

    def _wait_list(self, eng, reads, writes, is_dma):
        known = self.known[eng]
        waits = []
        for r in reads:
            self._need(eng, known, waits, r.w, True, is_dma)
        for r in writes:
            self._need(eng, known, waits, r.w, False, is_dma)
            for tk in r.rs.values():
                self._need(eng, known, waits, tk, False, is_dma)
        return waits

    @staticmethod
    def _res(x):
        return x.res if isinstance(x, T) else x

    def op(self, eng, fn, reads=(), writes=()):
        reads = [self._res(r) for r in reads]
        writes = [self._res(r) for r in writes]
        waits = self._wait_list(eng, reads, writes, False)
        self.n[eng] += 1
        idx = self.n[eng]
        clock = {k: v for k, v in self.known[eng].items() if k in CENG}
        tok = Tok("c", eng, idx, clock)
        self.ops[eng].append((waits, fn, ("c", idx)))
        self.last[eng] = tok
        for r in reads:
            r.rs[eng] = tok
        for r in writes:
            r.w = tok
            r.rs = {}
        return tok

    def dma(self, eng, out_ap, in_ap, reads=(), writes=(), key=None):
        reads = [self._res(r) for r in reads]
        writes = [self._res(r) for r in writes]
        dkey = ("d", key)
        waits = self._wait_list(eng, reads, writes, True)
        cnt = self.dcount.get(dkey, 0) + 1
        self.dcount[dkey] = cnt
        clock = {k: v for k, v in self.known[eng].items() if k in CENG}
        tok = Tok("d", dkey, cnt, clock)

        nc = self.nc

        def fn(e, out_ap=out_ap, in_ap=in_ap):
            if out_ap.dtype == F32R:
                nc.dge_precook = False
                r = e.dma_start(out=out_ap, in_=in_ap)
                nc.dge_precook = True
                return r
            return e.dma_start(out=out_ap, in_=in_ap)

        self.ops[eng].append((waits, fn, ("d", dkey)))
        self.last[dkey] = tok
        for r in reads:
            r.rs[dkey] = tok
        for r in writes:
            r.w = tok
            r.rs = {}
        return tok

    def barrier(self, skip=(), skip_keys=()):
        toks = []
        for k, tok in self.last.items():
            if tok.kind == "d" and isinstance(tok.eng[1], tuple) and tok.eng[1][0] in skip_keys:
                continue
            toks.append(tok)
        for eng in ENGS:
            if eng in skip:
                continue
            known = self.known[eng]
            waits = []
            for tok in toks:
                self._need(eng, known, waits, tok, True, True)
            if waits:
                self.ops[eng].append((waits, None, None))

    def final_wait(self, eng, toks):
        known = self.known[eng]
        waits = []
        for tok in toks:
            self._need(eng, known, waits, tok, True, True)
        self.ops[eng].append((waits, None, None))

    def emit(self):
        nc = self.nc
        rank = {}
        for e in CENG:
            s = sorted(self.needed[e])
            rank[e] = {idx: i + 1 for i, idx in enumerate(s)}
            assert len(s) < 60000, (e, len(s))
        sems = {e: self.stack.enter_context(nc.semaphore("sem_" + e)) for e in CENG}
        dsems = {}
        for dkey in self.dcount:
            dsems[dkey] = self.stack.enter_context(nc.semaphore("dsem%d" % len(dsems)))
        block = self.stack.enter_context(nc.Block())

        def run(engname, eng):
            for waits, fn, info in self.ops[engname]:
                for kind, k, idx in waits:
                    if kind == "c":
                        eng.wait_ge(sems[k], rank[k][idx])
                    else:
                        eng.wait_ge(dsems[k], 16 * idx)
                if fn is None:
                    continue
                ins = fn(eng)
                if info[0] == "c":
                    if info[1] in rank[engname]:
                        ins.then_inc(sems[engname], 1)
                else:
                    ins.then_inc(dsems[info[1]], 16)

        @block.tensor
        def _(e):
            run("pe", e)

        @block.scalar
        def _(e):
            run("act", e)

        @block.vector
        def _(e):
            run("dve", e)

        @block.gpsimd
        def _(e):
            run("pool", e)

        @block.sync
        def _(e):
            run("sp", e)


D = 1024
S = 2048
TT = 512
NSUB = 4
D_IN = 10264
C_A = 0
C_BQKV = 512
C_BBETA = 2048
C_BGATE = 2056
C_CQKV = 2568
C_DQKV = 4104
C_DLR = 5640
C_DGATE = 5656
C_MERGE = 6168
ALPHA = 8.0 ** 0.25
LN_EPS = 1e-5
NORM_EPS = 1e-6

PP_CAW = 0
PP_CAB = 16
PP_RBA = 20
PP_RBX = 24
PP_LAM = 28
PP_GCW = 32
PP_GNW = 80
PP_LNW = 81
PP_DTB = 82
PP_ALOG = 86
PP_RB = 90
PP_GLB = 106
PP_LN1G = 618
PP_LN1B = 1642
PP_LN2G = 2666
PP_LN2B = 3690
PP_ONE = 4714
PP_EPS = 4715
PP_LNEPS = 4716
PP_N = 4717

K_ID = 0
K_LE = 128
K_GE = 256
K_GT = 384
K_ONE = 512
K_LE16 = 640
K_GT16 = 768
K_LT = 896
K_SBM = 1024
K_N = 1024 + 2048


def host_consts():
    p = np.arange(128)[:, None]
    f = np.arange(128)[None, :]
    c = np.zeros((128, K_N), np.float32)
    c[:, K_ID:K_ID + 128] = (p == f)
    c[:, K_LE:K_LE + 128] = (p <= f)
    c[:, K_GE:K_GE + 128] = (p >= f)
    c[:, K_GT:K_GT + 128] = (p > f)
    c[:, K_ONE:K_ONE + 128] = 1.0
    c[:, K_LE16:K_LE16 + 128] = (p <= f) * (-1.0 / 16.0)
    c[:, K_GT16:K_GT16 + 128] = (p > f) * (-1.0 / 16.0)
    c[:, K_LT:K_LT + 128] = (p < f)
    f5 = np.arange(512)[None, :]
    for jo in range(4):
        c[:, K_SBM + jo * 512:K_SBM + (jo + 1) * 512] = (jo * 128 + p < f5)
    return c


def build(nc, L=4, NSEQ=2, taps=(), en="ABCD", moe=True):
    NTOK = NSEQ * S
    st = ExitStack()
    P = Prog(nc, st)
    dr = lambda name, shape, kind="ExternalInput": nc.dram_tensor(name, list(shape), F32, kind=kind).ap()
    x_d = dr("x", [NTOK, D])
    w_in_d = dr("w_in", [4, D, D_IN])
    w_br_d = dr("w_branch", [4, 4, 512, D])
    w_out_d = dr("w_out", [4, D, D])
    w_g_d = dr("w_gate", [4, 16, D, 512])
    w_u_d = dr("w_up", [4, 16, D, 512])
    w_d_d = dr("w_down", [4, 16, 512, D])
    w_r_d = dr("w_router", [D, 16])
    pp_d = dr("pp", [4, 128, PP_N])
    bd_d = dr("bd", [4, 2, 4, 128, 128])
    gup_d = dr("gla_up", [4, 16, 512])
    k_d = dr("consts", [128, K_N])
    out_d = dr("out", [NTOK, D], "ExternalOutput")
    xs_d = dr("xs_scr", [2, NTOK, D], "Internal")
    tap_d = {}
    for name, shape in taps:
        tap_d[name] = dr("tap_" + name, shape, "ExternalOutput")
    dres = {}

    def DR(key):
        if key not in dres:
            dres[key] = Res("dram:" + str(key))
        return dres[key]

    out_toks = []

    kc = P.sb("kc", [128, 1024])
    kb = P.sb("kb", [128, 2048], BF16)
    pp = P.sb("pp_sb", [128, PP_N])
    NB = 4
    wring = [P.sb("wr%d" % i, [128, 2048]) for i in range(NB)]
    wri = [0]
    bdw = P.sb("bdw", [128, 8, 128])
    gup = P.sb("gup", [16, 512])
    wrt = P.sb("wrt", [128, 8, 16])
    lam8 = P.sb("lam8", [128, 4])
    aexp = P.sb("aexp", [128, 4])
    nge = P.sb("nge", [128, 128], BF16)
    none_ = P.sb("none", [128, 128], BF16)
    ARENA = 36000 - 12288 - 2048
    arena = P.sb("arena", [128, ARENA])
    arenaR = P.sb("arenaR", [128, 12288])
    xst = [P.sb("xst0", [128, 1024]), P.sb("xst1", [128, 1024])]
    banks = [P.ps("bank%d" % i, [128, 512]) for i in range(8)]

    aoff = [0]

    live = {"a": [], "r": []}

    def inherit(which, start, end, t_new):
        keep = []
        for (s0, e0, t_old) in live[which]:
            if s0 < end and start < e0:
                toks = list(t_old.res.rs.values())
                if t_old.res.w is not None:
                    toks.append(t_old.res.w)
                for tk in toks:
                    cur = t_new.res.rs.get(tk.eng)
                    if cur is None or cur.idx < tk.idx:
                        t_new.res.rs[tk.eng] = tk
            else:
                keep.append((s0, e0, t_old))
        keep.append((start, end, t_new))
        live[which] = keep

    def carve(name, ncols, shape=None, dt=F32):
        DBG[name] = (aoff[0], ncols)
        ap = arena[:, aoff[0]:aoff[0] + ncols]
        start = aoff[0]
        aoff[0] += ncols
        assert aoff[0] <= ARENA, (name, aoff[0])
        if dt == BF16:
            ap = ap.bitcast(BF16)
        if shape is not None and len(shape) == 3:
            ap = ap.rearrange("p (a b) -> p a b", a=shape[1])
        t_new = T(ap, name)
        inherit("a", start, start + ncols, t_new)
        return t_new

    roff = [0]

    def carveR(name, ncols, shape=None):
        ap = arenaR[:, roff[0]:roff[0] + ncols]
        roff[0] += ncols
        assert roff[0] <= 12288
        if shape is not None and len(shape) == 3:
            ap = ap.rearrange("p (a b) -> p a b", a=shape[1])
        t_new = T(ap, name)
        inherit("r", roff[0] - ncols, roff[0], t_new)
        return t_new

    def r32(ap):
        return ap.bitcast(F32R)

    def v3(ap, a):
        return ap.rearrange("p (a b) -> p a b", a=a)

    def mm(out_t, out_ap, l_t, l_ap, r_t, r_ap, start, stop):
        P.op("pe", lambda e: e.matmul(out_ap, l_ap, r_ap, start=start, stop=stop), reads=[l_t, r_t], writes=[out_t])

    def tr(out_t, out_ap, in_t, in_ap):
        P.op("pe", lambda e: e.transpose(out_ap, in_ap, kc[:, K_ID:K_ID + 128]), reads=[in_t, kc], writes=[out_t])

    def act(out_t, out_ap, in_t, in_ap, func, bias=None, scale=None, extra=()):
        kw = {}
        if bias is not None:
            kw["bias"] = bias
        if scale is not None:
            kw["scale"] = scale
        P.op("act", lambda e: e.activation(out_ap, in_ap, func, **kw), reads=[in_t] + list(extra), writes=[out_t])

    def ts(eng, out_t, out_ap, in_t, in_ap, s1, s2, op0, op1=None, extra=()):
        if op1 is None:
            P.op(eng, lambda e: e.tensor_scalar(out_ap, in_ap, s1, None, op0), reads=[in_t] + list(extra), writes=[out_t])
        else:
            P.op(eng, lambda e: e.tensor_scalar(out_ap, in_ap, s1, s2, op0, op1), reads=[in_t] + list(extra), writes=[out_t])

    def tt(eng, out_t, out_ap, a_t, a_ap, b_t, b_ap, op):
        P.op(eng, lambda e: e.tensor_tensor(out_ap, a_ap, b_ap, op), reads=[a_t, b_t], writes=[out_t])

    def stt(out_t, out_ap, a_t, a_ap, sc, b_t, b_ap, op0, op1, extra=()):
        P.op("dve", lambda e: e.scalar_tensor_tensor(out_ap, a_ap, sc, b_ap, op0, op1),
             reads=[a_t, b_t] + list(extra), writes=[out_t])

    def cp(eng, out_t, out_ap, in_t, in_ap):
        if eng == "act":
            P.op("act", lambda e: e.copy(out_ap, in_ap), reads=[in_t], writes=[out_t])
        else:
            P.op(eng, lambda e: e.tensor_copy(out_ap, in_ap), reads=[in_t], writes=[out_t])

    def memset(eng, out_t, out_ap, val):
        P.op(eng, lambda e: e.memset(out_ap, val), writes=[out_t])

    def wload(src_ap):
        buf = wring[wri[0] % NB]
        wri[0] += 1
        n = 1
        for s_ in src_ap.shape[1:]:
            n *= s_
        dst = buf[:, 0:n]
        if len(src_ap.shape) == 3:
            dst = dst.rearrange("p (a b) -> p a b", a=src_ap.shape[1])
        P.dma("sp", dst.bitcast(F32R), src_ap.bitcast(F32R), writes=[buf], key=("wr", buf.name))
        return buf, dst

    def tap(name, src_t, src_ap, dst_ap_fn):
        if name in tap_d:
            tk = P.dma("pool", dst_ap_fn(tap_d[name]), src_ap, reads=[src_t], key=("tap", name))
            out_toks.append(tk)

    def barrier():
        pass

    P.dma("sp", kc[:], k_d[:, 0:1024], writes=[kc], key="kc")
    P.dma("sp", arena[:, 0:2048], k_d[:, 1024:3072], writes=[arena], key="kcm")
    cp("dve", kb, kb[:], arena, arena[:, 0:2048])
    P.barrier()
    P.dma("sp", wrt[:], w_r_d.rearrange("(k p) e -> p k e", p=128), writes=[wrt], key="wrt")
    ts("dve", nge, nge[:], kc, kc[:, K_GE:K_GE + 128], -1.0, None, ALU.mult)
    ts("dve", none_, none_[:], kc, kc[:, K_ONE:K_ONE + 128], -1.0, None, ALU.mult)
    ident = kc[:, K_ID:K_ID + 128]
    ones = kc[:, K_ONE:K_ONE + 128]

    def win(l, c0, n):
        return wload(w_in_d[l].rearrange("(k p) n -> p k n", p=128)[:, :, c0:c0 + n])

    for l in range(L):
        barrier()
        P.dma("sp", pp[:], pp_d[l], writes=[pp], key="pp")
        P.dma("sp", bdw[:], bd_d[l].rearrange("a c p j -> p (a c) j"), writes=[bdw], key="bdw")
        P.dma("sp", gup[:], gup_d[l], writes=[gup], key="gup")
        act(lam8, lam8[:], pp, pp[:, PP_LAM:PP_LAM + 4], AF.Exp, scale=-1.0)
        act(lam8, lam8[:], lam8, lam8[:], AF.Ln, bias=pp[:, PP_ONE:PP_ONE + 1], extra=[pp])
        ts("dve", lam8, lam8[:], lam8, lam8[:], -8.0, None, ALU.mult)
        act(aexp, aexp[:], pp, pp[:, PP_ALOG:PP_ALOG + 4], AF.Exp)
        ts("dve", aexp, aexp[:], aexp, aexp[:], -1.0, None, ALU.mult)

        for s in range(NSEQ):
            barrier()
            aoff[0] = 0
            roff[0] = 0
            xT = carveR("xT", 4096, [128, 8, 512])
            merged = carveR("merged", 4096, [128, 8, 512])
            yn = carveR("yn", 2048, [128, 4, 512])
            KT = carve("KT", 4096, [128, 4, 2048], BF16)
            Vc = carve("Vc", 4096, [128, 16, 512], BF16)
            utail = carve("utail", 64, [128, 16, 4])
            hst = carve("hst", 4)
            S_d = carve("S_d", 512, [128, 4, 128])
            S_b = carve("S_b", 512, [128, 4, 128])
            S_db = carve("S_db", 256, [128, 4, 128], BF16)
            gsig = carve("gsig", 512)
            tmpm = carve("tmpm", 512)
            OV = aoff[0]
            memset("pool", utail, utail[:], 0.0)
            memset("pool", hst, hst[:], 0.0)
            memset("pool", S_d, S_d[:], 0.0)
            memset("pool", S_b, S_b[:], 0.0)
            memset("pool", S_db, S_db[:], 0.0)

            for c in range(NSUB):
                row0 = s * S + c * TT

                def proj_fm(bk, wb, wap, j0, ncols):
                    for k in range(8):
                        mm(bk, bk[0:ncols, :], wb, r32(wap[:, k, j0:j0 + ncols]), xT, r32(xT[:, k, :]), k == 0, k == 7)

                def proj_tm(bk, sub, wb, wap, j0, ncols):
                    for k in range(8):
                        mm(bk, bk[:, 0:ncols], xT, r32(xT[:, k, sub * 128:(sub + 1) * 128]), wb, r32(wap[:, k, j0:j0 + ncols]),
                           k == 0, k == 7)

                def branch_merge(n, first):
                    for dq in range(4):
                        wbb, wbap = wload(w_br_d[l, n].rearrange("(k p) n -> p k n", p=128)[:, :, dq * 256:(dq + 1) * 256])
                        wgb, wgap = win(l, C_MERGE + n * 1024 + dq * 256, 256)
                        for h_ in range(2):
                            dch = dq * 2 + h_
                            bp = banks[4 + (dch % 2)]
                            bg = banks[6 + (dch % 2)]
                            for k in range(4):
                                mm(bp, bp[:, :], wbb, r32(wbap[:, k, h_ * 128:(h_ + 1) * 128]), yn, r32(yn[:, k, :]), k == 0, k == 3)
                            proj_fm(bg, wgb, wgap, h_ * 128, 128)
                            act(gsig, gsig[:], bg, bg[:], AF.Sigmoid)
                            if first:
                                tt("dve", merged, r32(merged[:, dch, :]), bp, bp[:], gsig, gsig[:], ALU.mult)
                            else:
                                tt("dve", tmpm, tmpm[:], bp, bp[:], gsig, gsig[:], ALU.mult)
                                tt("pool", merged, r32(merged[:, dch, :]), merged, merged[:, dch, :], tmpm, tmpm[:], ALU.add)

                def gated_norm(o_t, o_ap, h, gate_c0, nw_col, t1, t2, t3):
                    act(t1, t1[:], o_t, o_ap, AF.Square)
                    bs = banks[6]
                    mm(bs, bs[:], kc, ones, t1, t1[:], True, True)
                    act(t2, t2[:], bs, bs[:], AF.Ln, bias=pp[:, PP_EPS:PP_EPS + 1], scale=1.0 / 128.0, extra=[pp])
                    act(t2, t2[:], t2, t2[:], AF.Exp, scale=-0.5)
                    wgb, wgap = win(l, gate_c0 + h * 128, 128)
                    bg = banks[7]
                    proj_fm(bg, wgb, wgap, 0, 128)
                    act(t3, t3[:], bg, bg[:], AF.Silu)
                    stt(t1, t1[:], o_t, o_ap, pp[:, nw_col:nw_col + 1], t2, t2[:], ALU.mult, ALU.mult, extra=[pp])
                    tt("dve", yn, r32(yn[:, h, :]), t1, t1[:], t3, t3[:], ALU.mult)

                barrier()
                for a in range(4):
                    xs_ = xst[a % 2]
                    r0 = row0 + a * 128
                    if l == 0:
                        P.dma("sp", r32(xs_[:]), r32(x_d[r0:r0 + 128, :]), writes=[xs_], key=("xst", a % 2))
                    else:
                        P.dma("sp", r32(xs_[:]), r32(xs_d[0, r0:r0 + 128, :]), reads=[DR(("x0", s, c))], writes=[xs_], key=("xst", a % 2))
                    for hf in range(2):
                        bk = banks[hf]
                        for k in range(4):
                            kk = hf * 4 + k
                            tr(bk, bk[:, k * 128:(k + 1) * 128], xs_, xs_[:, kk * 128:(kk + 1) * 128])
                        cp("act" if hf else "dve", xT, r32(xT[:, hf * 4:hf * 4 + 4, a * 128:(a + 1) * 128]), bk, v3(bk[:], 4))

                first = True
                if "A" in en:
                    barrier()
                    aoff[0] = OV
                    ubuf = carve("ubuf", 516)
                    t1 = carve("t1", 512)
                    t2 = carve("t2", 512)
                    t3 = carve("t3", 512)
                    t4 = carve("t4", 512)
                    wa = [win(l, C_A, 256), win(l, C_A + 256, 256)]
                    for ch in range(4):
                        bk = banks[ch % 2]
                        proj_fm(bk, wa[ch // 2][0], wa[ch // 2][1], (ch % 2) * 128, 128)
                        cp("dve", ubuf, ubuf[:, 0:3], utail, utail[:, ch, 0:3])
                        cp("act", ubuf, ubuf[:, 3:515], bk, bk[:])
                        cp("pool", utail, utail[:, ch, 0:3], ubuf, ubuf[:, 512:515])
                        cw = lambda k: pp[:, PP_CAW + ch * 4 + k:PP_CAW + ch * 4 + k + 1]
                        ts("dve", t1, t1[:], ubuf, ubuf[:, 3:515], cw(3), pp[:, PP_CAB + ch:PP_CAB + ch + 1], ALU.mult, ALU.add, extra=[pp])
                        for k in range(3):
                            stt(t1, t1[:], ubuf, ubuf[:, k:k + 512], cw(k), t1, t1[:], ALU.mult, ALU.add, extra=[pp])
                        ba = banks[2]
                        bx = banks[3]
                        mm(ba, ba[:], bdw, bdw[:, ch, :], t1, t1[:], True, True)
                        mm(bx, bx[:], bdw, bdw[:, 4 + ch, :], t1, t1[:], True, True)
                        act(t2, t2[:], ba, ba[:], AF.Sigmoid, bias=pp[:, PP_RBA + ch:PP_RBA + ch + 1], extra=[pp])
                        act(t3, t3[:], bx, bx[:], AF.Sigmoid, bias=pp[:, PP_RBX + ch:PP_RBX + ch + 1], extra=[pp])
                        act(t2, t2[:], t2, t2[:], AF.Exp, scale=lam8[:, ch:ch + 1], extra=[lam8])
                        tt("dve", t4, t4[:], t2, t2[:], t2, t2[:], ALU.mult)
                        act(t4, t4[:], t4, t4[:], AF.Sqrt, bias=pp[:, PP_ONE:PP_ONE + 1], scale=-1.0, extra=[pp])
                        if c == 0:
                            memset("dve", t4, t4[:, 0:1], 1.0)
                        tt("dve", t3, t3[:], t3, t3[:], t1, t1[:], ALU.mult)
                        tt("dve", t3, t3[:], t3, t3[:], t4, t4[:], ALU.mult)
                        P.op("dve", lambda e, o_=r32(yn[:, ch, :]), a_=t2[:], b_=t3[:], i_=hst[:, ch:ch + 1]: e.tensor_tensor_scan(o_, a_, b_, i_, ALU.mult, ALU.add),
                             reads=[t2, t3, hst], writes=[yn])
                        cp("pool", hst, hst[:, ch:ch + 1], yn, yn[:, ch, 511:512])
                    tap("y_a", yn, yn[:], lambda d_: d_[:, :, c * TT:(c + 1) * TT])
                    branch_merge(0, first)
                    first = False

                if "B" in en:
                    barrier()
                    aoff[0] = OV
                    qnT = carve("qnT", 2048, [128, 4, 512])
                    knT = carve("knT", 2048, [128, 4, 512])
                    vT = carve("vT", 2048, [128, 4, 512])
                    ubuf = carve("ubuf", 516)
                    t1 = carve("t1", 512)
                    t2 = carve("t2", 512)
                    t3 = carve("t3", 512)
                    beta_t = carve("beta_t", 16)
                    g_t = carve("g_t", 16)
                    gcc = carve("gcc", 16)
                    bexp = carve("bexp", 16)
                    kdsc = carve("kdsc", 16)
                    gl = carve("gl", 16)
                    tsm = carve("tsm", 16)
                    tg = carve("tg", 128)
                    tmp1 = carve("tmp1", 128)
                    tmp2 = carve("tmp2", 128)
                    e1m = carve("e1m", 128)
                    e2m = carve("e2m", 128)
                    qdec = carve("qdec", 128)
                    AAT = [carve("AAT0", 256), carve("AAT1", 256)]
                    PT = carve("PT", 128)
                    attn_s = carve("attn_s", 128)
                    kbg = carve("kbg", 128)
                    kdec = carve("kdec", 128)
                    vb = carve("vb", 128)
                    u_s = carve("u_s", 128)
                    wT_s = carve("wT_s", 128)
                    vn_s = carve("vn_s", 128)
                    dests = [qnT, knT, vT]
                    for un in range(6):
                        wq = win(l, C_BQKV + un * 256, 256)
                        for h_ in range(2):
                            ci = un * 2 + h_
                            bk = banks[ci % 2]
                            proj_fm(bk, wq[0], wq[1], h_ * 128, 128)
                            cp("dve", ubuf, ubuf[:, 0:3], utail, utail[:, 4 + ci, 0:3])
                            cp("act", ubuf, ubuf[:, 3:515], bk, bk[:])
                            cp("pool", utail, utail[:, 4 + ci, 0:3], ubuf, ubuf[:, 512:515])
                            cw = lambda k: pp[:, PP_GCW + ci * 4 + k:PP_GCW + ci * 4 + k + 1]
                            ts("dve", t1, t1[:], ubuf, ubuf[:, 3:515], cw(3), None, ALU.mult, extra=[pp])
                            for k in range(3):
                                stt(t1, t1[:], ubuf, ubuf[:, k:k + 512], cw(k), t1, t1[:], ALU.mult, ALU.add, extra=[pp])
                            dst = dests[ci // 4]
                            act(dst, dst[:, ci % 4, :], t1, t1[:], AF.Silu)
                    for qi, dst in enumerate((qnT, knT)):
                        for h in range(4):
                            act(t1, t1[:], dst, dst[:, h, :], AF.Square)
                            bs = banks[2 + h % 2]
                            mm(bs, bs[:], kc, ones, t1, t1[:], True, True)
                            act(t2, t2[:], bs, bs[:], AF.Ln, bias=pp[:, PP_EPS:PP_EPS + 1], extra=[pp])
                            act(t2, t2[:], t2, t2[:], AF.Exp, scale=-0.5)
                            if qi == 0:
                                stt(dst, dst[:, h, :], dst, dst[:, h, :], 128.0 ** -0.5, t2, t2[:], ALU.mult, ALU.mult)
                            else:
                                tt("dve", dst, dst[:, h, :], dst, dst[:, h, :], t2, t2[:], ALU.mult)
                    wbg = win(l, C_BBETA, 8)
                    bsm = banks[4]
                    for sub in range(4):
                        for k in range(8):
                            mm(bsm, bsm[:, sub * 8:(sub + 1) * 8], xT, r32(xT[:, k, sub * 128:(sub + 1) * 128]), wbg[0], r32(wbg[1][:, k, 0:8]), k == 0, k == 7)
                    bsm3 = v3(bsm[:, 0:32], 4)
                    b3 = lambda t_: v3(t_[:], 4)
                    act(beta_t, b3(beta_t), bsm, bsm3[:, :, 0:4], AF.Sigmoid)
                    cp("act", tsm, b3(tsm), bsm, bsm3[:, :, 4:8])
                    tt("dve", tsm, b3(tsm), tsm, b3(tsm), pp, pp[:, PP_DTB:PP_DTB + 4].unsqueeze(1).to_broadcast([128, 4, 4]), ALU.add)
                    act(tsm, tsm[:], tsm, tsm[:], AF.Exp)
                    act(tsm, tsm[:], tsm, tsm[:], AF.Ln, bias=pp[:, PP_ONE:PP_ONE + 1], extra=[pp])
                    tt("dve", g_t, b3(g_t), tsm, b3(tsm), aexp, aexp[:].unsqueeze(1).to_broadcast([128, 4, 4]), ALU.mult)
                    bsm2 = banks[5]
                    for sub in range(4):
                        mm(bsm2, bsm2[:, sub * 4:(sub + 1) * 4], kc, kc[:, K_LE:K_LE + 128], g_t, g_t[:, sub * 4:(sub + 1) * 4], True, True)
                    for sub in range(4):
                        mm(bsm2, bsm2[:, 16 + sub * 4:16 + (sub + 1) * 4], kc, ones, g_t, g_t[:, sub * 4:(sub + 1) * 4], True, True)
                    cp("act", gcc, gcc[:], bsm2, bsm2[:, 0:16])
                    act(bexp, bexp[:], bsm2, bsm2[:, 0:16], AF.Exp)
                    tt("dve", bexp, bexp[:], bexp, bexp[:], beta_t, beta_t[:], ALU.mult)
                    act(gl, gl[:], bsm2, bsm2[:, 16:32], AF.Exp)
                    cp("act", kdsc, kdsc[:], bsm2, bsm2[:, 16:32])
                    tt("dve", kdsc, kdsc[:], kdsc, kdsc[:], gcc, gcc[:], ALU.subtract)
                    act(kdsc, kdsc[:], kdsc, kdsc[:], AF.Exp)
                    import os
                    GS = int(os.environ.get("GDN_STOP", "99"))
                    for h in range(4 if GS > 0 else 0):
                        bo = banks[5]
                        for sub in range(4):
                            cs = slice(sub * 128, (sub + 1) * 128)
                            si = sub * 4 + h
                            b1 = banks[0]
                            b2 = banks[1]
                            ts("dve", tg, tg[:], kc, kc[:, K_LE:K_LE + 128], g_t[:, si:si + 1], None, ALU.mult, extra=[g_t])
                            mm(b1, b1[:, 0:128], kc, ones, tg, tg[:], True, True)
                            mm(b1, b1[:, 128:256], knT, knT[:, h, cs], knT, knT[:, h, cs], True, True)
                            mm(b1, b1[:, 256:384], knT, knT[:, h, cs], qnT, qnT[:, h, cs], True, True)
                            mm(b1, b1[:, 384:512], knT, knT[:, h, cs], kc, ident, True, True)
                            mm(b2, b2[:, 0:128], vT, vT[:, h, cs], kc, ident, True, True)
                            ts("dve", tmp1, tmp1[:], b1, b1[:, 0:128], gcc[:, si:si + 1], 0.0, ALU.subtract, ALU.min, extra=[gcc])
                            ts("dve", tmp2, tmp2[:], b1, b1[:, 0:128], gcc[:, si:si + 1], 0.0, ALU.subtract, ALU.max, extra=[gcc])
                            act(tmp1, tmp1[:], tmp1, tmp1[:], AF.Exp)
                            act(tmp2, tmp2[:], tmp2, tmp2[:], AF.Exp, scale=-1.0)
                            tt("pool", e1m, e1m[:], tmp1, tmp1[:], kc, kc[:, K_LE:K_LE + 128], ALU.mult)
                            tt("pool", e2m, e2m[:], tmp2, tmp2[:], kc, kc[:, K_GT:K_GT + 128], ALU.mult)
                            act(tg, tg[:], b1, b1[:, 0:128], AF.Exp)
                            tt("dve", qdec, qdec[:], qnT, qnT[:, h, cs], tg, tg[:], ALU.mult)
                            cur = 0
                            stt(AAT[0], AAT[0][:, 0:128], b1, b1[:, 128:256], beta_t[:, si:si + 1], e2m, e2m[:], ALU.mult, ALU.mult, extra=[beta_t])
                            tt("dve", attn_s, attn_s[:], b1, b1[:, 256:384], e1m, e1m[:], ALU.mult)
                            ts("dve", kbg, kbg[:], b1, b1[:, 384:512], bexp[:, si:si + 1], None, ALU.mult, extra=[bexp])
                            ts("dve", kdec, kdec[:], b1, b1[:, 384:512], kdsc[:, si:si + 1], None, ALU.mult, extra=[kdsc])
                            ts("dve", vb, vb[:], b2, b2[:, 0:128], beta_t[:, si:si + 1], None, ALU.mult, extra=[beta_t])
                            if GS < 2:
                                continue
                            GV = int(os.environ.get("GDN_V", "9"))
                            mm(b2, b2[:, 128:256], AAT[0], AAT[0][:, 0:128], kc, ident, True, True)
                            if GV >= 2:
                                cp("act", AAT[0], AAT[0][:, 128:256], b2, b2[:, 128:256])
                            if GV >= 3:
                                act(PT, PT[:], b2, b2[:, 128:256], AF.Copy, scale=-1.0)
                                tt("pool", PT, PT[:], PT, PT[:], kc, ident, ALU.add)
                            for m in range(1, 1 + int(os.environ.get('GDN_LV', '6'))):
                                b3_ = banks[2 + m % 2]
                                A_c = AAT[cur]
                                A_n = AAT[1 - cur]
                                mm(b3_, b3_[:, 0:128], A_c, A_c[:, 128:256], A_c, A_c[:, 0:128], True, True)
                                if m < 6:
                                    mm(b3_, b3_[:, 128:256], A_c, A_c[:, 0:128], A_c, A_c[:, 128:256], True, True)
                                    cp("act", A_n, A_n[:, 0:256], b3_, b3_[:, 0:256])
                                else:
                                    cp("act", A_n, A_n[:, 0:128], b3_, b3_[:, 0:128])
                                mm(b3_, b3_[:, 256:384], A_n, A_n[:, 0:128], PT, PT[:], True, True)
                                tt("dve", PT, PT[:], PT, PT[:], b3_, b3_[:, 256:384], ALU.add)
                                cur = 1 - cur
                            if GS < 3:
                                continue
                            b5 = banks[4]
                            mm(b5, b5[:, 0:128], PT, PT[:], vb, vb[:], True, True)
                            mm(b5, b5[:, 128:256], kbg, kbg[:], PT, PT[:], True, True)
                            cp("act", u_s, u_s[:], b5, b5[:, 0:128])
                            cp("act", wT_s, wT_s[:], b5, b5[:, 128:256])
                            mm(b5, b5[:, 256:384], wT_s, wT_s[:], S_b, S_b[:, h, :], True, True)
                            tt("dve", vn_s, vn_s[:], u_s, u_s[:], b5, b5[:, 256:384], ALU.subtract)
                            mm(bo, bo[:, cs], S_b, S_b[:, h, :], qdec, qdec[:], True, False)
                            mm(bo, bo[:, cs], vn_s, vn_s[:], attn_s, attn_s[:], False, True)
                            mm(b5, b5[:, 384:512], kdec, kdec[:], vn_s, vn_s[:], True, True)
                            stt(S_b, S_b[:, h, :], S_b, S_b[:, h, :], gl[:, si:si + 1], b5, b5[:, 384:512], ALU.mult, ALU.add, extra=[gl])
                        if GS >= 3:
                            gated_norm(bo, bo[:], h, C_BGATE, PP_GNW, t1, t2, t3)
                    tap("y_b", yn, yn[:], lambda d_: d_[:, :, c * TT:(c + 1) * TT])
                    branch_merge(1, first)
                    first = False

                if "C" in en:
                    barrier()
                    aoff[0] = OV
                    qTb = carve("qTb", 2048, [128, 8, 512], BF16)
                    memset("pool", qTb, qTb[:], 0.0)
                    e_sb = carve("e_sb", 512)
                    sp_sb = carve("sp_sb", 256, None, BF16)
                    spsum = carve("spsum", 512)
                    spsum_b = carve("spsum_b", 256, None, BF16)
                    w_sb = carve("w_sb", 256, None, BF16)
                    qb0 = c * 4
                    for u in range(2):
                        wq = win(l, C_CQKV + u * 256, 256)
                        wk = win(l, C_CQKV + 512 + u * 256, 256)
                        for h_ in range(2):
                            pr = u * 2 + h_
                            bq = banks[0 + h_]
                            proj_fm(bq, wq[0], wq[1], h_ * 128, 128)
                            ts("dve", qTb, qTb[0:64, 2 * pr, :], bq, bq[0:64, :], 0.125, None, ALU.mult)
                            ts("dve", qTb, qTb[64:128, 2 * pr + 1, :], bq, bq[64:128, :], 0.125, None, ALU.mult)
                            bk_ = banks[2 + h_]
                            proj_fm(bk_, wk[0], wk[1], h_ * 128, 128)
                            cp("act", KT, KT[:, pr, c * TT:(c + 1) * TT], bk_, bk_[:])
                    wv0 = win(l, C_CQKV + 1024, 256)
                    wv1 = win(l, C_CQKV + 1280, 256)
                    for sub in range(4):
                        bv = banks[sub % 2]
                        proj_tm(bv, sub, wv0[0], wv0[1], 0, 256)
                        cp("act", Vc, Vc[:, qb0 + sub, 0:256], bv, bv[:, 0:256])
                        bv2 = banks[2 + sub % 2]
                        proj_tm(bv2, sub, wv1[0], wv1[1], 0, 256)
                        cp("dve", Vc, Vc[:, qb0 + sub, 256:512], bv2, bv2[:, 0:256])
                    for pr in range(4):
                        for hh in range(2):
                            bo = banks[6 + hh]
                            h = pr * 2 + hh
                            ps_ = slice(hh * 64, hh * 64 + 64)
                            nkb = qb0 + 4
                            memset("pool", spsum, spsum[:], 0.0)
                            for step, J in enumerate(range(nkb - 1, -1, -1)):
                                jo = J - qb0
                                q0 = max(jo, 0) * 128
                                qs = slice(q0, 512)
                                dg = slice(q0, q0 + 128)
                                bz = banks[step % 2]
                                bd_ = banks[2 + step % 2]
                                mm(bz, bz[:, qs], KT, KT[:, pr, J * 128:(J + 1) * 128], qTb, qTb[:, h, qs], True, True)
                                act(e_sb, e_sb[:, qs], bz, bz[:, qs], AF.Exp)
                                act(sp_sb, sp_sb[:, qs], e_sb, e_sb[:, qs], AF.Ln, bias=pp[:, PP_ONE:PP_ONE + 1], extra=[pp])
                                if jo >= 0:
                                    tt("pool", sp_sb, sp_sb[:, dg], sp_sb, sp_sb[:, dg], kb, kb[:, 0:128], ALU.mult)
                                mm(bd_, bd_[:, qs], KT, KT[:, pr, J * 128:(J + 1) * 128], qTb, qTb[:, h, qs], True, False)
                                mm(bd_, bd_[:, qs], nge, nge[:], sp_sb, sp_sb[:, qs], False, step == 0)
                                if step > 0:
                                    mm(bd_, bd_[:, qs], none_, none_[:], spsum_b, spsum_b[:, qs], False, True)
                                act(w_sb, w_sb[:, qs], bd_, bd_[:, qs], AF.Exp)
                                if jo >= 0:
                                    tt("pool", w_sb, w_sb[:, dg], w_sb, w_sb[:, dg], kb, kb[:, 0:128], ALU.mult)
                                mm(bo, bo[:, qs], Vc, Vc[:, J, pr * 128:(pr + 1) * 128], w_sb, w_sb[:, qs], step == 0, J == 0)
                                if J > 0:
                                    tt("dve", spsum, spsum[:, qs], spsum, spsum[:, qs], sp_sb, sp_sb[:, qs], ALU.add)
                                    cp("pool", spsum_b, spsum_b[:], spsum, spsum[:])
                            cp("act", yn, r32(yn[ps_, pr, :]), bo, bo[ps_, :])
                    tap("y_c", yn, yn[:], lambda d_: d_[:, :, c * TT:(c + 1) * TT])
                    branch_merge(2, first)
                    first = False

                if "D" in en:
                    barrier()
                    aoff[0] = OV
                    t1 = carve("t1", 512)
                    t2 = carve("t2", 512)
                    t3 = carve("t3", 512)
                    sp_tok = carve("sp_tok", 2048, [128, 4, 512])
                    qd = carve("qd", 1024, [128, 4, 512], BF16)
                    ki = carve("ki", 1024, [128, 4, 512], BF16)
                    kdt = carve("kdt", 1024, [128, 4, 512], BF16)
                    vtk = carve("vtk", 1024, [128, 4, 512], BF16)
                    lrT = carve("lrT", 512)
                    glast = carve("glast", 16)
                    attn = carve("attn", 64, None, BF16)
                    wl = win(l, C_DLR, 16)
                    b0 = banks[0]
                    proj_fm(b0, wl[0], wl[1], 0, 16)
                    cp("act", lrT, lrT[0:16, :], b0, b0[0:16, :])
                    for sub in range(4):
                        bg_ = banks[1 + sub % 2]
                        mm(bg_, bg_[:], lrT, lrT[0:16, sub * 128:(sub + 1) * 128], gup, gup[:], True, True)
                        tt("dve", t1, t1[:], bg_, bg_[:], pp, pp[:, PP_GLB:PP_GLB + 512], ALU.add)
                        act(t1, t1[:], t1, t1[:], AF.Exp, scale=-1.0)
                        act(sp_tok, sp_tok[:, sub, :], t1, t1[:], AF.Ln, bias=pp[:, PP_ONE:PP_ONE + 1], extra=[pp])
                    wk0 = win(l, C_DQKV + 512, 256)
                    wk1 = win(l, C_DQKV + 768, 256)
                    for sub in range(4):
                        br = banks[3]
                        mm(br, br[:], kc, kc[:, K_GT16:K_GT16 + 128], sp_tok, sp_tok[:, sub, :], True, True)
                        act(t2, t2[:], br, br[:], AF.Exp)
                        for hf, wk_ in enumerate((wk0, wk1)):
                            bkk = banks[4 + hf]
                            proj_tm(bkk, sub, wk_[0], wk_[1], 0, 256)
                            tt("dve", kdt, kdt[:, sub, hf * 256:(hf + 1) * 256], bkk, bkk[:, 0:256], t2, t2[:, hf * 256:(hf + 1) * 256], ALU.mult)
                    wv0 = win(l, C_DQKV + 1024, 256)
                    wv1 = win(l, C_DQKV + 1280, 256)
                    for sub in range(4):
                        for hf, wvv in enumerate((wv0, wv1)):
                            bvv = banks[6 + hf]
                            proj_tm(bvv, sub, wvv[0], wvv[1], 0, 256)
                            cp("act", vtk, vtk[:, sub, hf * 256:(hf + 1) * 256], bvv, bvv[:, 0:256])
                    for h in range(4):
                        bb = banks[0]
                        for sub in range(4):
                            mm(bb, bb[:, sub * 128:(sub + 1) * 128], sp_tok, sp_tok[:, sub, h * 128:(h + 1) * 128],
                               kc, kc[:, K_LE16:K_LE16 + 128], True, True)
                        act(t1, t1[:], bb, bb[:], AF.Exp)
                        act(t2, t2[:], bb, bb[:], AF.Exp, scale=-1.0)
                        for sub in range(4):
                            cp("pool", glast, glast[:, h * 4 + sub:h * 4 + sub + 1], t1, t1[:, sub * 128 + 127:sub * 128 + 128])
                        wq = win(l, C_DQKV + h * 128, 128)
                        bq = banks[1]
                        proj_fm(bq, wq[0], wq[1], 0, 128)
                        stt(qd, qd[:, h, :], bq, bq[:], 128.0 ** -0.5, t1, t1[:], ALU.mult, ALU.mult)
                        wkf = win(l, C_DQKV + 512 + h * 128, 128)
                        bk2 = banks[2]
                        proj_fm(bk2, wkf[0], wkf[1], 0, 128)
                        tt("dve", ki, ki[:, h, :], bk2, bk2[:], t2, t2[:], ALU.mult)
                    for h in range(4):
                        bo = banks[3 + h % 2]
                        for sub in range(4):
                            cs = slice(sub * 128, (sub + 1) * 128)
                            hs = slice(h * 128, (h + 1) * 128)
                            ba_ = banks[5]
                            mm(ba_, ba_[:, 0:128], ki, ki[:, h, cs], qd, qd[:, h, cs], True, True)
                            tt("dve", attn, attn[:], ba_, ba_[:, 0:128], kc, kc[:, K_LE:K_LE + 128], ALU.mult)
                            mm(bo, bo[:, cs], vtk, vtk[:, sub, hs], attn, attn[:], True, False)
                            mm(bo, bo[:, cs], S_db, S_db[:, h, :], qd, qd[:, h, cs], False, True)
                            bs_ = banks[6]
                            mm(bs_, bs_[:, 0:128], kdt, kdt[:, sub, hs], vtk, vtk[:, sub, hs], True, True)
                            stt(S_d, S_d[:, h, :], S_d, S_d[:, h, :], glast[:, h * 4 + sub:h * 4 + sub + 1], bs_, bs_[:, 0:128],
                                ALU.mult, ALU.add, extra=[glast])
                            cp("pool", S_db, S_db[:, h, :], S_d, S_d[:, h, :])
                        gated_norm(bo, bo[:], h, C_DGATE, PP_LNW, t1, t2, t3)
                    tap("y_d", yn, yn[:], lambda d_: d_[:, :, c * TT:(c + 1) * TT])
                    branch_merge(3, first)
                    first = False

                barrier()
                aoff[0] = OV
                x_tok = carve("x_tok", 4096, [128, 4, 1024])
                lnt = carve("lnt", 64)
                src = (x_d if l == 0 else xs_d[0])[row0:row0 + TT, :]
                P.dma("pool", x_tok[:], src.rearrange("(a p) d -> p a d", p=128),
                      reads=([DR(("x0", s, c))] if l > 0 else []), writes=[x_tok], key="xtok")
                for dq in range(4):
                    wo = wload(w_out_d[l].rearrange("(k p) n -> p k n", p=128)[:, :, dq * 256:(dq + 1) * 256])
                    for sub in range(4):
                        bo = banks[sub % 4]
                        for k in range(8):
                            mm(bo, bo[:, 0:256], merged, r32(merged[:, k, sub * 128:(sub + 1) * 128]), wo[0], r32(wo[1][:, k, :]), k == 0, k == 7)
                        stt(x_tok, x_tok[:, sub, dq * 256:(dq + 1) * 256], x_tok, x_tok[:, sub, dq * 256:(dq + 1) * 256], ALPHA,
                            bo, bo[:, 0:256], ALU.mult, ALU.add)
                for sub in range(4):
                    layer_norm(P, x_tok, x_tok[:, sub, :], lnt, pp, PP_LN1G, PP_LN1B)
                tap("x1", x_tok, x_tok[:], lambda d_: d_[c * TT:(c + 1) * TT, :].rearrange("(a p) d -> p a d", p=128))
                dst_d = xs_d[1] if moe else (out_d if l == L - 1 else xs_d[0])
                tk = P.dma("pool", dst_d[row0:row0 + TT, :].rearrange("(a p) d -> p a d", p=128), x_tok[:],
                           reads=[x_tok], writes=[DR((("x1" if moe else "x0"), s, c))], key="xst_out")
                if not moe and l == L - 1:
                    out_toks.append(tk)

            if not moe:
                continue
            for g in range(2):
                barrier()
                aoff[0] = 0
                roff[0] = 0
                x1T = carveR("x1T", 8192, [128, 8, 1024])
                hT = [carveR("hT0", 2048, [128, 2, 1024]), carveR("hT1", 2048, [128, 2, 1024])]
                yacc = carve("yacc", 8192, [128, 8, 1024])
                sg = [carve("sg0", 512), carve("sg1", 512)]
                sc = carve("sc", 128, [128, 8, 16])
                bi = carve("bi", 128, [128, 8, 16])
                mb = carve("mb", 128, [128, 8, 16])
                eq = carve("eq", 128, [128, 8, 16])
                sel = carve("sel", 128, [128, 8, 16])
                comb = carve("comb", 128, [128, 8, 16])
                m1 = carve("m1", 32)
                m2 = carve("m2", 32)
                gsel = carve("gsel", 32)
                gm = carve("gm", 8)
                lnt = carve("lnt", 64)
                grow0 = s * S + g * 1024
                P.dma("pool", yacc[:], xs_d[1, grow0:grow0 + 1024, :].rearrange("(a p) d -> p a d", p=128),
                      reads=[DR(("x1", s, 2 * g)), DR(("x1", s, 2 * g + 1))], writes=[yacc], key="yacc")
                for a in range(8):
                    xs_ = xst[a % 2]
                    P.dma("sp", r32(xs_[:]), r32(xs_d[1, grow0 + a * 128:grow0 + (a + 1) * 128, :]),
                          reads=[DR(("x1", s, 2 * g)), DR(("x1", s, 2 * g + 1))], writes=[xs_], key=("xst", a % 2))
                    for hf in range(2):
                        bk = banks[hf]
                        for k in range(4):
                            kk = hf * 4 + k
                            tr(bk, bk[:, k * 128:(k + 1) * 128], xs_, xs_[:, kk * 128:(kk + 1) * 128])
                        cp("act" if hf else "dve", x1T, r32(x1T[:, hf * 4:hf * 4 + 4, a * 128:(a + 1) * 128]), bk, v3(bk[:], 4))
                brt = banks[2]
                for a in range(8):
                    for k in range(8):
                        mm(brt, brt[:, a * 16:(a + 1) * 16], x1T, x1T[:, k, a * 128:(a + 1) * 128], wrt, wrt[:, k, :], k == 0, k == 7)
                act(sc, sc[:], brt, v3(brt[:, 0:128], 8), AF.Sigmoid)
                rb_b = pp[:, PP_RB:PP_RB + 16].unsqueeze(1).to_broadcast([128, 8, 16])
                tt("dve", bi, bi[:], sc, sc[:], pp, rb_b, ALU.add)
                bi4 = bi[:].rearrange("p a (g e) -> p (a g) e", g=4)
                mb4 = mb[:].rearrange("p a (g e) -> p (a g) e", g=4)
                eq4 = eq[:].rearrange("p a (g e) -> p (a g) e", g=4)
                P.op("dve", lambda e, o_=m1[:], i_=bi4: e.tensor_reduce(o_, i_, AX.X, ALU.max), reads=[bi], writes=[m1])
                tt("dve", eq, eq4, bi, bi4, m1, m1[:].unsqueeze(2).to_broadcast([128, 32, 4]), ALU.is_equal)
                stt(mb, mb4, eq, eq4, -1e30, bi, bi4, ALU.mult, ALU.add)
                P.op("dve", lambda e, o_=m2[:], i_=mb4: e.tensor_reduce(o_, i_, AX.X, ALU.max), reads=[mb], writes=[m2])
                tt("dve", m1, m1[:], m1, m1[:], m2, m2[:], ALU.add)
                m1g = m1[:].rearrange("p (a g) -> p a g", g=4)
                P.op("dve", lambda e, o_=gm[:], i_=m1g: e.tensor_reduce(o_, i_, AX.X, ALU.max), reads=[m1], writes=[gm])
                tt("dve", gsel, gsel[:].rearrange("p (a g) -> p a g", g=4), m1, m1g, gm, gm[:].unsqueeze(2).to_broadcast([128, 8, 4]), ALU.is_equal)
                ts("dve", gsel, gsel[:], gsel, gsel[:], -1.0, 1e30, ALU.add, ALU.mult)
                tt("dve", mb, mb4, bi, bi4, gsel, gsel[:].unsqueeze(2).to_broadcast([128, 32, 4]), ALU.add)
                P.op("dve", lambda e, o_=gm[:], i_=mb[:]: e.tensor_reduce(o_, i_, AX.X, ALU.max), reads=[mb], writes=[gm])
                tt("dve", sel, sel[:], mb, mb[:], gm, gm[:].unsqueeze(2).to_broadcast([128, 8, 16]), ALU.is_equal)
                stt(mb, mb[:], sel, sel[:], -1e30, mb, mb[:], ALU.mult, ALU.add)
                P.op("dve", lambda e, o_=gm[:], i_=mb[:]: e.tensor_reduce(o_, i_, AX.X, ALU.max), reads=[mb], writes=[gm])
                tt("dve", eq, eq[:], mb, mb[:], gm, gm[:].unsqueeze(2).to_broadcast([128, 8, 16]), ALU.is_equal)
                tt("dve", sel, sel[:], sel, sel[:], eq, eq[:], ALU.add)
                tt("dve", comb, comb[:], sel, sel[:], sc, sc[:], ALU.mult)
                P.op("dve", lambda e, o_=gm[:], i_=comb[:]: e.tensor_reduce(o_, i_, AX.X, ALU.add), reads=[comb], writes=[gm])
                P.op("dve", lambda e, o_=gm[:]: e.reciprocal(o_, o_), reads=[gm], writes=[gm])
                tt("dve", comb, comb[:], comb, comb[:], gm, gm[:].unsqueeze(2).to_broadcast([128, 8, 16]), ALU.mult)
                tap("comb", comb, comb[:], lambda d_: d_[g])
                for a in range(8):
                    ts("pool", yacc, yacc[:, a, :], yacc, yacc[:, a, :], ALPHA, None, ALU.mult)
                for ex in range(16):
                    for fh in range(2):
                        u_i = ex * 2 + fh
                        wg = wload(w_g_d[l, ex].rearrange("(k p) f -> p k f", p=128)[:, :, fh * 256:(fh + 1) * 256])
                        wu = wload(w_u_d[l, ex].rearrange("(k p) f -> p k f", p=128)[:, :, fh * 256:(fh + 1) * 256])
                        wd = wload(w_d_d[l, ex, fh * 256:(fh + 1) * 256, :].rearrange("(a p) d -> p a d", p=128))
                        hb = hT[u_i % 2]
                        for fc in range(2):
                            for th in range(2):
                                bg_ = banks[(fc * 2 + th) % 2]
                                bu_ = banks[2 + (fc * 2 + th) % 2]
                                for k in range(8):
                                    mm(bg_, bg_[:], wg[0], r32(wg[1][:, k, fc * 128:(fc + 1) * 128]), x1T, r32(x1T[:, k, th * 512:(th + 1) * 512]), k == 0, k == 7)
                                for k in range(8):
                                    mm(bu_, bu_[:], wu[0], r32(wu[1][:, k, fc * 128:(fc + 1) * 128]), x1T, r32(x1T[:, k, th * 512:(th + 1) * 512]), k == 0, k == 7)
                                sgt = sg[(fc * 2 + th) % 2]
                                act(sgt, sgt[:], bg_, bg_[:], AF.Silu)
                                tt("dve", hb, r32(hb[:, fc, th * 512:(th + 1) * 512]), bu_, bu_[:], sgt, sgt[:], ALU.mult)
                        for a in range(8):
                            for dh in range(2):
                                by = banks[4 + (a * 2 + dh) % 4]
                                for fc in range(2):
                                    mm(by, by[:], hb, r32(hb[:, fc, a * 128:(a + 1) * 128]), wd[0], r32(wd[1][:, fc, dh * 512:(dh + 1) * 512]), fc == 0, fc == 1)
                                stt(yacc, yacc[:, a, dh * 512:(dh + 1) * 512], by, by[:], comb[:, a, ex:ex + 1],
                                    yacc, yacc[:, a, dh * 512:(dh + 1) * 512], ALU.mult, ALU.add, extra=[comb])
                for a in range(8):
                    layer_norm(P, yacc, yacc[:, a, :], lnt, pp, PP_LN2G, PP_LN2B)
                last = (l == L - 1)
                dst_d = out_d if last else xs_d[0]
                tk = P.dma("pool", dst_d[grow0:grow0 + 1024, :].rearrange("(a p) d -> p a d", p=128), yacc[:],
                           reads=[yacc], writes=[DR(("x0", s, 2 * g)), DR(("x0", s, 2 * g + 1))], key="yst_out")
                if last:
                    out_toks.append(tk)
    P.final_wait("sp", out_toks)
    P.emit()
    return st


def layer_norm(P, x_t, x_ap, lnt, pp, gcol, bcol):
    stats = lnt[:, 0:12].rearrange("p (a b) -> p a b", a=2)
    mv = lnt[:, 12:14]
    rstd = lnt[:, 14:15]
    for hf in range(2):
        P.op("dve", lambda e, hf=hf: e.bn_stats(stats[:, hf, :], x_ap[:, hf * 512:(hf + 1) * 512]), reads=[x_t], writes=[lnt])
    P.op("dve", lambda e: e.bn_aggr(mv, lnt[:, 0:12]), reads=[lnt], writes=[lnt])
    P.op("act", lambda e: e.activation(rstd, lnt[:, 13:14], AF.Ln, bias=pp[:, PP_LNEPS:PP_LNEPS + 1]), reads=[lnt, pp], writes=[lnt])
    P.op("act", lambda e: e.activation(rstd, rstd, AF.Exp, scale=-0.5), reads=[lnt], writes=[lnt])
    P.op("dve", lambda e: e.tensor_scalar(x_ap, x_ap, lnt[:, 12:13], rstd, ALU.subtract, ALU.mult), reads=[x_t, lnt], writes=[x_t])
    P.op("pool", lambda e: e.tensor_tensor(x_ap, x_ap, pp[:, gcol:gcol + 1024], ALU.mult), reads=[x_t, pp], writes=[x_t])
    P.op("pool", lambda e: e.tensor_tensor(x_ap, x_ap, pp[:, bcol:bcol + 1024], ALU.add), reads=[x_t, pp], writes=[x_t])


def _pack_inputs(inp):
    L = 4
    pp = np.zeros((L, 128, PP_N), np.float32)
    bd = np.zeros((L, 2, 4, 128, 128), np.float32)
    for l in range(L):
        pp[l, :, PP_CAW:PP_CAW + 16] = inp["conv_a_w"][l].reshape(4, 4, 128).transpose(2, 1, 0).reshape(128, 16)
        pp[l, :, PP_CAB:PP_CAB + 4] = inp["conv_a_b"][l].reshape(4, 128).T
        pp[l, :, PP_RBA:PP_RBA + 4] = inp["rg_b_a"][l].reshape(4, 128).T
        pp[l, :, PP_RBX:PP_RBX + 4] = inp["rg_b_x"][l].reshape(4, 128).T
        pp[l, :, PP_LAM:PP_LAM + 4] = inp["rg_lambda"][l].reshape(4, 128).T
        pp[l, :, PP_GCW:PP_GCW + 48] = inp["gdn_conv_w"][l].reshape(4, 12, 128).transpose(2, 1, 0).reshape(128, 48)
        pp[l, :, PP_GNW] = inp["gdn_norm_w"][l]
        pp[l, :, PP_LNW] = inp["gla_norm_w"][l]
        pp[l, :, PP_DTB:PP_DTB + 4] = inp["gdn_dt_bias"][l][None, :]
        pp[l, :, PP_ALOG:PP_ALOG + 4] = inp["gdn_a_log"][l][None, :]
        pp[l, :, PP_RB:PP_RB + 16] = inp["router_bias"][None, :]
        pp[l, :, PP_GLB:PP_GLB + 512] = inp["gla_b_gate"][l][None, :]
        pp[l, :, PP_LN1G:PP_LN1G + 1024] = inp["ln1_g"][l][None, :]
        pp[l, :, PP_LN1B:PP_LN1B + 1024] = inp["ln1_b"][l][None, :]
        pp[l, :, PP_LN2G:PP_LN2G + 1024] = inp["ln2_g"][l][None, :]
        pp[l, :, PP_LN2B:PP_LN2B + 1024] = inp["ln2_b"][l][None, :]
        pp[l, :, PP_ONE] = 1.0
        pp[l, :, PP_EPS] = NORM_EPS
        pp[l, :, PP_LNEPS] = LN_EPS
        for a, nm in enumerate(("rg_w_a", "rg_w_x")):
            w = inp[nm][l]
            for ch in range(4):
                for gb in range(2):
                    bd[l, a, ch, gb * 64:(gb + 1) * 64, gb * 64:(gb + 1) * 64] = w[ch * 2 + gb]
    return pp, bd


_NC_CACHE = {}


def kernel(**inp):
    inp = {k: np.ascontiguousarray(np.asarray(v)) for k, v in inp.items()}
    n = 8
    nseq = 2
    pp, bd = _pack_inputs(inp)
    consts = host_consts()
    if "nc" not in _NC_CACHE:
        nc = bass.Bass("TRN2", target_bir_lowering=False)
        st = build(nc, L=4, NSEQ=nseq)
        _NC_CACHE["nc"] = (nc, st)
    nc = _NC_CACHE["nc"][0]
    x = inp["x"].reshape(n, nseq * S, D)
    shared = {"w_in": inp["w_in"], "w_branch": inp["w_branch"], "w_out": inp["w_out"], "w_gate": inp["w_gate"],
              "w_up": inp["w_up"], "w_down": inp["w_down"], "w_router": inp["w_router"], "pp": pp, "bd": bd,
              "gla_up": inp["gla_w_gate_up"], "consts": consts}
    in_maps = [dict(shared, x=x[i]) for i in range(n)]
    res = run_bass_kernel_spmd(nc, in_maps, core_ids=list(range(n)))
    out = np.stack([np.asarray(r["out"]) for r in res.results], 0)
    return out.reshape(16, S, D).astype(np.float32)
```

```python
from contextlib import ExitStack
import numpy as np
import concourse.bass as bass
import concourse.mybir as mybir
from concourse.bass_utils import run_bass_kernel_spmd

F32 = mybir.dt.float32
F32R = mybir.dt.float32r
BF16 = mybir.dt.bfloat16
AF = mybir.ActivationFunctionType
ALU = mybir.AluOpType
AX = mybir.AxisListType

STRICT = False
DBG = {}
ENGS = ("pe", "act", "dve", "pool", "sp")
CENG = ("pe", "act", "dve", "pool")


class Res:
    __slots__ = ("name", "w", "rs")

    def __init__(self, name):
        self.name = name
        self.w = None
        self.rs = {}


class Tok:
    __slots__ = ("kind", "eng", "idx", "clock")

    def __init__(self, kind, eng, idx, clock):
        self.kind = kind
        self.eng = eng
        self.idx = idx
        self.clock = clock


class T:
    __slots__ = ("t", "res", "name")

    def __init__(self, t, name):
        self.t = t
        self.res = Res(name)
        self.name = name

    def __getitem__(self, k):
        return self.t[k]


class Prog:
    def __init__(self, nc, stack):
        self.nc = nc
        self.stack = stack
        self.ops = {e: [] for e in ENGS}
        self.known = {e: {} for e in ENGS}
        self.n = {e: 0 for e in ENGS}
        self.last = {}
        self.dcount = {}
        self.needed = {e: set() for e in CENG}

    def sb(self, name, shape, dt=F32):
        t = self.stack.enter_context(self.nc.sbuf_tensor(name, list(shape), dt))
        return T(t, name)

    def ps(self, name, shape, dt=F32):
        t = self.stack.enter_context(self.nc.psum_tensor(name, list(shape), dt))
        return T(t, name)

    def _need(self, eng, known, waits, tok, raw, is_dma):
        if tok is None:
            return
        if tok.kind == "c":
            if tok.eng == eng and not raw and not is_dma and (not STRICT or eng == 'pe'):
                return
            if known.get(tok.eng, 0) >= tok.idx:
                return
            waits.append(("c", tok.eng, tok.idx))
            self.needed[tok.eng].add(tok.idx)
        else:
            if known.get(tok.eng, 0) >= tok.idx:
                return
            waits.append(("d", tok.eng, tok.idx))
        for k, v in tok.clock.items():
            if known.get(k, 0) < v:
                known[k] = v
        known[tok.eng] = tok.idx

    def _wait_list(self, eng, reads, writes, is_dma):
        known = self.known[eng]
        waits = []
        for r in reads:
            self._need(eng, known, waits, r.w, True, is_dma)
        for r in writes:
            self._need(eng, known, waits, r.w, False, is_dma)
            for tk in r.rs.values():
                self._need(eng, known, waits, tk, False, is_dma)
        return waits

    @staticmethod
    def _res(x):
        return x.res if isinstance(x, T) else x

    def op(self, eng, fn, reads=(), writes=()):
        reads = [self._res(r) for r in reads]
        writes = [self._res(r) for r in writes]
        waits = self._wait_list(eng, reads, writes, False)
        self.n[eng] += 1
        idx = self.n[eng]
        clock = {k: v for k, v in self.known[eng].items() if k in CENG}
        tok = Tok("c", eng, idx, clock)
        self.ops[eng].append((waits, fn, ("c", idx)))
        self.last[eng] = tok
        for r in reads:
            r.rs[eng] = tok
        for r in writes:
            r.w = tok
            r.rs = {}
        return tok

    def dma(self, eng, out_ap, in_ap, reads=(), writes=(), key=None):
        reads = [self._res(r) for r in reads]
        writes = [self._res(r) for r in writes]
        dkey = ("d", key)
        waits = self._wait_list(eng, reads, writes, True)
        cnt = self.dcount.get(dkey, 0) + 1
        self.dcount[dkey] = cnt
        clock = {k: v for k, v in self.known[eng].items() if k in CENG}
        tok = Tok("d", dkey, cnt, clock)

        nc = self.nc

        def fn(e, out_ap=out_ap, in_ap=in_ap):
            if out_ap.dtype == F32R:
                nc.dge_precook = False
                r = e.dma_start(out=out_ap, in_=in_ap)
                nc.dge_precook = True
                return r
            return e.dma_start(out=out_ap, in_=in_ap)

        self.ops[eng].append((waits, fn, ("d", dkey)))
        self.last[dkey] = tok
        for r in reads:
            r.rs[dkey] = tok
        for r in writes:
            r.w = tok
            r.rs = {}
        return tok

    def barrier(self, skip=(), skip_keys=()):
        toks = []
        for k, tok in self.last.items():
            if tok.kind == "d" and isinstance(tok.eng[1], tuple) and tok.eng[1][0] in skip_keys:
                continue
            toks.append(tok)
        for eng in ENGS:
            if eng in skip:
                continue
            known = self.known[eng]
            waits = []
            for tok in toks:
                self._need(eng, known, waits, tok, True, True)
            if waits:
                self.ops[eng].append((waits, None, None))

    def final_wait(self, eng, toks):
        known = self.known[eng]
        waits = []
        for tok in toks:
            self._need(eng, known, waits, tok, True, True)
        self.ops[eng].append((waits, None, None))

    def emit(self):
        nc = self.nc
        rank = {}
        for e in CENG:
            s = sorted(self.needed[e])
            rank[e] = {idx: i + 1 for i, idx in enumerate(s)}
            assert len(s) < 60000, (e, len(s))
        sems = {e: self.stack.enter_context(nc.semaphore("sem_" + e)) for e in CENG}
        dsems = {}
        for dkey in self.dcount:
            dsems[dkey] = self.stack.enter_context(nc.semaphore("dsem%d" % len(dsems)))
        block = self.stack.enter_context(nc.Block())

        def run(engname, eng):
            for waits, fn, info in self.ops[engname]:
                for kind, k, idx in waits:
                    if kind == "c":
                        eng.wait_ge(sems[k], rank[k][idx])
                    else:
                        eng.wait_ge(dsems[k], 16 * idx)
                if fn is None:
                    continue
                ins = fn(eng)
                if info[0] == "c":
                    if info[1] in rank[engname]:
                        ins.then_inc(sems[engname], 1)
                else:
                    ins.then_inc(dsems[info[1]], 16)

        @block.tensor
        def _(e):
            run("pe", e)

        @block.scalar
        def _(e):
            run("act", e)

        @block.vector
        def _(e):
            run("dve", e)

        @block.gpsimd
        def _(e):
            run("pool", e)

        @block.sync
        def _(e):
            run("sp", e)


D = 1024
S = 2048
TT = 512
NSUB = 4
D_IN = 10264
C_A = 0
C_BQKV = 512
C_BBETA = 2048
C_BGATE = 2056
C_CQKV = 2568
C_DQKV = 4104
C_DLR = 5640
C_DGATE = 5656
C_MERGE = 6168
ALPHA = 8.0 ** 0.25
LN_EPS = 1e-5
NORM_EPS = 1e-6

PP_CAW = 0
PP_CAB = 16
PP_RBA = 20
PP_RBX = 24
PP_LAM = 28
PP_GCW = 32
PP_GNW = 80
PP_LNW = 81
PP_DTB = 82
PP_ALOG = 86
PP_RB = 90
PP_GLB = 106
PP_LN1G = 618
PP_LN1B = 1642
PP_LN2G = 2666
PP_LN2B = 3690
PP_ONE = 4714
PP_EPS = 4715
PP_LNEPS = 4716
PP_N = 4717

K_ID = 0
K_LE = 128
K_GE = 256
K_GT = 384
K_ONE = 512
K_LE16 = 640
K_GT16 = 768
K_LT = 896
K_SBM = 1024
K_N = 1024 + 2048


def host_consts():
    p = np.arange(128)[:, None]
    f = np.arange(128)[None, :]
    c = np.zeros((128, K_N), np.float32)
    c[:, K_ID:K_ID + 128] = (p == f)
    c[:, K_LE:K_LE + 128] = (p <= f)
    c[:, K_GE:K_GE + 128] = (p >= f)
    c[:, K_GT:K_GT + 128] = (p > f)
    c[:, K_ONE:K_ONE + 128] = 1.0
    c[:, K_LE16:K_LE16 + 128] = (p <= f) * (-1.0 / 16.0)
    c[:, K_GT16:K_GT16 + 128] = (p > f) * (-1.0 / 16.0)
    c[:, K_LT:K_LT + 128] = (p < f)
    f5 = np.arange(512)[None, :]
    for jo in range(4):
        c[:, K_SBM + jo * 512:K_SBM + (jo + 1) * 512] = (jo * 128 + p < f5)
    return c


def build(nc, L=4, NSEQ=2, taps=(), en="ABCD", moe=True):
    NTOK = NSEQ * S
    st = ExitStack()
    P = Prog(nc, st)
    dr = lambda name, shape, kind="ExternalInput": nc.dram_tensor(name, list(shape), F32, kind=kind).ap()
    x_d = dr("x", [NTOK, D])
    w_in_d = dr("w_in", [4, D, D_IN])
    w_br_d = dr("w_branch", [4, 4, 512, D])
    w_out_d = dr("w_out", [4, D, D])
    w_g_d = dr("w_gate", [4, 16, D, 512])
    w_u_d = dr("w_up", [4, 16, D, 512])
    w_d_d = dr("w_down", [4, 16, 512, D])
    w_r_d = dr("w_router", [D, 16])
    pp_d = dr("pp", [4, 128, PP_N])
    bd_d = dr("bd", [4, 2, 4, 128, 128])
    gup_d = dr("gla_up", [4, 16, 512])
    k_d = dr("consts", [128, K_N])
    out_d = dr("out", [NTOK, D], "ExternalOutput")
    xs_d = dr("xs_scr", [2, NTOK, D], "Internal")
    tap_d = {}
    for name, shape in taps:
        tap_d[name] = dr("tap_" + name, shape, "ExternalOutput")
    dres = {}

    def DR(key):
        if key not in dres:
            dres[key] = Res("dram:" + str(key))
        return dres[key]

    out_toks = []

    kc = P.sb("kc", [128, 1024])
    kb = P.sb("kb", [128, 2048], BF16)
    pp = P.sb("pp_sb", [128, PP_N])
    NB = 4
    wring = [P.sb("wr%d" % i, [128, 2048]) for i in range(NB)]
    wri = [0]
    bdw = P.sb("bdw", [128, 8, 128])
    gup = P.sb("gup", [16, 512])
    wrt = P.sb("wrt", [128, 8, 16])
    lam8 = P.sb("lam8", [128, 4])
    aexp = P.sb("aexp", [128, 4])
    nge = P.sb("nge", [128, 128], BF16)
    none_ = P.sb("none", [128, 128], BF16)
    ARENA = 36000 - 12288 - 2048
    arena = P.sb("arena", [128, ARENA])
    arenaR = P.sb("arenaR", [128, 12288])
    xst = [P.sb("xst0", [128, 1024]), P.sb("xst1", [128, 1024])]
    banks = [P.ps("bank%d" % i, [128, 512]) for i in range(8)]

    aoff = [0]

    live = {"a": [], "r": []}

    def inherit(which, start, end, t_new):
        keep = []
        for (s0, e0, t_old) in live[which]:
            if s0 < end and start < e0:
                toks = list(t_old.res.rs.values())
                if t_old.res.w is not None:
                    toks.append(t_old.res.w)
                for tk in toks:
                    cur = t_new.res.rs.get(tk.eng)
                    if cur is None or cur.idx < tk.idx:
                        t_new.res.rs[tk.eng] = tk
            else:
                keep.append((s0, e0, t_old))
        keep.append((start, end, t_new))
        live[which] = keep

    def carve(name, ncols, shape=None, dt=F32):
        DBG[name] = (aoff[0], ncols)
        ap = arena[:, aoff[0]:aoff[0] + ncols]
        start = aoff[0]
        aoff[0] += ncols
        assert aoff[0] <= ARENA, (name, aoff[0])
        if dt == BF16:
            ap = ap.bitcast(BF16)
        if shape is not None and len(shape) == 3:
            ap = ap.rearrange("p (a b) -> p a b", a=shape[1])
        t_new = T(ap, name)
        inherit("a", start, start + ncols, t_new)
        return t_new

    roff = [0]

    def carveR(name, ncols, shape=None):
        ap = arenaR[:, roff[0]:roff[0] + ncols]
        roff[0] += ncols
        assert roff[0] <= 12288
        if shape is not None and len(shape) == 3:
            ap = ap.rearrange("p (a b) -> p a b", a=shape[1])
        t_new = T(ap, name)
        inherit("r", roff[0] - ncols, roff[0], t_new)
        return t_new

    def r32(ap):
        return ap.bitcast(F32R)

    def v3(ap, a):
        return ap.rearrange("p (a b) -> p a b", a=a)

    def mm(out_t, out_ap, l_t, l_ap, r_t, r_ap, start, stop):
        P.op("pe", lambda e: e.matmul(out_ap, l_ap, r_ap, start=start, stop=stop), reads=[l_t, r_t], writes=[out_t])

    def tr(out_t, out_ap, in_t, in_ap):
        P.op("pe", lambda e: e.transpose(out_ap, in_ap, kc[:, K_ID:K_ID + 128]), reads=[in_t, kc], writes=[out_t])

    def act(out_t, out_ap, in_t, in_ap, func, bias=None, scale=None, extra=()):
        kw = {}
        if bias is not None:
            kw["bias"] = bias
        if scale is not None:
            kw["scale"] = scale
        P.op("act", lambda e: e.activation(out_ap, in_ap, func, **kw), reads=[in_t] + list(extra), writes=[out_t])

    def ts(eng, out_t, out_ap, in_t, in_ap, s1, s2, op0, op1=None, extra=()):
        if op1 is None:
            P.op(eng, lambda e: e.tensor_scalar(out_ap, in_ap, s1, None, op0), reads=[in_t] + list(extra), writes=[out_t])
        else:
            P.op(eng, lambda e: e.tensor_scalar(out_ap, in_ap, s1, s2, op0, op1), reads=[in_t] + list(extra), writes=[out_t])

    def tt(eng, out_t, out_ap, a_t, a_ap, b_t, b_ap, op):
        P.op(eng, lambda e: e.tensor_tensor(out_ap, a_ap, b_ap, op), reads=[a_t, b_t], writes=[out_t])

    def stt(out_t, out_ap, a_t, a_ap, sc, b_t, b_ap, op0, op1, extra=()):
        P.op("dve", lambda e: e.scalar_tensor_tensor(out_ap, a_ap, sc, b_ap, op0, op1),
             reads=[a_t, b_t] + list(extra), writes=[out_t])

    def cp(eng, out_t, out_ap, in_t, in_ap):
        if eng == "act":
            P.op("act", lambda e: e.copy(out_ap, in_ap), reads=[in_t], writes=[out_t])
        else:
            P.op(eng, lambda e: e.tensor_copy(out_ap, in_ap), reads=[in_t], writes=[out_t])

    def memset(eng, out_t, out_ap, val):
        P.op(eng, lambda e: e.memset(out_ap, val), writes=[out_t])

    def wload(src_ap):
        buf = wring[wri[0] % NB]
        wri[0] += 1
        n = 1
        for s_ in src_ap.shape[1:]:
            n *= s_
        dst = buf[:, 0:n]
        if len(src_ap.shape) == 3:
            dst = dst.rearrange("p (a b) -> p a b", a=src_ap.shape[1])
        P.dma("sp", dst.bitcast(F32R), src_ap.bitcast(F32R), writes=[buf], key=("wr", buf.name))
        return buf, dst

    def tap(name, src_t, src_ap, dst_ap_fn):
        if name in tap_d:
            tk = P.dma("pool", dst_ap_fn(tap_d[name]), src_ap, reads=[src_t], key=("tap", name))
            out_toks.append(tk)

    def barrier():
        pass

    P.dma("sp", kc[:], k_d[:, 0:1024], writes=[kc], key="kc")
    P.dma("sp", arena[:, 0:2048], k_d[:, 1024:3072], writes=[arena], key="kcm")
    cp("dve", kb, kb[:], arena, arena[:, 0:2048])
    P.barrier()
    P.dma("sp", wrt[:], w_r_d.rearrange("(k p) e -> p k e", p=128), writes=[wrt], key="wrt")
    ts("dve", nge, nge[:], kc, kc[:, K_GE:K_GE + 128], -1.0, None, ALU.mult)
    ts("dve", none_, none_[:], kc, kc[:, K_ONE:K_ONE + 128], -1.0, None, ALU.mult)
    ident = kc[:, K_ID:K_ID + 128]
    ones = kc[:, K_ONE:K_ONE + 128]

    def win(l, c0, n):
        return wload(w_in_d[l].rearrange("(k p) n -> p k n", p=128)[:, :, c0:c0 + n])

    for l in range(L):
        barrier()
        P.dma("sp", pp[:], pp_d[l], writes=[pp], key="pp")
        P.dma("sp", bdw[:], bd_d[l].rearrange("a c p j -> p (a c) j"), writes=[bdw], key="bdw")
        P.dma("sp", gup[:], gup_d[l], writes=[gup], key="gup")
        act(lam8, lam8[:], pp, pp[:, PP_LAM:PP_LAM + 4], AF.Exp, scale=-1.0)
        act(lam8, lam8[:], lam8, lam8[:], AF.Ln, bias=pp[:, PP_ONE:PP_ONE + 1], extra=[pp])
        ts("dve", lam8, lam8[:], lam8, lam8[:], -8.0, None, ALU.mult)
        act(aexp, aexp[:], pp, pp[:, PP_ALOG:PP_ALOG + 4], AF.Exp)
        ts("dve", aexp, aexp[:], aexp, aexp[:], -1.0, None, ALU.mult)

        for s in range(NSEQ):
            barrier()
            aoff[0] = 0
            roff[0] = 0
            xT = carveR("xT", 4096, [128, 8, 512])
            merged = carveR("merged", 4096, [128, 8, 512])
            yn = carveR("yn", 2048, [128, 4, 512])
            KT = carve("KT", 4096, [128, 4, 2048], BF16)
            Vc = carve("Vc", 4096, [128, 16, 512], BF16)
            utail = carve("utail", 64, [128, 16, 4])
            hst = carve("hst", 4)
            S_d = [carve("S_d%d" % i_, 128) for i_ in range(4)]
            S_b = carve("S_b", 512, [128, 4, 128])
            S_db = [carve("S_db%d" % i_, 64, None, BF16) for i_ in range(4)]
            gsig = carve("gsig", 512)
            tmpm = carve("tmpm", 512)
            OV = aoff[0]
            memset("pool", utail, utail[:], 0.0)
            memset("pool", hst, hst[:], 0.0)
            for i_ in range(4):
                memset("pool", S_d[i_], S_d[i_][:], 0.0)
                memset("pool", S_db[i_], S_db[i_][:], 0.0)
            memset("pool", S_b, S_b[:], 0.0)

            for c in range(NSUB):
                row0 = s * S + c * TT

                def proj_fm(bk, wb, wap, j0, ncols):
                    for k in range(8):
                        mm(bk, bk[0:ncols, :], wb, r32(wap[:, k, j0:j0 + ncols]), xT, r32(xT[:, k, :]), k == 0, k == 7)

                def proj_tm(bk, sub, wb, wap, j0, ncols):
                    for k in range(8):
                        mm(bk, bk[:, 0:ncols], xT, r32(xT[:, k, sub * 128:(sub + 1) * 128]), wb, r32(wap[:, k, j0:j0 + ncols]),
                           k == 0, k == 7)

                def branch_merge(n, first):
                    for dq in range(4):
                        wbb, wbap = wload(w_br_d[l, n].rearrange("(k p) n -> p k n", p=128)[:, :, dq * 256:(dq + 1) * 256])
                        wgb, wgap = win(l, C_MERGE + n * 1024 + dq * 256, 256)
                        for h_ in range(2):
                            dch = dq * 2 + h_
                            bp = banks[4 + (dch % 2)]
                            bg = banks[6 + (dch % 2)]
                            for k in range(4):
                                mm(bp, bp[:, :], wbb, r32(wbap[:, k, h_ * 128:(h_ + 1) * 128]), yn, r32(yn[:, k, :]), k == 0, k == 3)
                            proj_fm(bg, wgb, wgap, h_ * 128, 128)
                            act(gsig, gsig[:], bg, bg[:], AF.Sigmoid)
                            if first:
                                tt("dve", merged, r32(merged[:, dch, :]), bp, bp[:], gsig, gsig[:], ALU.mult)
                            else:
                                tt("dve", tmpm, tmpm[:], bp, bp[:], gsig, gsig[:], ALU.mult)
                                tt("pool", merged, r32(merged[:, dch, :]), merged, merged[:, dch, :], tmpm, tmpm[:], ALU.add)

                def gated_norm(o_t, o_ap, h, gate_c0, nw_col, t1, t2, t3):
                    act(t1, t1[:], o_t, o_ap, AF.Square)
                    bs = banks[6]
                    mm(bs, bs[:], kc, ones, t1, t1[:], True, True)
                    act(t2, t2[:], bs, bs[:], AF.Ln, bias=pp[:, PP_EPS:PP_EPS + 1], scale=1.0 / 128.0, extra=[pp])
                    act(t2, t2[:], t2, t2[:], AF.Exp, scale=-0.5)
                    wgb, wgap = win(l, gate_c0 + h * 128, 128)
                    bg = banks[7]
                    proj_fm(bg, wgb, wgap, 0, 128)
                    act(t3, t3[:], bg, bg[:], AF.Silu)
                    stt(t1, t1[:], o_t, o_ap, pp[:, nw_col:nw_col + 1], t2, t2[:], ALU.mult, ALU.mult, extra=[pp])
                    tt("dve", yn, r32(yn[:, h, :]), t1, t1[:], t3, t3[:], ALU.mult)

                barrier()
                for a in range(4):
                    xs_ = xst[a % 2]
                    r0 = row0 + a * 128
                    if l == 0:
                        P.dma("sp", r32(xs_[:]), r32(x_d[r0:r0 + 128, :]), writes=[xs_], key=("xst", a % 2))
                    else:
                        P.dma("sp", r32(xs_[:]), r32(xs_d[0, r0:r0 + 128, :]), reads=[DR(("x0", s, c))], writes=[xs_], key=("xst", a % 2))
                    for hf in range(2):
                        bk = banks[hf]
                        for k in range(4):
                            kk = hf * 4 + k
                            tr(bk, bk[:, k * 128:(k + 1) * 128], xs_, xs_[:, kk * 128:(kk + 1) * 128])
                        cp("act" if hf else "dve", xT, r32(xT[:, hf * 4:hf * 4 + 4, a * 128:(a + 1) * 128]), bk, v3(bk[:], 4))

                first = True
                if "A" in en:
                    barrier()
                    aoff[0] = OV
                    ubuf = carve("ubuf", 516)
                    t1 = carve("t1", 512)
                    t2 = carve("t2", 512)
                    t3 = carve("t3", 512)
                    t4 = carve("t4", 512)
                    wa = [win(l, C_A, 256), win(l, C_A + 256, 256)]
                    for ch in range(4):
                        bk = banks[ch % 2]
                        proj_fm(bk, wa[ch // 2][0], wa[ch // 2][1], (ch % 2) * 128, 128)
                        cp("dve", ubuf, ubuf[:, 0:3], utail, utail[:, ch, 0:3])
                        cp("act", ubuf, ubuf[:, 3:515], bk, bk[:])
                        cp("pool", utail, utail[:, ch, 0:3], ubuf, ubuf[:, 512:515])
                        cw = lambda k: pp[:, PP_CAW + ch * 4 + k:PP_CAW + ch * 4 + k + 1]
                        ts("dve", t1, t1[:], ubuf, ubuf[:, 3:515], cw(3), pp[:, PP_CAB + ch:PP_CAB + ch + 1], ALU.mult, ALU.add, extra=[pp])
                        for k in range(3):
                            stt(t1, t1[:], ubuf, ubuf[:, k:k + 512], cw(k), t1, t1[:], ALU.mult, ALU.add, extra=[pp])
                        ba = banks[2]
                        bx = banks[3]
                        mm(ba, ba[:], bdw, bdw[:, ch, :], t1, t1[:], True, True)
                        mm(bx, bx[:], bdw, bdw[:, 4 + ch, :], t1, t1[:], True, True)
                        act(t2, t2[:], ba, ba[:], AF.Sigmoid, bias=pp[:, PP_RBA + ch:PP_RBA + ch + 1], extra=[pp])
                        act(t3, t3[:], bx, bx[:], AF.Sigmoid, bias=pp[:, PP_RBX + ch:PP_RBX + ch + 1], extra=[pp])
                        act(t2, t2[:], t2, t2[:], AF.Exp, scale=lam8[:, ch:ch + 1], extra=[lam8])
                        tt("dve", t4, t4[:], t2, t2[:], t2, t2[:], ALU.mult)
                        act(t4, t4[:], t4, t4[:], AF.Sqrt, bias=pp[:, PP_ONE:PP_ONE + 1], scale=-1.0, extra=[pp])
                        if c == 0:
                            memset("dve", t4, t4[:, 0:1], 1.0)
                        tt("dve", t3, t3[:], t3, t3[:], t1, t1[:], ALU.mult)
                        tt("dve", t3, t3[:], t3, t3[:], t4, t4[:], ALU.mult)
                        P.op("dve", lambda e, o_=r32(yn[:, ch, :]), a_=t2[:], b_=t3[:], i_=hst[:, ch:ch + 1]: e.tensor_tensor_scan(o_, a_, b_, i_, ALU.mult, ALU.add),
                             reads=[t2, t3, hst], writes=[yn])
                        cp("pool", hst, hst[:, ch:ch + 1], yn, yn[:, ch, 511:512])
                    tap("y_a", yn, yn[:], lambda d_: d_[:, :, c * TT:(c + 1) * TT])
                    branch_merge(0, first)
                    first = False

                if "B" in en:
                    barrier()
                    aoff[0] = OV
                    qnT = carve("qnT", 2048, [128, 4, 512])
                    knT = carve("knT", 2048, [128, 4, 512])
                    vT = carve("vT", 2048, [128, 4, 512])
                    ubuf = carve("ubuf", 516)
                    t1 = carve("t1", 512)
                    t2 = carve("t2", 512)
                    t3 = carve("t3", 512)
                    beta_t = carve("beta_t", 16)
                    g_t = carve("g_t", 16)
                    gcc = carve("gcc", 16)
                    bexp = carve("bexp", 16)
                    kdsc = carve("kdsc", 16)
                    gl = carve("gl", 16)
                    tsm = carve("tsm", 16)
                    tg = carve("tg", 128)
                    tmp1 = carve("tmp1", 128)
                    tmp2 = carve("tmp2", 128)
                    e1m = carve("e1m", 128)
                    e2m = carve("e2m", 128)
                    qdec = carve("qdec", 128)
                    AAT = [carve("AAT0", 256), carve("AAT1", 256)]
                    PT = carve("PT", 128)
                    attn_s = carve("attn_s", 128)
                    kbg = carve("kbg", 128)
                    kdec = carve("kdec", 128)
                    vb = carve("vb", 128)
                    u_s = carve("u_s", 128)
                    wT_s = carve("wT_s", 128)
                    vn_s = carve("vn_s", 128)
                    dests = [qnT, knT, vT]
                    for un in range(6):
                        wq = win(l, C_BQKV + un * 256, 256)
                        for h_ in range(2):
                            ci = un * 2 + h_
                            bk = banks[ci % 2]
                            proj_fm(bk, wq[0], wq[1], h_ * 128, 128)
                            cp("dve", ubuf, ubuf[:, 0:3], utail, utail[:, 4 + ci, 0:3])
                            cp("act", ubuf, ubuf[:, 3:515], bk, bk[:])
                            cp("pool", utail, utail[:, 4 + ci, 0:3], ubuf, ubuf[:, 512:515])
                            cw = lambda k: pp[:, PP_GCW + ci * 4 + k:PP_GCW + ci * 4 + k + 1]
                            ts("dve", t1, t1[:], ubuf, ubuf[:, 3:515], cw(3), None, ALU.mult, extra=[pp])
                            for k in range(3):
                                stt(t1, t1[:], ubuf, ubuf[:, k:k + 512], cw(k), t1, t1[:], ALU.mult, ALU.add, extra=[pp])
                            dst = dests[ci // 4]
                            act(dst, dst[:, ci % 4, :], t1, t1[:], AF.Silu)
                    for qi, dst in enumerate((qnT, knT)):
                        for h in range(4):
                            act(t1, t1[:], dst, dst[:, h, :], AF.Square)
                            bs = banks[2 + h % 2]
                            mm(bs, bs[:], kc, ones, t1, t1[:], True, True)
                            act(t2, t2[:], bs, bs[:], AF.Ln, bias=pp[:, PP_EPS:PP_EPS + 1], extra=[pp])
                            act(t2, t2[:], t2, t2[:], AF.Exp, scale=-0.5)
                            if qi == 0:
                                stt(dst, dst[:, h, :], dst, dst[:, h, :], 128.0 ** -0.5, t2, t2[:], ALU.mult, ALU.mult)
                            else:
                                tt("dve", dst, dst[:, h, :], dst, dst[:, h, :], t2, t2[:], ALU.mult)
                    wbg = win(l, C_BBETA, 8)
                    bsm = banks[4]
                    for sub in range(4):
                        for k in range(8):
                            mm(bsm, bsm[:, sub * 8:(sub + 1) * 8], xT, r32(xT[:, k, sub * 128:(sub + 1) * 128]), wbg[0], r32(wbg[1][:, k, 0:8]), k == 0, k == 7)
                    bsm3 = v3(bsm[:, 0:32], 4)
                    b3 = lambda t_: v3(t_[:], 4)
                    act(beta_t, b3(beta_t), bsm, bsm3[:, :, 0:4], AF.Sigmoid)
                    cp("act", tsm, b3(tsm), bsm, bsm3[:, :, 4:8])
                    tt("dve", tsm, b3(tsm), tsm, b3(tsm), pp, pp[:, PP_DTB:PP_DTB + 4].unsqueeze(1).to_broadcast([128, 4, 4]), ALU.add)
                    act(tsm, tsm[:], tsm, tsm[:], AF.Exp)
                    act(tsm, tsm[:], tsm, tsm[:], AF.Ln, bias=pp[:, PP_ONE:PP_ONE + 1], extra=[pp])
                    tt("dve", g_t, b3(g_t), tsm, b3(tsm), aexp, aexp[:].unsqueeze(1).to_broadcast([128, 4, 4]), ALU.mult)
                    bsm2 = banks[5]
                    for sub in range(4):
                        mm(bsm2, bsm2[:, sub * 4:(sub + 1) * 4], kc, kc[:, K_LE:K_LE + 128], g_t, g_t[:, sub * 4:(sub + 1) * 4], True, True)
                    for sub in range(4):
                        mm(bsm2, bsm2[:, 16 + sub * 4:16 + (sub + 1) * 4], kc, ones, g_t, g_t[:, sub * 4:(sub + 1) * 4], True, True)
                    cp("act", gcc, gcc[:], bsm2, bsm2[:, 0:16])
                    act(bexp, bexp[:], bsm2, bsm2[:, 0:16], AF.Exp)
                    tt("dve", bexp, bexp[:], bexp, bexp[:], beta_t, beta_t[:], ALU.mult)
                    act(gl, gl[:], bsm2, bsm2[:, 16:32], AF.Exp)
                    cp("act", kdsc, kdsc[:], bsm2, bsm2[:, 16:32])
                    tt("dve", kdsc, kdsc[:], kdsc, kdsc[:], gcc, gcc[:], ALU.subtract)
                    act(kdsc, kdsc[:], kdsc, kdsc[:], AF.Exp)
                    import os
                    GS = int(os.environ.get("GDN_STOP", "99"))
                    for h in range(4 if GS > 0 else 0):
                        bo = banks[5]
                        for sub in range(4):
                            cs = slice(sub * 128, (sub + 1) * 128)
                            si = sub * 4 + h
                            b1 = banks[0]
                            b2 = banks[1]
                            ts("dve", tg, tg[:], kc, kc[:, K_LE:K_LE + 128], g_t[:, si:si + 1], None, ALU.mult, extra=[g_t])
                            mm(b1, b1[:, 0:128], kc, ones, tg, tg[:], True, True)
                            mm(b1, b1[:, 128:256], knT, knT[:, h, cs], knT, knT[:, h, cs], True, True)
                            mm(b1, b1[:, 256:384], knT, knT[:, h, cs], qnT, qnT[:, h, cs], True, True)
                            mm(b1, b1[:, 384:512], knT, knT[:, h, cs], kc, ident, True, True)
                            mm(b2, b2[:, 0:128], vT, vT[:, h, cs], kc, ident, True, True)
                            ts("dve", tmp1, tmp1[:], b1, b1[:, 0:128], gcc[:, si:si + 1], 0.0, ALU.subtract, ALU.min, extra=[gcc])
                            ts("dve", tmp2, tmp2[:], b1, b1[:, 0:128], gcc[:, si:si + 1], 0.0, ALU.subtract, ALU.max, extra=[gcc])
                            act(tmp1, tmp1[:], tmp1, tmp1[:], AF.Exp)
                            act(tmp2, tmp2[:], tmp2, tmp2[:], AF.Exp, scale=-1.0)
                            tt("pool", e1m, e1m[:], tmp1, tmp1[:], kc, kc[:, K_LE:K_LE + 128], ALU.mult)
                            tt("pool", e2m, e2m[:], tmp2, tmp2[:], kc, kc[:, K_GT:K_GT + 128], ALU.mult)
                            act(tg, tg[:], b1, b1[:, 0:128], AF.Exp)
                            tt("dve", qdec, qdec[:], qnT, qnT[:, h, cs], tg, tg[:], ALU.mult)
                            cur = 0
                            stt(AAT[0], AAT[0][:, 0:128], b1, b1[:, 128:256], beta_t[:, si:si + 1], e2m, e2m[:], ALU.mult, ALU.mult, extra=[beta_t])
                            tt("dve", attn_s, attn_s[:], b1, b1[:, 256:384], e1m, e1m[:], ALU.mult)
                            ts("dve", kbg, kbg[:], b1, b1[:, 384:512], bexp[:, si:si + 1], None, ALU.mult, extra=[bexp])
                            ts("dve", kdec, kdec[:], b1, b1[:, 384:512], kdsc[:, si:si + 1], None, ALU.mult, extra=[kdsc])
                            ts("dve", vb, vb[:], b2, b2[:, 0:128], beta_t[:, si:si + 1], None, ALU.mult, extra=[beta_t])
                            if GS < 2:
                                continue
                            GV = int(os.environ.get("GDN_V", "9"))
                            mm(b2, b2[:, 128:256], AAT[0], AAT[0][:, 0:128], kc, ident, True, True)
                            if GV >= 2:
                                cp("act", AAT[0], AAT[0][:, 128:256], b2, b2[:, 128:256])
                            if GV >= 3:
                                act(PT, PT[:], b2, b2[:, 128:256], AF.Copy, scale=-1.0)
                                tt("pool", PT, PT[:], PT, PT[:], kc, ident, ALU.add)
                            for m in range(1, 1 + int(os.environ.get('GDN_LV', '6'))):
                                b3_ = banks[2 + m % 2]
                                A_c = AAT[cur]
                                A_n = AAT[1 - cur]
                                mm(b3_, b3_[:, 0:128], A_c, A_c[:, 128:256], A_c, A_c[:, 0:128], True, True)
                                if m < 6:
                                    mm(b3_, b3_[:, 128:256], A_c, A_c[:, 0:128], A_c, A_c[:, 128:256], True, True)
                                    cp("act", A_n, A_n[:, 0:256], b3_, b3_[:, 0:256])
                                else:
                                    cp("act", A_n, A_n[:, 0:128], b3_, b3_[:, 0:128])
                                mm(b3_, b3_[:, 256:384], A_n, A_n[:, 0:128], PT, PT[:], True, True)
                                tt("dve", PT, PT[:], PT, PT[:], b3_, b3_[:, 256:384], ALU.add)
                                cur = 1 - cur
                            if GS < 3:
                                continue
                            b5 = banks[4]
                            mm(b5, b5[:, 0:128], PT, PT[:], vb, vb[:], True, True)
                            mm(b5, b5[:, 128:256], kbg, kbg[:], PT, PT[:], True, True)
                            cp("act", u_s, u_s[:], b5, b5[:, 0:128])
                            cp("act", wT_s, wT_s[:], b5, b5[:, 128:256])
                            mm(b5, b5[:, 256:384], wT_s, wT_s[:], S_b, S_b[:, h, :], True, True)
                            tt("dve", vn_s, vn_s[:], u_s, u_s[:], b5, b5[:, 256:384], ALU.subtract)
                            mm(bo, bo[:, cs], S_b, S_b[:, h, :], qdec, qdec[:], True, False)
                            mm(bo, bo[:, cs], vn_s, vn_s[:], attn_s, attn_s[:], False, True)
                            mm(b5, b5[:, 384:512], kdec, kdec[:], vn_s, vn_s[:], True, True)
                            stt(S_b, S_b[:, h, :], S_b, S_b[:, h, :], gl[:, si:si + 1], b5, b5[:, 384:512], ALU.mult, ALU.add, extra=[gl])
                        if GS >= 3:
                            gated_norm(bo, bo[:], h, C_BGATE, PP_GNW, t1, t2, t3)
                    tap("y_b", yn, yn[:], lambda d_: d_[:, :, c * TT:(c + 1) * TT])
                    branch_merge(1, first)
                    first = False

                if "C" in en:
                    barrier()
                    aoff[0] = OV
                    qTb = carve("qTb", 2048, [128, 8, 512], BF16)
                    memset("pool", qTb, qTb[:], 0.0)
                    e_sb = [carve("e_sb0", 512), carve("e_sb1", 512)]
                    sp_sb = [carve("sp_sb0", 256, None, BF16), carve("sp_sb1", 256, None, BF16)]
                    spsum = [carve("spsum0", 512), carve("spsum1", 512)]
                    spsum_b = [carve("spsum_b0", 256, None, BF16), carve("spsum_b1", 256, None, BF16)]
                    w_sb = [carve("w_sb0", 256, None, BF16), carve("w_sb1", 256, None, BF16)]
                    qb0 = c * 4
                    for u in range(2):
                        wq = win(l, C_CQKV + u * 256, 256)
                        wk = win(l, C_CQKV + 512 + u * 256, 256)
                        for h_ in range(2):
                            pr = u * 2 + h_
                            bq = banks[0 + h_]
                            proj_fm(bq, wq[0], wq[1], h_ * 128, 128)
                            ts("dve", qTb, qTb[0:64, 2 * pr, :], bq, bq[0:64, :], 0.125, None, ALU.mult)
                            ts("dve", qTb, qTb[64:128, 2 * pr + 1, :], bq, bq[64:128, :], 0.125, None, ALU.mult)
                            bk_ = banks[2 + h_]
                            proj_fm(bk_, wk[0], wk[1], h_ * 128, 128)
                            cp("act", KT, KT[:, pr, c * TT:(c + 1) * TT], bk_, bk_[:])
                    wv0 = win(l, C_CQKV + 1024, 256)
                    wv1 = win(l, C_CQKV + 1280, 256)
                    for sub in range(4):
                        bv = banks[sub % 2]
                        proj_tm(bv, sub, wv0[0], wv0[1], 0, 256)
                        cp("act", Vc, Vc[:, qb0 + sub, 0:256], bv, bv[:, 0:256])
                        bv2 = banks[2 + sub % 2]
                        proj_tm(bv2, sub, wv1[0], wv1[1], 0, 256)
                        cp("dve", Vc, Vc[:, qb0 + sub, 256:512], bv2, bv2[:, 0:256])
                    for pr in range(4):
                        nkb = qb0 + 4
                        for hh in range(2):
                            memset("pool", spsum[hh], spsum[hh][:], 0.0)
                        for step, J in enumerate(range(nkb - 1, -1, -1)):
                            for hh in range(2):
                                bo = banks[6 + hh]
                                h = pr * 2 + hh
                                e_h, sp_h, su_h, sub_h, w_h = e_sb[hh], sp_sb[hh], spsum[hh], spsum_b[hh], w_sb[hh]
                                jo = J - qb0
                                q0 = max(jo, 0) * 128
                                qs = slice(q0, 512)
                                dg = slice(q0, q0 + 128)
                                bz = banks[hh + 4 * (step % 2)]
                                bd_ = banks[2 + hh]
                                mm(bz, bz[:, qs], KT, KT[:, pr, J * 128:(J + 1) * 128], qTb, qTb[:, h, qs], True, True)
                                act(e_h, e_h[:, qs], bz, bz[:, qs], AF.Exp)
                                act(sp_h, sp_h[:, qs], e_h, e_h[:, qs], AF.Ln, bias=pp[:, PP_ONE:PP_ONE + 1], extra=[pp])
                                if jo >= 0:
                                    tt("pool", sp_h, sp_h[:, dg], sp_h, sp_h[:, dg], kb, kb[:, 0:128], ALU.mult)
                                mm(bd_, bd_[:, qs], KT, KT[:, pr, J * 128:(J + 1) * 128], qTb, qTb[:, h, qs], True, False)
                                mm(bd_, bd_[:, qs], nge, nge[:], sp_h, sp_h[:, qs], False, step == 0)
                                if step > 0:
                                    mm(bd_, bd_[:, qs], none_, none_[:], sub_h, sub_h[:, qs], False, True)
                                act(w_h, w_h[:, qs], bd_, bd_[:, qs], AF.Exp)
                                if jo >= 0:
                                    tt("pool", w_h, w_h[:, dg], w_h, w_h[:, dg], kb, kb[:, 0:128], ALU.mult)
                                mm(bo, bo[:, qs], Vc, Vc[:, J, pr * 128:(pr + 1) * 128], w_h, w_h[:, qs], step == 0, J == 0)
                                if J > 0:
                                    tt("dve", su_h, su_h[:, qs], su_h, su_h[:, qs], sp_h, sp_h[:, qs], ALU.add)
                                    cp("pool", sub_h, sub_h[:], su_h, su_h[:])
                        for hh in range(2):
                            bo = banks[6 + hh]
                            ps_ = slice(hh * 64, hh * 64 + 64)
                            cp("act", yn, r32(yn[ps_, pr, :]), bo, bo[ps_, :])
                    tap("y_c", yn, yn[:], lambda d_: d_[:, :, c * TT:(c + 1) * TT])
                    branch_merge(2, first)
                    first = False

                if "D" in en:
                    barrier()
                    aoff[0] = OV
                    t1 = carve("t1", 512)
                    t2 = carve("t2", 512)
                    t3 = carve("t3", 512)
                    sp_tok = carve("sp_tok", 2048, [128, 4, 512])
                    qd = carve("qd", 1024, [128, 4, 512], BF16)
                    ki = carve("ki", 1024, [128, 4, 512], BF16)
                    kdt = carve("kdt", 1024, [128, 4, 512], BF16)
                    vtk = carve("vtk", 1024, [128, 4, 512], BF16)
                    lrT = carve("lrT", 512)
                    glast = carve("glast", 16)
                    attn = [carve("attn%d" % i_, 64, None, BF16) for i_ in range(4)]
                    wl = win(l, C_DLR, 16)
                    b0 = banks[0]
                    proj_fm(b0, wl[0], wl[1], 0, 16)
                    cp("act", lrT, lrT[0:16, :], b0, b0[0:16, :])
                    for sub in range(4):
                        bg_ = banks[1 + sub % 2]
                        mm(bg_, bg_[:], lrT, lrT[0:16, sub * 128:(sub + 1) * 128], gup, gup[:], True, True)
                        tt("dve", t1, t1[:], bg_, bg_[:], pp, pp[:, PP_GLB:PP_GLB + 512], ALU.add)
                        act(t1, t1[:], t1, t1[:], AF.Exp, scale=-1.0)
                        act(sp_tok, sp_tok[:, sub, :], t1, t1[:], AF.Ln, bias=pp[:, PP_ONE:PP_ONE + 1], extra=[pp])
                    wk0 = win(l, C_DQKV + 512, 256)
                    wk1 = win(l, C_DQKV + 768, 256)
                    for sub in range(4):
                        br = banks[3]
                        mm(br, br[:], kc, kc[:, K_GT16:K_GT16 + 128], sp_tok, sp_tok[:, sub, :], True, True)
                        act(t2, t2[:], br, br[:], AF.Exp)
                        for hf, wk_ in enumerate((wk0, wk1)):
                            bkk = banks[4 + hf]
                            proj_tm(bkk, sub, wk_[0], wk_[1], 0, 256)
                            tt("dve", kdt, kdt[:, sub, hf * 256:(hf + 1) * 256], bkk, bkk[:, 0:256], t2, t2[:, hf * 256:(hf + 1) * 256], ALU.mult)
                    wv0 = win(l, C_DQKV + 1024, 256)
                    wv1 = win(l, C_DQKV + 1280, 256)
                    for sub in range(4):
                        for hf, wvv in enumerate((wv0, wv1)):
                            bvv = banks[6 + hf]
                            proj_tm(bvv, sub, wvv[0], wvv[1], 0, 256)
                            cp("act", vtk, vtk[:, sub, hf * 256:(hf + 1) * 256], bvv, bvv[:, 0:256])
                    for h in range(4):
                        bb = banks[0]
                        for sub in range(4):
                            mm(bb, bb[:, sub * 128:(sub + 1) * 128], sp_tok, sp_tok[:, sub, h * 128:(h + 1) * 128],
                               kc, kc[:, K_LE16:K_LE16 + 128], True, True)
                        act(t1, t1[:], bb, bb[:], AF.Exp)
                        act(t2, t2[:], bb, bb[:], AF.Exp, scale=-1.0)
                        for sub in range(4):
                            cp("pool", glast, glast[:, h * 4 + sub:h * 4 + sub + 1], t1, t1[:, sub * 128 + 127:sub * 128 + 128])
                        wq = win(l, C_DQKV + h * 128, 128)
                        bq = banks[1]
                        proj_fm(bq, wq[0], wq[1], 0, 128)
                        stt(qd, qd[:, h, :], bq, bq[:], 128.0 ** -0.5, t1, t1[:], ALU.mult, ALU.mult)
                        wkf = win(l, C_DQKV + 512 + h * 128, 128)
                        bk2 = banks[2]
                        proj_fm(bk2, wkf[0], wkf[1], 0, 128)
                        tt("dve", ki, ki[:, h, :], bk2, bk2[:], t2, t2[:], ALU.mult)
                    for sub in range(4):
                        for h in range(4):
                            bo = banks[h]
                            at_h = attn[h]
                            cs = slice(sub * 128, (sub + 1) * 128)
                            hs = slice(h * 128, (h + 1) * 128)
                            ba_ = banks[4 + h % 2]
                            mm(ba_, ba_[:, 0:128], ki, ki[:, h, cs], qd, qd[:, h, cs], True, True)
                            tt("dve", at_h, at_h[:], ba_, ba_[:, 0:128], kc, kc[:, K_LE:K_LE + 128], ALU.mult)
                            mm(bo, bo[:, cs], vtk, vtk[:, sub, hs], at_h, at_h[:], True, False)
                            mm(bo, bo[:, cs], S_db[h], S_db[h][:], qd, qd[:, h, cs], False, True)
                            bs_ = banks[6 + h % 2]
                            mm(bs_, bs_[:, 0:128], kdt, kdt[:, sub, hs], vtk, vtk[:, sub, hs], True, True)
                            stt(S_d[h], S_d[h][:], S_d[h], S_d[h][:], glast[:, h * 4 + sub:h * 4 + sub + 1], bs_, bs_[:, 0:128],
                                ALU.mult, ALU.add, extra=[glast])
                            cp("pool", S_db[h], S_db[h][:], S_d[h], S_d[h][:])
                    for h in range(4):
                        gated_norm(banks[h], banks[h][:], h, C_DGATE, PP_LNW, t1, t2, t3)
                    tap("y_d", yn, yn[:], lambda d_: d_[:, :, c * TT:(c + 1) * TT])
                    branch_merge(3, first)
                    first = False

                barrier()
                aoff[0] = OV
                x_tok = carve("x_tok", 4096, [128, 4, 1024])
                lnt = carve("lnt", 64)
                src = (x_d if l == 0 else xs_d[0])[row0:row0 + TT, :]
                P.dma("pool", x_tok[:], src.rearrange("(a p) d -> p a d", p=128),
                      reads=([DR(("x0", s, c))] if l > 0 else []), writes=[x_tok], key="xtok")
                for dq in range(4):
                    wo = wload(w_out_d[l].rearrange("(k p) n -> p k n", p=128)[:, :, dq * 256:(dq + 1) * 256])
                    for sub in range(4):
                        bo = banks[sub % 4]
                        for k in range(8):
                            mm(bo, bo[:, 0:256], merged, r32(merged[:, k, sub * 128:(sub + 1) * 128]), wo[0], r32(wo[1][:, k, :]), k == 0, k == 7)
                        stt(x_tok, x_tok[:, sub, dq * 256:(dq + 1) * 256], x_tok, x_tok[:, sub, dq * 256:(dq + 1) * 256], ALPHA,
                            bo, bo[:, 0:256], ALU.mult, ALU.add)
                for sub in range(4):
                    layer_norm(P, x_tok, x_tok[:, sub, :], lnt, pp, PP_LN1G, PP_LN1B)
                tap("x1", x_tok, x_tok[:], lambda d_: d_[c * TT:(c + 1) * TT, :].rearrange("(a p) d -> p a d", p=128))
                dst_d = xs_d[1] if moe else (out_d if l == L - 1 else xs_d[0])
                tk = P.dma("pool", dst_d[row0:row0 + TT, :].rearrange("(a p) d -> p a d", p=128), x_tok[:],
                           reads=[x_tok], writes=[DR((("x1" if moe else "x0"), s, c))], key="xst_out")
                if not moe and l == L - 1:
                    out_toks.append(tk)

            if not moe:
                continue
            for g in range(2):
                barrier()
                aoff[0] = 0
                roff[0] = 0
                x1T = carveR("x1T", 8192, [128, 8, 1024])
                hT = [carveR("hT0", 2048, [128, 2, 1024]), carveR("hT1", 2048, [128, 2, 1024])]
                yacc = carve("yacc", 8192, [128, 8, 1024])
                sg = [carve("sg0", 512), carve("sg1", 512)]
                sc = carve("sc", 128, [128, 8, 16])
                bi = carve("bi", 128, [128, 8, 16])
                mb = carve("mb", 128, [128, 8, 16])
                eq = carve("eq", 128, [128, 8, 16])
                sel = carve("sel", 128, [128, 8, 16])
                comb = carve("comb", 128, [128, 8, 16])
                m1 = carve("m1", 32)
                m2 = carve("m2", 32)
                gsel = carve("gsel", 32)
                gm = carve("gm", 8)
                lnt = carve("lnt", 64)
                grow0 = s * S + g * 1024
                P.dma("pool", yacc[:], xs_d[1, grow0:grow0 + 1024, :].rearrange("(a p) d -> p a d", p=128),
                      reads=[DR(("x1", s, 2 * g)), DR(("x1", s, 2 * g + 1))], writes=[yacc], key="yacc")
                for a in range(8):
                    xs_ = xst[a % 2]
                    P.dma("sp", r32(xs_[:]), r32(xs_d[1, grow0 + a * 128:grow0 + (a + 1) * 128, :]),
                          reads=[DR(("x1", s, 2 * g)), DR(("x1", s, 2 * g + 1))], writes=[xs_], key=("xst", a % 2))
                    for hf in range(2):
                        bk = banks[hf]
                        for k in range(4):
                            kk = hf * 4 + k
                            tr(bk, bk[:, k * 128:(k + 1) * 128], xs_, xs_[:, kk * 128:(kk + 1) * 128])
                        cp("act" if hf else "dve", x1T, r32(x1T[:, hf * 4:hf * 4 + 4, a * 128:(a + 1) * 128]), bk, v3(bk[:], 4))
                brt = banks[2]
                for a in range(8):
                    for k in range(8):
                        mm(brt, brt[:, a * 16:(a + 1) * 16], x1T, x1T[:, k, a * 128:(a + 1) * 128], wrt, wrt[:, k, :], k == 0, k == 7)
                act(sc, sc[:], brt, v3(brt[:, 0:128], 8), AF.Sigmoid)
                rb_b = pp[:, PP_RB:PP_RB + 16].unsqueeze(1).to_broadcast([128, 8, 16])
                tt("dve", bi, bi[:], sc, sc[:], pp, rb_b, ALU.add)
                bi4 = bi[:].rearrange("p a (g e) -> p (a g) e", g=4)
                mb4 = mb[:].rearrange("p a (g e) -> p (a g) e", g=4)
                eq4 = eq[:].rearrange("p a (g e) -> p (a g) e", g=4)
                P.op("dve", lambda e, o_=m1[:], i_=bi4: e.tensor_reduce(o_, i_, AX.X, ALU.max), reads=[bi], writes=[m1])
                tt("dve", eq, eq4, bi, bi4, m1, m1[:].unsqueeze(2).to_broadcast([128, 32, 4]), ALU.is_equal)
                stt(mb, mb4, eq, eq4, -1e30, bi, bi4, ALU.mult, ALU.add)
                P.op("dve", lambda e, o_=m2[:], i_=mb4: e.tensor_reduce(o_, i_, AX.X, ALU.max), reads=[mb], writes=[m2])
                tt("dve", m1, m1[:], m1, m1[:], m2, m2[:], ALU.add)
                m1g = m1[:].rearrange("p (a g) -> p a g", g=4)
                P.op("dve", lambda e, o_=gm[:], i_=m1g: e.tensor_reduce(o_, i_, AX.X, ALU.max), reads=[m1], writes=[gm])
                tt("dve", gsel, gsel[:].rearrange("p (a g) -> p a g", g=4), m1, m1g, gm, gm[:].unsqueeze(2).to_broadcast([128, 8, 4]), ALU.is_equal)
                ts("dve", gsel, gsel[:], gsel, gsel[:], -1.0, 1e30, ALU.add, ALU.mult)
                tt("dve", mb, mb4, bi, bi4, gsel, gsel[:].unsqueeze(2).to_broadcast([128, 32, 4]), ALU.add)
                P.op("dve", lambda e, o_=gm[:], i_=mb[:]: e.tensor_reduce(o_, i_, AX.X, ALU.max), reads=[mb], writes=[gm])
                tt("dve", sel, sel[:], mb, mb[:], gm, gm[:].unsqueeze(2).to_broadcast([128, 8, 16]), ALU.is_equal)
                stt(mb, mb[:], sel, sel[:], -1e30, mb, mb[:], ALU.mult, ALU.add)
                P.op("dve", lambda e, o_=gm[:], i_=mb[:]: e.tensor_reduce(o_, i_, AX.X, ALU.max), reads=[mb], writes=[gm])
                tt("dve", eq, eq[:], mb, mb[:], gm, gm[:].unsqueeze(2).to_broadcast([128, 8, 16]), ALU.is_equal)
                tt("dve", sel, sel[:], sel, sel[:], eq, eq[:], ALU.add)
                tt("dve", comb, comb[:], sel, sel[:], sc, sc[:], ALU.mult)
                P.op("dve", lambda e, o_=gm[:], i_=comb[:]: e.tensor_reduce(o_, i_, AX.X, ALU.add), reads=[comb], writes=[gm])
                P.op("dve", lambda e, o_=gm[:]: e.reciprocal(o_, o_), reads=[gm], writes=[gm])
                tt("dve", comb, comb[:], comb, comb[:], gm, gm[:].unsqueeze(2).to_broadcast([128, 8, 16]), ALU.mult)
                tap("comb", comb, comb[:], lambda d_: d_[g])
                for a in range(8):
                    ts("pool", yacc, yacc[:, a, :], yacc, yacc[:, a, :], ALPHA, None, ALU.mult)
                for ex in range(16):
                    for fh in range(2):
                        u_i = ex * 2 + fh
                        wg = wload(w_g_d[l, ex].rearrange("(k p) f -> p k f", p=128)[:, :, fh * 256:(fh + 1) * 256])
                        wu = wload(w_u_d[l, ex].rearrange("(k p) f -> p k f", p=128)[:, :, fh * 256:(fh + 1) * 256])
                        wd = wload(w_d_d[l, ex, fh * 256:(fh + 1) * 256, :].rearrange("(a p) d -> p a d", p=128))
                        hb = hT[u_i % 2]
                        for fc in range(2):
                            for th in range(2):
                                bg_ = banks[(fc * 2 + th) % 2]
                                bu_ = banks[2 + (fc * 2 + th) % 2]
                                for k in range(8):
                                    mm(bg_, bg_[:], wg[0], r32(wg[1][:, k, fc * 128:(fc + 1) * 128]), x1T, r32(x1T[:, k, th * 512:(th + 1) * 512]), k == 0, k == 7)
                                for k in range(8):
                                    mm(bu_, bu_[:], wu[0], r32(wu[1][:, k, fc * 128:(fc + 1) * 128]), x1T, r32(x1T[:, k, th * 512:(th + 1) * 512]), k == 0, k == 7)
                                sgt = sg[(fc * 2 + th) % 2]
                                act(sgt, sgt[:], bg_, bg_[:], AF.Silu)
                                tt("dve", hb, r32(hb[:, fc, th * 512:(th + 1) * 512]), bu_, bu_[:], sgt, sgt[:], ALU.mult)
                        for a in range(8):
                            for dh in range(2):
                                by = banks[4 + (a * 2 + dh) % 4]
                                for fc in range(2):
                                    mm(by, by[:], hb, r32(hb[:, fc, a * 128:(a + 1) * 128]), wd[0], r32(wd[1][:, fc, dh * 512:(dh + 1) * 512]), fc == 0, fc == 1)
                                stt(yacc, yacc[:, a, dh * 512:(dh + 1) * 512], by, by[:], comb[:, a, ex:ex + 1],
                                    yacc, yacc[:, a, dh * 512:(dh + 1) * 512], ALU.mult, ALU.add, extra=[comb])
                for a in range(8):
                    layer_norm(P, yacc, yacc[:, a, :], lnt, pp, PP_LN2G, PP_LN2B)
                last = (l == L - 1)
                dst_d = out_d if last else xs_d[0]
                tk = P.dma("pool", dst_d[grow0:grow0 + 1024, :].rearrange("(a p) d -> p a d", p=128), yacc[:],
                           reads=[yacc], writes=[DR(("x0", s, 2 * g)), DR(("x0", s, 2 * g + 1))], key="yst_out")
                if last:
                    out_toks.append(tk)
    P.final_wait("sp", out_toks)
    P.emit()
    return st


def layer_norm(P, x_t, x_ap, lnt, pp, gcol, bcol):
    stats = lnt[:, 0:12].rearrange("p (a b) -> p a b", a=2)
    mv = lnt[:, 12:14]
    rstd = lnt[:, 14:15]
    for hf in range(2):
        P.op("dve", lambda e, hf=hf: e.bn_stats(stats[:, hf, :], x_ap[:, hf * 512:(hf + 1) * 512]), reads=[x_t], writes=[lnt])
    P.op("dve", lambda e: e.bn_aggr(mv, lnt[:, 0:12]), reads=[lnt], writes=[lnt])
    P.op("act", lambda e: e.activation(rstd, lnt[:, 13:14], AF.Ln, bias=pp[:, PP_LNEPS:PP_LNEPS + 1]), reads=[lnt, pp], writes=[lnt])
    P.op("act", lambda e: e.activation(rstd, rstd, AF.Exp, scale=-0.5), reads=[lnt], writes=[lnt])
    P.op("dve", lambda e: e.tensor_scalar(x_ap, x_ap, lnt[:, 12:13], rstd, ALU.subtract, ALU.mult), reads=[x_t, lnt], writes=[x_t])
    P.op("pool", lambda e: e.tensor_tensor(x_ap, x_ap, pp[:, gcol:gcol + 1024], ALU.mult), reads=[x_t, pp], writes=[x_t])
    P.op("pool", lambda e: e.tensor_tensor(x_ap, x_ap, pp[:, bcol:bcol + 1024], ALU.add), reads=[x_t, pp], writes=[x_t])


def _pack_inputs(inp):
    L = 4
    pp = np.zeros((L, 128, PP_N), np.float32)
    bd = np.zeros((L, 2, 4, 128, 128), np.float32)
    for l in range(L):
        pp[l, :, PP_CAW:PP_CAW + 16] = inp["conv_a_w"][l].reshape(4, 4, 128).transpose(2, 1, 0).reshape(128, 16)
        pp[l, :, PP_CAB:PP_CAB + 4] = inp["conv_a_b"][l].reshape(4, 128).T
        pp[l, :, PP_RBA:PP_RBA + 4] = inp["rg_b_a"][l].reshape(4, 128).T
        pp[l, :, PP_RBX:PP_RBX + 4] = inp["rg_b_x"][l].reshape(4, 128).T
        pp[l, :, PP_LAM:PP_LAM + 4] = inp["rg_lambda"][l].reshape(4, 128).T
        pp[l, :, PP_GCW:PP_GCW + 48] = inp["gdn_conv_w"][l].reshape(4, 12, 128).transpose(2, 1, 0).reshape(128, 48)
        pp[l, :, PP_GNW] = inp["gdn_norm_w"][l]
        pp[l, :, PP_LNW] = inp["gla_norm_w"][l]
        pp[l, :, PP_DTB:PP_DTB + 4] = inp["gdn_dt_bias"][l][None, :]
        pp[l, :, PP_ALOG:PP_ALOG + 4] = inp["gdn_a_log"][l][None, :]
        pp[l, :, PP_RB:PP_RB + 16] = inp["router_bias"][None, :]
        pp[l, :, PP_GLB:PP_GLB + 512] = inp["gla_b_gate"][l][None, :]
        pp[l, :, PP_LN1G:PP_LN1G + 1024] = inp["ln1_g"][l][None, :]
        pp[l, :, PP_LN1B:PP_LN1B + 1024] = inp["ln1_b"][l][None, :]
        pp[l, :, PP_LN2G:PP_LN2G + 1024] = inp["ln2_g"][l][None, :]
        pp[l, :, PP_LN2B:PP_LN2B + 1024] = inp["ln2_b"][l][None, :]
        pp[l, :, PP_ONE] = 1.0
        pp[l, :, PP_EPS] = NORM_EPS
        pp[l, :, PP_LNEPS] = LN_EPS
        for a, nm in enumerate(("rg_w_a", "rg_w_x")):
            w = inp[nm][l]
            for ch in range(4):
                for gb in range(2):
                    bd[l, a, ch, gb * 64:(gb + 1) * 64, gb * 64:(gb + 1) * 64] = w[ch * 2 + gb]
    return pp, bd


_NC_CACHE = {}


def kernel(**inp):
    inp = {k: np.ascontiguousarray(np.asarray(v)) for k, v in inp.items()}
    n = 8
    nseq = 2
    pp, bd = _pack_inputs(inp)
    consts = host_consts()
    if "nc" not in _NC_CACHE:
        nc = bass.Bass("TRN2", target_bir_lowering=False)
        st = build(nc, L=4, NSEQ=nseq)
        _NC_CACHE["nc"] = (nc, st)
    nc = _NC_CACHE["nc"][0]
    x = inp["x"].reshape(n, nseq * S, D)
    shared = {"w_in": inp["w_in"], "w_branch": inp["w_branch"], "w_out": inp["w_out"], "w_gate": inp["w_gate"],
              "w_up": inp["w_up"], "w_down": inp["w_down"], "w_router": inp["w_router"], "pp": pp, "bd": bd,
              "gla_up": inp["gla_w_gate_up"], "consts": consts}
    in_maps = [dict(shared, x=x[i]) for i in range(n)]
    res = run_bass_kernel_spmd(nc, in_maps, core_ids=list(range(n)))
    out = np.stack([np.asarray(r["out"]) for r in res.results], 0)
    return out.reshape(16, S, D).astype(np.float32)
```

```python
from contextlib import ExitStack
import numpy as np
import concourse.bass as bass
import concourse.mybir as mybir
from concourse.bass_utils import run_bass_kernel_spmd

F32 = mybir.dt.float32
F32R = mybir.dt.float32r
BF16 = mybir.dt.bfloat16
AF = mybir.ActivationFunctionType
ALU = mybir.AluOpType
AX = mybir.AxisListType

STRICT = False
DBG = {}
ENGS = ("pe", "act", "dve", "pool", "sp")
CENG = ("pe", "act", "dve", "pool")


class Res:
    __slots__ = ("name", "w", "rs")

    def __init__(self, name):
        self.name = name
        self.w = None
        self.rs = {}


class Tok:
    __slots__ = ("kind", "eng", "idx", "clock")

    def __init__(self, kind, eng, idx, clock):
        self.kind = kind
        self.eng = eng
        self.idx = idx
        self.clock = clock


class T:
    __slots__ = ("t", "res", "name")

    def __init__(self, t, name):
        self.t = t
        self.res = Res(name)
        self.name = name

    def __getitem__(self, k):
        return self.t[k]


class Prog:
    def __init__(self, nc, stack):
        self.nc = nc
        self.stack = stack
        self.ops = {e: [] for e in ENGS}
        self.known = {e: {} for e in ENGS}
        self.n = {e: 0 for e in ENGS}
        self.last = {}
        self.dcount = {}
        self.needed = {e: set() for e in CENG}

    def sb(self, name, shape, dt=F32):
        t = self.stack.enter_context(self.nc.sbuf_tensor(name, list(shape), dt))
        return T(t, name)

    def ps(self, name, shape, dt=F32):
        t = self.stack.enter_context(self.nc.psum_tensor(name, list(shape), dt))
        return T(t, name)

    def _need(self, eng, known, waits, tok, raw, is_dma):
        if tok is None:
            return
        if tok.kind == "c":
            if tok.eng == eng and not raw and not is_dma and (not STRICT or eng == 'pe'):
                return
            if known.get(tok.eng, 0) >= tok.idx:
                return
            waits.append(("c", tok.eng, tok.idx))
            self.needed[tok.eng].add(tok.idx)
        else:
            if known.get(tok.eng, 0) >= tok.idx:
                return
            waits.append(("d", tok.eng, tok.idx))
        for k, v in tok.clock.items():
            if known.get(k, 0) < v:
                known[k] = v
        known[tok.eng] = tok.idx

    def _wait_list(self, eng, reads, writes, is_dma):
        known = self.known[eng]
        waits = []
        for r in reads:
            self._need(eng, known, waits, r.w, True, is_dma)
        for r in writes:
            self._need(eng, known, waits, r.w, False, is_dma)
            for tk in r.rs.values():
                self._need(eng, known, waits, tk, False, is_dma)
        return waits

    @staticmethod
    def _res(x):
        return x.res if isinstance(x, T) else x

    def op(self, eng, fn, reads=(), writes=()):
        reads = [self._res(r) for r in reads]
        writes = [self._res(r) for r in writes]
        waits = self._wait_list(eng, reads, writes, False)
        self.n[eng] += 1
        idx = self.n[eng]
        clock = {k: v for k, v in self.known[eng].items() if k in CENG}
        tok = Tok("c", eng, idx, clock)
        self.ops[eng].append((waits, fn, ("c", idx)))
        self.last[eng] = tok
        for r in reads:
            r.rs[eng] = tok
        for r in writes:
            r.w = tok
            r.rs = {}
        return tok

    def dma(self, eng, out_ap, in_ap, reads=(), writes=(), key=None):
        reads = [self._res(r) for r in reads]
        writes = [self._res(r) for r in writes]
        dkey = ("d", key)
        waits = self._wait_list(eng, reads, writes, True)
        cnt = self.dcount.get(dkey, 0) + 1
        self.dcount[dkey] = cnt
        clock = {k: v for k, v in self.known[eng].items() if k in CENG}
        tok = Tok("d", dkey, cnt, clock)

        nc = self.nc

        def fn(e, out_ap=out_ap, in_ap=in_ap):
            if out_ap.dtype == F32R:
                nc.dge_precook = False
                r = e.dma_start(out=out_ap, in_=in_ap)
                nc.dge_precook = True
                return r
            return e.dma_start(out=out_ap, in_=in_ap)

        self.ops[eng].append((waits, fn, ("d", dkey)))
        self.last[dkey] = tok
        for r in reads:
            r.rs[dkey] = tok
        for r in writes:
            r.w = tok
            r.rs = {}
        return tok

    def barrier(self, skip=(), skip_keys=()):
        toks = []
        for k, tok in self.last.items():
            if tok.kind == "d" and isinstance(tok.eng[1], tuple) and tok.eng[1][0] in skip_keys:
                continue
            toks.append(tok)
        for eng in ENGS:
            if eng in skip:
                continue
            known = self.known[eng]
            waits = []
            for tok in toks:
                self._need(eng, known, waits, tok, True, True)
            if waits:
                self.ops[eng].append((waits, None, None))

    def final_wait(self, eng, toks):
        known = self.known[eng]
        waits = []
        for tok in toks:
            self._need(eng, known, waits, tok, True, True)
        self.ops[eng].append((waits, None, None))

    def emit(self):
        nc = self.nc
        rank = {}
        for e in CENG:
            s = sorted(self.needed[e])
            rank[e] = {idx: i + 1 for i, idx in enumerate(s)}
            assert len(s) < 60000, (e, len(s))
        sems = {e: self.stack.enter_context(nc.semaphore("sem_" + e)) for e in CENG}
        dsems = {}
        for dkey in self.dcount:
            dsems[dkey] = self.stack.enter_context(nc.semaphore("dsem%d" % len(dsems)))
        block = self.stack.enter_context(nc.Block())

        def run(engname, eng):
            for waits, fn, info in self.ops[engname]:
                for kind, k, idx in waits:
                    if kind == "c":
                        eng.wait_ge(sems[k], rank[k][idx])
                    else:
                        eng.wait_ge(dsems[k], 16 * idx)
                if fn is None:
                    continue
                ins = fn(eng)
                if info[0] == "c":
                    if info[1] in rank[engname]:
                        ins.then_inc(sems[engname], 1)
                else:
                    ins.then_inc(dsems[info[1]], 16)

        @block.tensor
        def _(e):
            run("pe", e)

        @block.scalar
        def _(e):
            run("act", e)

        @block.vector
        def _(e):
            run("dve", e)

        @block.gpsimd
        def _(e):
            run("pool", e)

        @block.sync
        def _(e):
            run("sp", e)


D = 1024
S = 2048
TT = 512
NSUB = 4
D_IN = 10264
C_A = 0
C_BQKV = 512
C_BBETA = 2048
C_BGATE = 2056
C_CQKV = 2568
C_DQKV = 4104
C_DLR = 5640
C_DGATE = 5656
C_MERGE = 6168
ALPHA = 8.0 ** 0.25
LN_EPS = 1e-5
NORM_EPS = 1e-6

PP_CAW = 0
PP_CAB = 16
PP_RBA = 20
PP_RBX = 24
PP_LAM = 28
PP_GCW = 32
PP_GNW = 80
PP_LNW = 81
PP_DTB = 82
PP_ALOG = 86
PP_RB = 90
PP_GLB = 106
PP_LN1G = 618
PP_LN1B = 1642
PP_LN2G = 2666
PP_LN2B = 3690
PP_ONE = 4714
PP_EPS = 4715
PP_LNEPS = 4716
PP_N = 4717

K_ID = 0
K_LE = 128
K_GE = 256
K_GT = 384
K_ONE = 512
K_LE16 = 640
K_GT16 = 768
K_LT = 896
K_SBM = 1024
K_N = 1024 + 2048


def host_consts():
    p = np.arange(128)[:, None]
    f = np.arange(128)[None, :]
    c = np.zeros((128, K_N), np.float32)
    c[:, K_ID:K_ID + 128] = (p == f)
    c[:, K_LE:K_LE + 128] = (p <= f)
    c[:, K_GE:K_GE + 128] = (p >= f)
    c[:, K_GT:K_GT + 128] = (p > f)
    c[:, K_ONE:K_ONE + 128] = 1.0
    c[:, K_LE16:K_LE16 + 128] = (p <= f) * (-1.0 / 16.0)
    c[:, K_GT16:K_GT16 + 128] = (p > f) * (-1.0 / 16.0)
    c[:, K_LT:K_LT + 128] = (p < f)
    f5 = np.arange(512)[None, :]
    for jo in range(4):
        c[:, K_SBM + jo * 512:K_SBM + (jo + 1) * 512] = (jo * 128 + p < f5)
    return c


def build(nc, L=4, NSEQ=2, taps=(), en="ABCD", moe=True):
    NTOK = NSEQ * S
    st = ExitStack()
    P = Prog(nc, st)
    dr = lambda name, shape, kind="ExternalInput": nc.dram_tensor(name, list(shape), F32, kind=kind).ap()
    x_d = dr("x", [NTOK, D])
    w_in_d = dr("w_in", [4, D, D_IN])
    w_br_d = dr("w_branch", [4, 4, 512, D])
    w_out_d = dr("w_out", [4, D, D])
    w_g_d = dr("w_gate", [4, 16, D, 512])
    w_u_d = dr("w_up", [4, 16, D, 512])
    w_d_d = dr("w_down", [4, 16, 512, D])
    w_r_d = dr("w_router", [D, 16])
    pp_d = dr("pp", [4, 128, PP_N])
    bd_d = dr("bd", [4, 2, 4, 128, 128])
    gup_d = dr("gla_up", [4, 16, 512])
    k_d = dr("consts", [128, K_N])
    out_d = dr("out", [NTOK, D], "ExternalOutput")
    xs_d = dr("xs_scr", [2, NTOK, D], "Internal")
    tap_d = {}
    for name, shape in taps:
        tap_d[name] = dr("tap_" + name, shape, "ExternalOutput")
    dres = {}

    def DR(key):
        if key not in dres:
            dres[key] = Res("dram:" + str(key))
        return dres[key]

    out_toks = []

    kc = P.sb("kc", [128, 1024])
    kb = P.sb("kb", [128, 2048], BF16)
    pp = P.sb("pp_sb", [128, PP_N])
    NB = 4
    wring = [P.sb("wr%d" % i, [128, 2048]) for i in range(NB)]
    wri = [0]
    bdw = P.sb("bdw", [128, 8, 128])
    gup = P.sb("gup", [16, 512])
    wrt = P.sb("wrt", [128, 8, 16])
    lam8 = P.sb("lam8", [128, 4])
    aexp = P.sb("aexp", [128, 4])
    nge = P.sb("nge", [128, 128], BF16)
    none_ = P.sb("none", [128, 128], BF16)
    ARENA = 36000 - 12288 - 2048
    arena = P.sb("arena", [128, ARENA])
    arenaR = P.sb("arenaR", [128, 12288])
    xst = [P.sb("xst0", [128, 1024]), P.sb("xst1", [128, 1024])]
    banks = [P.ps("bank%d" % i, [128, 512]) for i in range(8)]

    aoff = [0]

    live = {"a": [], "r": []}

    def inherit(which, start, end, t_new):
        keep = []
        for (s0, e0, t_old) in live[which]:
            if s0 < end and start < e0:
                toks = list(t_old.res.rs.values())
                if t_old.res.w is not None:
                    toks.append(t_old.res.w)
                for tk in toks:
                    cur = t_new.res.rs.get(tk.eng)
                    if cur is None or cur.idx < tk.idx:
                        t_new.res.rs[tk.eng] = tk
            else:
                keep.append((s0, e0, t_old))
        keep.append((start, end, t_new))
        live[which] = keep

    def carve(name, ncols, shape=None, dt=F32):
        DBG[name] = (aoff[0], ncols)
        ap = arena[:, aoff[0]:aoff[0] + ncols]
        start = aoff[0]
        aoff[0] += ncols
        assert aoff[0] <= ARENA, (name, aoff[0])
        if dt == BF16:
            ap = ap.bitcast(BF16)
        if shape is not None and len(shape) == 3:
            ap = ap.rearrange("p (a b) -> p a b", a=shape[1])
        t_new = T(ap, name)
        inherit("a", start, start + ncols, t_new)
        return t_new

    roff = [0]

    def carveR(name, ncols, shape=None):
        ap = arenaR[:, roff[0]:roff[0] + ncols]
        roff[0] += ncols
        assert roff[0] <= 12288
        if shape is not None and len(shape) == 3:
            ap = ap.rearrange("p (a b) -> p a b", a=shape[1])
        t_new = T(ap, name)
        inherit("r", roff[0] - ncols, roff[0], t_new)
        return t_new

    def r32(ap):
        return ap.bitcast(F32R)

    def v3(ap, a):
        return ap.rearrange("p (a b) -> p a b", a=a)

    def mm(out_t, out_ap, l_t, l_ap, r_t, r_ap, start, stop):
        P.op("pe", lambda e: e.matmul(out_ap, l_ap, r_ap, start=start, stop=stop), reads=[l_t, r_t], writes=[out_t])

    def tr(out_t, out_ap, in_t, in_ap):
        P.op("pe", lambda e: e.transpose(out_ap, in_ap, kc[:, K_ID:K_ID + 128]), reads=[in_t, kc], writes=[out_t])

    def act(out_t, out_ap, in_t, in_ap, func, bias=None, scale=None, extra=()):
        kw = {}
        if bias is not None:
            kw["bias"] = bias
        if scale is not None:
            kw["scale"] = scale
        P.op("act", lambda e: e.activation(out_ap, in_ap, func, **kw), reads=[in_t] + list(extra), writes=[out_t])

    def ts(eng, out_t, out_ap, in_t, in_ap, s1, s2, op0, op1=None, extra=()):
        if op1 is None:
            P.op(eng, lambda e: e.tensor_scalar(out_ap, in_ap, s1, None, op0), reads=[in_t] + list(extra), writes=[out_t])
        else:
            P.op(eng, lambda e: e.tensor_scalar(out_ap, in_ap, s1, s2, op0, op1), reads=[in_t] + list(extra), writes=[out_t])

    def tt(eng, out_t, out_ap, a_t, a_ap, b_t, b_ap, op):
        P.op(eng, lambda e: e.tensor_tensor(out_ap, a_ap, b_ap, op), reads=[a_t, b_t], writes=[out_t])

    def stt(out_t, out_ap, a_t, a_ap, sc, b_t, b_ap, op0, op1, extra=()):
        P.op("dve", lambda e: e.scalar_tensor_tensor(out_ap, a_ap, sc, b_ap, op0, op1),
             reads=[a_t, b_t] + list(extra), writes=[out_t])

    def cp(eng, out_t, out_ap, in_t, in_ap):
        if eng == "act":
            P.op("act", lambda e: e.copy(out_ap, in_ap), reads=[in_t], writes=[out_t])
        else:
            P.op(eng, lambda e: e.tensor_copy(out_ap, in_ap), reads=[in_t], writes=[out_t])

    def memset(eng, out_t, out_ap, val):
        P.op(eng, lambda e: e.memset(out_ap, val), writes=[out_t])

    def wload(src_ap):
        buf = wring[wri[0] % NB]
        wri[0] += 1
        n = 1
        for s_ in src_ap.shape[1:]:
            n *= s_
        dst = buf[:, 0:n]
        if len(src_ap.shape) == 3:
            dst = dst.rearrange("p (a b) -> p a b", a=src_ap.shape[1])
        P.dma("sp", dst.bitcast(F32R), src_ap.bitcast(F32R), writes=[buf], key=("wr", buf.name))
        return buf, dst

    def tap(name, src_t, src_ap, dst_ap_fn):
        if name in tap_d:
            tk = P.dma("pool", dst_ap_fn(tap_d[name]), src_ap, reads=[src_t], key=("tap", name))
            out_toks.append(tk)

    def barrier():
        pass

    P.dma("sp", kc[:], k_d[:, 0:1024], writes=[kc], key="kc")
    P.dma("sp", arena[:, 0:2048], k_d[:, 1024:3072], writes=[arena], key="kcm")
    cp("dve", kb, kb[:], arena, arena[:, 0:2048])
    P.barrier()
    P.dma("sp", wrt[:], w_r_d.rearrange("(k p) e -> p k e", p=128), writes=[wrt], key="wrt")
    ts("dve", nge, nge[:], kc, kc[:, K_GE:K_GE + 128], -1.0, None, ALU.mult)
    ts("dve", none_, none_[:], kc, kc[:, K_ONE:K_ONE + 128], -1.0, None, ALU.mult)
    ident = kc[:, K_ID:K_ID + 128]
    ones = kc[:, K_ONE:K_ONE + 128]

    def win(l, c0, n):
        return wload(w_in_d[l].rearrange("(k p) n -> p k n", p=128)[:, :, c0:c0 + n])

    for l in range(L):
        barrier()
        P.dma("sp", pp[:], pp_d[l], writes=[pp], key="pp")
        P.dma("sp", bdw[:], bd_d[l].rearrange("a c p j -> p (a c) j"), writes=[bdw], key="bdw")
        P.dma("sp", gup[:], gup_d[l], writes=[gup], key="gup")
        act(lam8, lam8[:], pp, pp[:, PP_LAM:PP_LAM + 4], AF.Exp, scale=-1.0)
        act(lam8, lam8[:], lam8, lam8[:], AF.Ln, bias=pp[:, PP_ONE:PP_ONE + 1], extra=[pp])
        ts("dve", lam8, lam8[:], lam8, lam8[:], -8.0, None, ALU.mult)
        act(aexp, aexp[:], pp, pp[:, PP_ALOG:PP_ALOG + 4], AF.Exp)
        ts("dve", aexp, aexp[:], aexp, aexp[:], -1.0, None, ALU.mult)

        for s in range(NSEQ):
            barrier()
            aoff[0] = 0
            roff[0] = 0
            xT = carveR("xT", 4096, [128, 8, 512])
            merged = carveR("merged", 4096, [128, 8, 512])
            yn = carveR("yn", 2048, [128, 4, 512])
            KT = carve("KT", 4096, [128, 4, 2048], BF16)
            Vc = carve("Vc", 4096, [128, 16, 512], BF16)
            utail = carve("utail", 64, [128, 16, 4])
            hst = carve("hst", 4)
            S_d = [carve("S_d%d" % i_, 128) for i_ in range(4)]
            S_b = [carve("S_b%d" % i_, 128) for i_ in range(4)]
            S_db = [carve("S_db%d" % i_, 64, None, BF16) for i_ in range(4)]
            gsig = carve("gsig", 512)
            tmpm = carve("tmpm", 512)
            OV = aoff[0]
            memset("pool", utail, utail[:], 0.0)
            memset("pool", hst, hst[:], 0.0)
            for i_ in range(4):
                memset("pool", S_d[i_], S_d[i_][:], 0.0)
                memset("pool", S_db[i_], S_db[i_][:], 0.0)
            for i_ in range(4):
                memset("pool", S_b[i_], S_b[i_][:], 0.0)

            for c in range(NSUB):
                row0 = s * S + c * TT

                def proj_fm(bk, wb, wap, j0, ncols):
                    for k in range(8):
                        mm(bk, bk[0:ncols, :], wb, r32(wap[:, k, j0:j0 + ncols]), xT, r32(xT[:, k, :]), k == 0, k == 7)

                def proj_tm(bk, sub, wb, wap, j0, ncols):
                    for k in range(8):
                        mm(bk, bk[:, 0:ncols], xT, r32(xT[:, k, sub * 128:(sub + 1) * 128]), wb, r32(wap[:, k, j0:j0 + ncols]),
                           k == 0, k == 7)

                def branch_merge(n, first):
                    for dq in range(4):
                        wbb, wbap = wload(w_br_d[l, n].rearrange("(k p) n -> p k n", p=128)[:, :, dq * 256:(dq + 1) * 256])
                        wgb, wgap = win(l, C_MERGE + n * 1024 + dq * 256, 256)
                        for h_ in range(2):
                            dch = dq * 2 + h_
                            bp = banks[4 + (dch % 2)]
                            bg = banks[6 + (dch % 2)]
                            for k in range(4):
                                mm(bp, bp[:, :], wbb, r32(wbap[:, k, h_ * 128:(h_ + 1) * 128]), yn, r32(yn[:, k, :]), k == 0, k == 3)
                            proj_fm(bg, wgb, wgap, h_ * 128, 128)
                            act(gsig, gsig[:], bg, bg[:], AF.Sigmoid)
                            if first:
                                tt("dve", merged, r32(merged[:, dch, :]), bp, bp[:], gsig, gsig[:], ALU.mult)
                            else:
                                tt("dve", tmpm, tmpm[:], bp, bp[:], gsig, gsig[:], ALU.mult)
                                tt("pool", merged, r32(merged[:, dch, :]), merged, merged[:, dch, :], tmpm, tmpm[:], ALU.add)

                def gated_norm(o_t, o_ap, h, gate_c0, nw_col, t1, t2, t3, bs=None, bg=None):
                    act(t1, t1[:], o_t, o_ap, AF.Square)
                    bs = banks[6] if bs is None else bs
                    mm(bs, bs[:], kc, ones, t1, t1[:], True, True)
                    act(t2, t2[:], bs, bs[:], AF.Ln, bias=pp[:, PP_EPS:PP_EPS + 1], scale=1.0 / 128.0, extra=[pp])
                    act(t2, t2[:], t2, t2[:], AF.Exp, scale=-0.5)
                    wgb, wgap = win(l, gate_c0 + h * 128, 128)
                    bg = banks[7] if bg is None else bg
                    proj_fm(bg, wgb, wgap, 0, 128)
                    act(t3, t3[:], bg, bg[:], AF.Silu)
                    stt(t1, t1[:], o_t, o_ap, pp[:, nw_col:nw_col + 1], t2, t2[:], ALU.mult, ALU.mult, extra=[pp])
                    tt("dve", yn, r32(yn[:, h, :]), t1, t1[:], t3, t3[:], ALU.mult)

                barrier()
                for a in range(4):
                    xs_ = xst[a % 2]
                    r0 = row0 + a * 128
                    if l == 0:
                        P.dma("sp", r32(xs_[:]), r32(x_d[r0:r0 + 128, :]), writes=[xs_], key=("xst", a % 2))
                    else:
                        P.dma("sp", r32(xs_[:]), r32(xs_d[0, r0:r0 + 128, :]), reads=[DR(("x0", s, c))], writes=[xs_], key=("xst", a % 2))
                    for hf in range(2):
                        bk = banks[hf]
                        for k in range(4):
                            kk = hf * 4 + k
                            tr(bk, bk[:, k * 128:(k + 1) * 128], xs_, xs_[:, kk * 128:(kk + 1) * 128])
                        cp("act" if hf else "dve", xT, r32(xT[:, hf * 4:hf * 4 + 4, a * 128:(a + 1) * 128]), bk, v3(bk[:], 4))

                first = True
                if "A" in en:
                    barrier()
                    aoff[0] = OV
                    ubuf = carve("ubuf", 516)
                    t1 = carve("t1", 512)
                    t2 = carve("t2", 512)
                    t3 = carve("t3", 512)
                    t4 = carve("t4", 512)
                    wa = [win(l, C_A, 256), win(l, C_A + 256, 256)]
                    for ch in range(4):
                        bk = banks[ch % 2]
                        proj_fm(bk, wa[ch // 2][0], wa[ch // 2][1], (ch % 2) * 128, 128)
                        cp("dve", ubuf, ubuf[:, 0:3], utail, utail[:, ch, 0:3])
                        cp("act", ubuf, ubuf[:, 3:515], bk, bk[:])
                        cp("pool", utail, utail[:, ch, 0:3], ubuf, ubuf[:, 512:515])
                        cw = lambda k: pp[:, PP_CAW + ch * 4 + k:PP_CAW + ch * 4 + k + 1]
                        ts("dve", t1, t1[:], ubuf, ubuf[:, 3:515], cw(3), pp[:, PP_CAB + ch:PP_CAB + ch + 1], ALU.mult, ALU.add, extra=[pp])
                        for k in range(3):
                            stt(t1, t1[:], ubuf, ubuf[:, k:k + 512], cw(k), t1, t1[:], ALU.mult, ALU.add, extra=[pp])
                        ba = banks[2]
                        bx = banks[3]
                        mm(ba, ba[:], bdw, bdw[:, ch, :], t1, t1[:], True, True)
                        mm(bx, bx[:], bdw, bdw[:, 4 + ch, :], t1, t1[:], True, True)
                        act(t2, t2[:], ba, ba[:], AF.Sigmoid, bias=pp[:, PP_RBA + ch:PP_RBA + ch + 1], extra=[pp])
                        act(t3, t3[:], bx, bx[:], AF.Sigmoid, bias=pp[:, PP_RBX + ch:PP_RBX + ch + 1], extra=[pp])
                        act(t2, t2[:], t2, t2[:], AF.Exp, scale=lam8[:, ch:ch + 1], extra=[lam8])
                        tt("dve", t4, t4[:], t2, t2[:], t2, t2[:], ALU.mult)
                        act(t4, t4[:], t4, t4[:], AF.Sqrt, bias=pp[:, PP_ONE:PP_ONE + 1], scale=-1.0, extra=[pp])
                        if c == 0:
                            memset("dve", t4, t4[:, 0:1], 1.0)
                        tt("dve", t3, t3[:], t3, t3[:], t1, t1[:], ALU.mult)
                        tt("dve", t3, t3[:], t3, t3[:], t4, t4[:], ALU.mult)
                        P.op("dve", lambda e, o_=r32(yn[:, ch, :]), a_=t2[:], b_=t3[:], i_=hst[:, ch:ch + 1]: e.tensor_tensor_scan(o_, a_, b_, i_, ALU.mult, ALU.add),
                             reads=[t2, t3, hst], writes=[yn])
                        cp("pool", hst, hst[:, ch:ch + 1], yn, yn[:, ch, 511:512])
                    tap("y_a", yn, yn[:], lambda d_: d_[:, :, c * TT:(c + 1) * TT])
                    branch_merge(0, first)
                    first = False

                if "B" in en:
                    barrier()
                    aoff[0] = OV
                    qnT = carve("qnT", 2048, [128, 4, 512])
                    knT = carve("knT", 2048, [128, 4, 512])
                    vT = carve("vT", 2048, [128, 4, 512])
                    ubuf = carve("ubuf", 516)
                    t1 = carve("t1", 512)
                    t2 = carve("t2", 512)
                    t3 = carve("t3", 512)
                    beta_t = carve("beta_t", 16)
                    g_t = carve("g_t", 16)
                    gcc = carve("gcc", 16)
                    bexp = carve("bexp", 16)
                    kdsc = carve("kdsc", 16)
                    gl = carve("gl", 16)
                    tsm = carve("tsm", 16)
                    dests = [qnT, knT, vT]
                    for un in range(6):
                        wq = win(l, C_BQKV + un * 256, 256)
                        for h_ in range(2):
                            ci = un * 2 + h_
                            bk = banks[ci % 2]
                            proj_fm(bk, wq[0], wq[1], h_ * 128, 128)
                            cp("dve", ubuf, ubuf[:, 0:3], utail, utail[:, 4 + ci, 0:3])
                            cp("act", ubuf, ubuf[:, 3:515], bk, bk[:])
                            cp("pool", utail, utail[:, 4 + ci, 0:3], ubuf, ubuf[:, 512:515])
                            cw = lambda k: pp[:, PP_GCW + ci * 4 + k:PP_GCW + ci * 4 + k + 1]
                            ts("dve", t1, t1[:], ubuf, ubuf[:, 3:515], cw(3), None, ALU.mult, extra=[pp])
                            for k in range(3):
                                stt(t1, t1[:], ubuf, ubuf[:, k:k + 512], cw(k), t1, t1[:], ALU.mult, ALU.add, extra=[pp])
                            dst = dests[ci // 4]
                            act(dst, dst[:, ci % 4, :], t1, t1[:], AF.Silu)
                    for qi, dst in enumerate((qnT, knT)):
                        for h in range(4):
                            act(t1, t1[:], dst, dst[:, h, :], AF.Square)
                            bs = banks[2 + h % 2]
                            mm(bs, bs[:], kc, ones, t1, t1[:], True, True)
                            act(t2, t2[:], bs, bs[:], AF.Ln, bias=pp[:, PP_EPS:PP_EPS + 1], extra=[pp])
                            act(t2, t2[:], t2, t2[:], AF.Exp, scale=-0.5)
                            if qi == 0:
                                stt(dst, dst[:, h, :], dst, dst[:, h, :], 128.0 ** -0.5, t2, t2[:], ALU.mult, ALU.mult)
                            else:
                                tt("dve", dst, dst[:, h, :], dst, dst[:, h, :], t2, t2[:], ALU.mult)
                    wbg = win(l, C_BBETA, 8)
                    bsm = banks[4]
                    for sub in range(4):
                        for k in range(8):
                            mm(bsm, bsm[:, sub * 8:(sub + 1) * 8], xT, r32(xT[:, k, sub * 128:(sub + 1) * 128]), wbg[0], r32(wbg[1][:, k, 0:8]), k == 0, k == 7)
                    bsm3 = v3(bsm[:, 0:32], 4)
                    b3 = lambda t_: v3(t_[:], 4)
                    act(beta_t, b3(beta_t), bsm, bsm3[:, :, 0:4], AF.Sigmoid)
                    cp("act", tsm, b3(tsm), bsm, bsm3[:, :, 4:8])
                    tt("dve", tsm, b3(tsm), tsm, b3(tsm), pp, pp[:, PP_DTB:PP_DTB + 4].unsqueeze(1).to_broadcast([128, 4, 4]), ALU.add)
                    act(tsm, tsm[:], tsm, tsm[:], AF.Exp)
                    act(tsm, tsm[:], tsm, tsm[:], AF.Ln, bias=pp[:, PP_ONE:PP_ONE + 1], extra=[pp])
                    tt("dve", g_t, b3(g_t), tsm, b3(tsm), aexp, aexp[:].unsqueeze(1).to_broadcast([128, 4, 4]), ALU.mult)
                    bsm2 = banks[5]
                    for sub in range(4):
                        mm(bsm2, bsm2[:, sub * 4:(sub + 1) * 4], kc, kc[:, K_LE:K_LE + 128], g_t, g_t[:, sub * 4:(sub + 1) * 4], True, True)
                    for sub in range(4):
                        mm(bsm2, bsm2[:, 16 + sub * 4:16 + (sub + 1) * 4], kc, ones, g_t, g_t[:, sub * 4:(sub + 1) * 4], True, True)
                    cp("act", gcc, gcc[:], bsm2, bsm2[:, 0:16])
                    act(bexp, bexp[:], bsm2, bsm2[:, 0:16], AF.Exp)
                    tt("dve", bexp, bexp[:], bexp, bexp[:], beta_t, beta_t[:], ALU.mult)
                    act(gl, gl[:], bsm2, bsm2[:, 16:32], AF.Exp)
                    cp("act", kdsc, kdsc[:], bsm2, bsm2[:, 16:32])
                    tt("dve", kdsc, kdsc[:], kdsc, kdsc[:], gcc, gcc[:], ALU.subtract)
                    act(kdsc, kdsc[:], kdsc, kdsc[:], AF.Exp)
                    TOPB = aoff[0]
                    names = ("tg", "tmp1", "tmp2", "qdec", "PT", "attn_s", "kbg", "kdec", "vb", "u_s", "wT_s", "vn_s")

                    def mk_tmp(tagc):
                        d = {n_: carve(n_ + tagc, 128) for n_ in names}
                        d["AAT"] = [carve("AAT0" + tagc, 256), carve("AAT1" + tagc, 256)]
                        return d

                    def chain(h, sub, tm, bA, bB, bC, bo):
                        cs = slice(sub * 128, (sub + 1) * 128)
                        si = sub * 4 + h
                        tg, tmp1, tmp2, qdec, PT, attn_s = tm["tg"], tm["tmp1"], tm["tmp2"], tm["qdec"], tm["PT"], tm["attn_s"]
                        kbg, kdec, vb, u_s, wT_s, vn_s, AAT = tm["kbg"], tm["kdec"], tm["vb"], tm["u_s"], tm["wT_s"], tm["vn_s"], tm["AAT"]
                        ts("dve", tg, tg[:], kc, kc[:, K_LE:K_LE + 128], g_t[:, si:si + 1], None, ALU.mult, extra=[g_t])
                        mm(bA, bA[:, 0:128], kc, ones, tg, tg[:], True, True)
                        mm(bA, bA[:, 128:256], knT, knT[:, h, cs], knT, knT[:, h, cs], True, True)
                        mm(bA, bA[:, 256:384], knT, knT[:, h, cs], qnT, qnT[:, h, cs], True, True)
                        mm(bA, bA[:, 384:512], knT, knT[:, h, cs], kc, ident, True, True)
                        mm(bB, bB[:, 0:128], vT, vT[:, h, cs], kc, ident, True, True)
                        yield
                        ts("dve", tmp1, tmp1[:], bA, bA[:, 0:128], gcc[:, si:si + 1], 0.0, ALU.subtract, ALU.min, extra=[gcc])
                        ts("dve", tmp2, tmp2[:], bA, bA[:, 0:128], gcc[:, si:si + 1], 0.0, ALU.subtract, ALU.max, extra=[gcc])
                        act(tmp1, tmp1[:], tmp1, tmp1[:], AF.Exp)
                        act(tmp2, tmp2[:], tmp2, tmp2[:], AF.Exp, scale=-1.0)
                        tt("pool", tmp1, tmp1[:], tmp1, tmp1[:], kc, kc[:, K_LE:K_LE + 128], ALU.mult)
                        tt("pool", tmp2, tmp2[:], tmp2, tmp2[:], kc, kc[:, K_GT:K_GT + 128], ALU.mult)
                        act(tg, tg[:], bA, bA[:, 0:128], AF.Exp)
                        yield
                        tt("dve", qdec, qdec[:], qnT, qnT[:, h, cs], tg, tg[:], ALU.mult)
                        stt(AAT[0], AAT[0][:, 0:128], bA, bA[:, 128:256], beta_t[:, si:si + 1], tmp2, tmp2[:], ALU.mult, ALU.mult, extra=[beta_t])
                        tt("dve", attn_s, attn_s[:], bA, bA[:, 256:384], tmp1, tmp1[:], ALU.mult)
                        ts("dve", kbg, kbg[:], bA, bA[:, 384:512], bexp[:, si:si + 1], None, ALU.mult, extra=[bexp])
                        ts("dve", kdec, kdec[:], bA, bA[:, 384:512], kdsc[:, si:si + 1], None, ALU.mult, extra=[kdsc])
                        ts("dve", vb, vb[:], bB, bB[:, 0:128], beta_t[:, si:si + 1], None, ALU.mult, extra=[beta_t])
                        mm(bB, bB[:, 128:256], AAT[0], AAT[0][:, 0:128], kc, ident, True, True)
                        yield
                        cp("act", AAT[0], AAT[0][:, 128:256], bB, bB[:, 128:256])
                        act(PT, PT[:], bB, bB[:, 128:256], AF.Copy, scale=-1.0)
                        tt("pool", PT, PT[:], PT, PT[:], kc, ident, ALU.add)
                        cur = 0
                        for m in range(1, 7):
                            A_c = AAT[cur]
                            A_n = AAT[1 - cur]
                            mm(bC, bC[:, 0:128], A_c, A_c[:, 128:256], A_c, A_c[:, 0:128], True, True)
                            if m < 6:
                                mm(bC, bC[:, 128:256], A_c, A_c[:, 0:128], A_c, A_c[:, 128:256], True, True)
                            yield
                            if m < 6:
                                cp("act", A_n, A_n[:, 0:256], bC, bC[:, 0:256])
                            else:
                                cp("act", A_n, A_n[:, 0:128], bC, bC[:, 0:128])
                            mm(bC, bC[:, 256:384], A_n, A_n[:, 0:128], PT, PT[:], True, True)
                            yield
                            tt("dve", PT, PT[:], PT, PT[:], bC, bC[:, 256:384], ALU.add)
                            cur = 1 - cur
                        mm(bB, bB[:, 256:384], PT, PT[:], vb, vb[:], True, True)
                        mm(bB, bB[:, 384:512], kbg, kbg[:], PT, PT[:], True, True)
                        yield
                        cp("act", u_s, u_s[:], bB, bB[:, 256:384])
                        cp("act", wT_s, wT_s[:], bB, bB[:, 384:512])
                        mm(bC, bC[:, 384:512], wT_s, wT_s[:], S_b[h], S_b[h][:], True, True)
                        yield
                        tt("dve", vn_s, vn_s[:], u_s, u_s[:], bC, bC[:, 384:512], ALU.subtract)
                        mm(bo, bo[:, cs], S_b[h], S_b[h][:], qdec, qdec[:], True, False)
                        mm(bo, bo[:, cs], vn_s, vn_s[:], attn_s, attn_s[:], False, True)
                        mm(bC, bC[:, 0:128], kdec, kdec[:], vn_s, vn_s[:], True, True)
                        yield
                        stt(S_b[h], S_b[h][:], S_b[h], S_b[h][:], gl[:, si:si + 1], bC, bC[:, 0:128], ALU.mult, ALU.add, extra=[gl])
                        yield

                    for hp in range(2):
                        aoff[0] = OV + 6144
                        tm1 = mk_tmp("_b")
                        aoff[0] = max(aoff[0], TOPB)
                        tm0 = mk_tmp("_a")
                        tms = (tm0, tm1)
                        for sub in range(4):
                            gens = []
                            for j in range(2):
                                h = 2 * hp + j
                                gens.append(chain(h, sub, tms[j], banks[4 * j], banks[4 * j + 1], banks[4 * j + 2], banks[4 * j + 3]))
                            alive = list(gens)
                            while alive:
                                nxt = []
                                for g_ in alive:
                                    try:
                                        next(g_)
                                        nxt.append(g_)
                                    except StopIteration:
                                        pass
                                alive = nxt
                        aoff[0] = OV + 6144
                        ubuf = carve("ubuf", 516)
                        t1 = carve("t1", 512)
                        t2 = carve("t2", 512)
                        t3 = carve("t3", 512)
                        for j in range(2):
                            h = 2 * hp + j
                            gated_norm(banks[4 * j + 3], banks[4 * j + 3][:], h, C_BGATE, PP_GNW, t1, t2, t3, bs=banks[4 * j], bg=banks[4 * j + 1])
                    tap("y_b", yn, yn[:], lambda d_: d_[:, :, c * TT:(c + 1) * TT])
                    branch_merge(1, first)
                    first = False

                if "C" in en:
                    barrier()
                    aoff[0] = OV
                    qTb = carve("qTb", 2048, [128, 8, 512], BF16)
                    memset("pool", qTb, qTb[:], 0.0)
                    e_sb = [carve("e_sb0", 512), carve("e_sb1", 512)]
                    sp_sb = [carve("sp_sb0", 256, None, BF16), carve("sp_sb1", 256, None, BF16)]
                    spsum = [carve("spsum0", 512), carve("spsum1", 512)]
                    spsum_b = [carve("spsum_b0", 256, None, BF16), carve("spsum_b1", 256, None, BF16)]
                    w_sb = [carve("w_sb0", 256, None, BF16), carve("w_sb1", 256, None, BF16)]
                    qb0 = c * 4
                    for u in range(2):
                        wq = win(l, C_CQKV + u * 256, 256)
                        wk = win(l, C_CQKV + 512 + u * 256, 256)
                        for h_ in range(2):
                            pr = u * 2 + h_
                            bq = banks[0 + h_]
                            proj_fm(bq, wq[0], wq[1], h_ * 128, 128)
                            ts("dve", qTb, qTb[0:64, 2 * pr, :], bq, bq[0:64, :], 0.125, None, ALU.mult)
                            ts("dve", qTb, qTb[64:128, 2 * pr + 1, :], bq, bq[64:128, :], 0.125, None, ALU.mult)
                            bk_ = banks[2 + h_]
                            proj_fm(bk_, wk[0], wk[1], h_ * 128, 128)
                            cp("act", KT, KT[:, pr, c * TT:(c + 1) * TT], bk_, bk_[:])
                    wv0 = win(l, C_CQKV + 1024, 256)
                    wv1 = win(l, C_CQKV + 1280, 256)
                    for sub in range(4):
                        bv = banks[sub % 2]
                        proj_tm(bv, sub, wv0[0], wv0[1], 0, 256)
                        cp("act", Vc, Vc[:, qb0 + sub, 0:256], bv, bv[:, 0:256])
                        bv2 = banks[2 + sub % 2]
                        proj_tm(bv2, sub, wv1[0], wv1[1], 0, 256)
                        cp("dve", Vc, Vc[:, qb0 + sub, 256:512], bv2, bv2[:, 0:256])
                    for pr in range(4):
                        nkb = qb0 + 4
                        for hh in range(2):
                            memset("pool", spsum[hh], spsum[hh][:], 0.0)
                        for step, J in enumerate(range(nkb - 1, -1, -1)):
                            for hh in range(2):
                                bo = banks[6 + hh]
                                h = pr * 2 + hh
                                e_h, sp_h, su_h, sub_h, w_h = e_sb[hh], sp_sb[hh], spsum[hh], spsum_b[hh], w_sb[hh]
                                jo = J - qb0
                                q0 = max(jo, 0) * 128
                                qs = slice(q0, 512)
                                dg = slice(q0, q0 + 128)
                                bz = banks[hh + 4 * (step % 2)]
                                bd_ = banks[2 + hh]
                                mm(bz, bz[:, qs], KT, KT[:, pr, J * 128:(J + 1) * 128], qTb, qTb[:, h, qs], True, True)
                                act(e_h, e_h[:, qs], bz, bz[:, qs], AF.Exp)
                                act(sp_h, sp_h[:, qs], e_h, e_h[:, qs], AF.Ln, bias=pp[:, PP_ONE:PP_ONE + 1], extra=[pp])
                                if jo >= 0:
                                    tt("pool", sp_h, sp_h[:, dg], sp_h, sp_h[:, dg], kb, kb[:, 0:128], ALU.mult)
                                mm(bd_, bd_[:, qs], KT, KT[:, pr, J * 128:(J + 1) * 128], qTb, qTb[:, h, qs], True, False)
                                mm(bd_, bd_[:, qs], nge, nge[:], sp_h, sp_h[:, qs], False, step == 0)
                                if step > 0:
                                    mm(bd_, bd_[:, qs], none_, none_[:], sub_h, sub_h[:, qs], False, True)
                                act(w_h, w_h[:, qs], bd_, bd_[:, qs], AF.Exp)
                                if jo >= 0:
                                    tt("pool", w_h, w_h[:, dg], w_h, w_h[:, dg], kb, kb[:, 0:128], ALU.mult)
                                mm(bo, bo[:, qs], Vc, Vc[:, J, pr * 128:(pr + 1) * 128], w_h, w_h[:, qs], step == 0, J == 0)
                                if J > 0:
                                    tt("dve", su_h, su_h[:, qs], su_h, su_h[:, qs], sp_h, sp_h[:, qs], ALU.add)
                                    cp("pool", sub_h, sub_h[:], su_h, su_h[:])
                        for hh in range(2):
                            bo = banks[6 + hh]
                            ps_ = slice(hh * 64, hh * 64 + 64)
                            cp("act", yn, r32(yn[ps_, pr, :]), bo, bo[ps_, :])
                    tap("y_c", yn, yn[:], lambda d_: d_[:, :, c * TT:(c + 1) * TT])
                    branch_merge(2, first)
                    first = False

                if "D" in en:
                    barrier()
                    aoff[0] = OV
                    t1 = carve("t1", 512)
                    t2 = carve("t2", 512)
                    t3 = carve("t3", 512)
                    sp_tok = carve("sp_tok", 2048, [128, 4, 512])
                    qd = carve("qd", 1024, [128, 4, 512], BF16)
                    ki = carve("ki", 1024, [128, 4, 512], BF16)
                    kdt = carve("kdt", 1024, [128, 4, 512], BF16)
                    vtk = carve("vtk", 1024, [128, 4, 512], BF16)
                    lrT = carve("lrT", 512)
                    glast = carve("glast", 16)
                    attn = [carve("attn%d" % i_, 64, None, BF16) for i_ in range(4)]
                    wl = win(l, C_DLR, 16)
                    b0 = banks[0]
                    proj_fm(b0, wl[0], wl[1], 0, 16)
                    cp("act", lrT, lrT[0:16, :], b0, b0[0:16, :])
                    for sub in range(4):
                        bg_ = banks[1 + sub % 2]
                        mm(bg_, bg_[:], lrT, lrT[0:16, sub * 128:(sub + 1) * 128], gup, gup[:], True, True)
                        tt("dve", t1, t1[:], bg_, bg_[:], pp, pp[:, PP_GLB:PP_GLB + 512], ALU.add)
                        act(t1, t1[:], t1, t1[:], AF.Exp, scale=-1.0)
                        act(sp_tok, sp_tok[:, sub, :], t1, t1[:], AF.Ln, bias=pp[:, PP_ONE:PP_ONE + 1], extra=[pp])
                    wk0 = win(l, C_DQKV + 512, 256)
                    wk1 = win(l, C_DQKV + 768, 256)
                    for sub in range(4):
                        br = banks[3]
                        mm(br, br[:], kc, kc[:, K_GT16:K_GT16 + 128], sp_tok, sp_tok[:, sub, :], True, True)
                        act(t2, t2[:], br, br[:], AF.Exp)
                        for hf, wk_ in enumerate((wk0, wk1)):
                            bkk = banks[4 + hf]
                            proj_tm(bkk, sub, wk_[0], wk_[1], 0, 256)
                            tt("dve", kdt, kdt[:, sub, hf * 256:(hf + 1) * 256], bkk, bkk[:, 0:256], t2, t2[:, hf * 256:(hf + 1) * 256], ALU.mult)
                    wv0 = win(l, C_DQKV + 1024, 256)
                    wv1 = win(l, C_DQKV + 1280, 256)
                    for sub in range(4):
                        for hf, wvv in enumerate((wv0, wv1)):
                            bvv = banks[6 + hf]
                            proj_tm(bvv, sub, wvv[0], wvv[1], 0, 256)
                            cp("act", vtk, vtk[:, sub, hf * 256:(hf + 1) * 256], bvv, bvv[:, 0:256])
                    for h in range(4):
                        bb = banks[0]
                        for sub in range(4):
                            mm(bb, bb[:, sub * 128:(sub + 1) * 128], sp_tok, sp_tok[:, sub, h * 128:(h + 1) * 128],
                               kc, kc[:, K_LE16:K_LE16 + 128], True, True)
                        act(t1, t1[:], bb, bb[:], AF.Exp)
                        act(t2, t2[:], bb, bb[:], AF.Exp, scale=-1.0)
                        for sub in range(4):
                            cp("pool", glast, glast[:, h * 4 + sub:h * 4 + sub + 1], t1, t1[:, sub * 128 + 127:sub * 128 + 128])
                        wq = win(l, C_DQKV + h * 128, 128)
                        bq = banks[1]
                        proj_fm(bq, wq[0], wq[1], 0, 128)
                        stt(qd, qd[:, h, :], bq, bq[:], 128.0 ** -0.5, t1, t1[:], ALU.mult, ALU.mult)
                        wkf = win(l, C_DQKV + 512 + h * 128, 128)
                        bk2 = banks[2]
                        proj_fm(bk2, wkf[0], wkf[1], 0, 128)
                        tt("dve", ki, ki[:, h, :], bk2, bk2[:], t2, t2[:], ALU.mult)
                    for sub in range(4):
                        for h in range(4):
                            bo = banks[h]
                            at_h = attn[h]
                            cs = slice(sub * 128, (sub + 1) * 128)
                            hs = slice(h * 128, (h + 1) * 128)
                            ba_ = banks[4 + h % 2]
                            mm(ba_, ba_[:, 0:128], ki, ki[:, h, cs], qd, qd[:, h, cs], True, True)
                            tt("dve", at_h, at_h[:], ba_, ba_[:, 0:128], kc, kc[:, K_LE:K_LE + 128], ALU.mult)
                            mm(bo, bo[:, cs], vtk, vtk[:, sub, hs], at_h, at_h[:], True, False)
                            mm(bo, bo[:, cs], S_db[h], S_db[h][:], qd, qd[:, h, cs], False, True)
                            bs_ = banks[6 + h % 2]
                            mm(bs_, bs_[:, 0:128], kdt, kdt[:, sub, hs], vtk, vtk[:, sub, hs], True, True)
                            stt(S_d[h], S_d[h][:], S_d[h], S_d[h][:], glast[:, h * 4 + sub:h * 4 + sub + 1], bs_, bs_[:, 0:128],
                                ALU.mult, ALU.add, extra=[glast])
                            cp("pool", S_db[h], S_db[h][:], S_d[h], S_d[h][:])
                    for h in range(4):
                        gated_norm(banks[h], banks[h][:], h, C_DGATE, PP_LNW, t1, t2, t3)
                    tap("y_d", yn, yn[:], lambda d_: d_[:, :, c * TT:(c + 1) * TT])
                    branch_merge(3, first)
                    first = False

                barrier()
                aoff[0] = OV
                x_tok = carve("x_tok", 4096, [128, 4, 1024])
                lnt = carve("lnt", 64)
                src = (x_d if l == 0 else xs_d[0])[row0:row0 + TT, :]
                P.dma("pool", x_tok[:], src.rearrange("(a p) d -> p a d", p=128),
                      reads=([DR(("x0", s, c))] if l > 0 else []), writes=[x_tok], key="xtok")
                for dq in range(4):
                    wo = wload(w_out_d[l].rearrange("(k p) n -> p k n", p=128)[:, :, dq * 256:(dq + 1) * 256])
                    for sub in range(4):
                        bo = banks[sub % 4]
                        for k in range(8):
                            mm(bo, bo[:, 0:256], merged, r32(merged[:, k, sub * 128:(sub + 1) * 128]), wo[0], r32(wo[1][:, k, :]), k == 0, k == 7)
                        stt(x_tok, x_tok[:, sub, dq * 256:(dq + 1) * 256], x_tok, x_tok[:, sub, dq * 256:(dq + 1) * 256], ALPHA,
                            bo, bo[:, 0:256], ALU.mult, ALU.add)
                for sub in range(4):
                    layer_norm(P, x_tok, x_tok[:, sub, :], lnt, pp, PP_LN1G, PP_LN1B)
                tap("x1", x_tok, x_tok[:], lambda d_: d_[c * TT:(c + 1) * TT, :].rearrange("(a p) d -> p a d", p=128))
                dst_d = xs_d[1] if moe else (out_d if l == L - 1 else xs_d[0])
                tk = P.dma("pool", dst_d[row0:row0 + TT, :].rearrange("(a p) d -> p a d", p=128), x_tok[:],
                           reads=[x_tok], writes=[DR((("x1" if moe else "x0"), s, c))], key="xst_out")
                if not moe and l == L - 1:
                    out_toks.append(tk)

            if not moe:
                continue
            for g in range(2):
                barrier()
                aoff[0] = 0
                roff[0] = 0
                x1T = carveR("x1T", 8192, [128, 8, 1024])
                hT = [carveR("hT0", 2048, [128, 2, 1024]), carveR("hT1", 2048, [128, 2, 1024])]
                yacc = carve("yacc", 8192, [128, 8, 1024])
                sg = [carve("sg0", 512), carve("sg1", 512)]
                sc = carve("sc", 128, [128, 8, 16])
                bi = carve("bi", 128, [128, 8, 16])
                mb = carve("mb", 128, [128, 8, 16])
                eq = carve("eq", 128, [128, 8, 16])
                sel = carve("sel", 128, [128, 8, 16])
                comb = carve("comb", 128, [128, 8, 16])
                m1 = carve("m1", 32)
                m2 = carve("m2", 32)
                gsel = carve("gsel", 32)
                gm = carve("gm", 8)
                lnt = carve("lnt", 64)
                grow0 = s * S + g * 1024
                P.dma("pool", yacc[:], xs_d[1, grow0:grow0 + 1024, :].rearrange("(a p) d -> p a d", p=128),
                      reads=[DR(("x1", s, 2 * g)), DR(("x1", s, 2 * g + 1))], writes=[yacc], key="yacc")
                for a in range(8):
                    xs_ = xst[a % 2]
                    P.dma("sp", r32(xs_[:]), r32(xs_d[1, grow0 + a * 128:grow0 + (a + 1) * 128, :]),
                          reads=[DR(("x1", s, 2 * g)), DR(("x1", s, 2 * g + 1))], writes=[xs_], key=("xst", a % 2))
                    for hf in range(2):
                        bk = banks[hf]
                        for k in range(4):
                            kk = hf * 4 + k
                            tr(bk, bk[:, k * 128:(k + 1) * 128], xs_, xs_[:, kk * 128:(kk + 1) * 128])
                        cp("act" if hf else "dve", x1T, r32(x1T[:, hf * 4:hf * 4 + 4, a * 128:(a + 1) * 128]), bk, v3(bk[:], 4))
                brt = banks[2]
                for a in range(8):
                    for k in range(8):
                        mm(brt, brt[:, a * 16:(a + 1) * 16], x1T, x1T[:, k, a * 128:(a + 1) * 128], wrt, wrt[:, k, :], k == 0, k == 7)
                act(sc, sc[:], brt, v3(brt[:, 0:128], 8), AF.Sigmoid)
                rb_b = pp[:, PP_RB:PP_RB + 16].unsqueeze(1).to_broadcast([128, 8, 16])
                tt("dve", bi, bi[:], sc, sc[:], pp, rb_b, ALU.add)
                bi4 = bi[:].rearrange("p a (g e) -> p (a g) e", g=4)
                mb4 = mb[:].rearrange("p a (g e) -> p (a g) e", g=4)
                eq4 = eq[:].rearrange("p a (g e) -> p (a g) e", g=4)
                P.op("dve", lambda e, o_=m1[:], i_=bi4: e.tensor_reduce(o_, i_, AX.X, ALU.max), reads=[bi], writes=[m1])
                tt("dve", eq, eq4, bi, bi4, m1, m1[:].unsqueeze(2).to_broadcast([128, 32, 4]), ALU.is_equal)
                stt(mb, mb4, eq, eq4, -1e30, bi, bi4, ALU.mult, ALU.add)
                P.op("dve", lambda e, o_=m2[:], i_=mb4: e.tensor_reduce(o_, i_, AX.X, ALU.max), reads=[mb], writes=[m2])
                tt("dve", m1, m1[:], m1, m1[:], m2, m2[:], ALU.add)
                m1g = m1[:].rearrange("p (a g) -> p a g", g=4)
                P.op("dve", lambda e, o_=gm[:], i_=m1g: e.tensor_reduce(o_, i_, AX.X, ALU.max), reads=[m1], writes=[gm])
                tt("dve", gsel, gsel[:].rearrange("p (a g) -> p a g", g=4), m1, m1g, gm, gm[:].unsqueeze(2).to_broadcast([128, 8, 4]), ALU.is_equal)
                ts("dve", gsel, gsel[:], gsel, gsel[:], -1.0, 1e30, ALU.add, ALU.mult)
                tt("dve", mb, mb4, bi, bi4, gsel, gsel[:].unsqueeze(2).to_broadcast([128, 32, 4]), ALU.add)
                P.op("dve", lambda e, o_=gm[:], i_=mb[:]: e.tensor_reduce(o_, i_, AX.X, ALU.max), reads=[mb], writes=[gm])
                tt("dve", sel, sel[:], mb, mb[:], gm, gm[:].unsqueeze(2).to_broadcast([128, 8, 16]), ALU.is_equal)
                stt(mb, mb[:], sel, sel[:], -1e30, mb, mb[:], ALU.mult, ALU.add)
                P.op("dve", lambda e, o_=gm[:], i_=mb[:]: e.tensor_reduce(o_, i_, AX.X, ALU.max), reads=[mb], writes=[gm])
                tt("dve", eq, eq[:], mb, mb[:], gm, gm[:].unsqueeze(2).to_broadcast([128, 8, 16]), ALU.is_equal)
                tt("dve", sel, sel[:], sel, sel[:], eq, eq[:], ALU.add)
                tt("dve", comb, comb[:], sel, sel[:], sc, sc[:], ALU.mult)
                P.op("dve", lambda e, o_=gm[:], i_=comb[:]: e.tensor_reduce(o_, i_, AX.X, ALU.add), reads=[comb], writes=[gm])
                P.op("dve", lambda e, o_=gm[:]: e.reciprocal(o_, o_), reads=[gm], writes=[gm])
                tt("dve", comb, comb[:], comb, comb[:], gm, gm[:].unsqueeze(2).to_broadcast([128, 8, 16]), ALU.mult)
                tap("comb", comb, comb[:], lambda d_: d_[g])
                for a in range(8):
                    ts("pool", yacc, yacc[:, a, :], yacc, yacc[:, a, :], ALPHA, None, ALU.mult)
                for ex in range(16):
                    for fh in range(2):
                        u_i = ex * 2 + fh
                        wg = wload(w_g_d[l, ex].rearrange("(k p) f -> p k f", p=128)[:, :, fh * 256:(fh + 1) * 256])
                        wu = wload(w_u_d[l, ex].rearrange("(k p) f -> p k f", p=128)[:, :, fh * 256:(fh + 1) * 256])
                        wd = wload(w_d_d[l, ex, fh * 256:(fh + 1) * 256, :].rearrange("(a p) d -> p a d", p=128))
                        hb = hT[u_i % 2]
                        for fc in range(2):
                            for th in range(2):
                                bg_ = banks[(fc * 2 + th) % 2]
                                bu_ = banks[2 + (fc * 2 + th) % 2]
                                for k in range(8):
                                    mm(bg_, bg_[:], wg[0], r32(wg[1][:, k, fc * 128:(fc + 1) * 128]), x1T, r32(x1T[:, k, th * 512:(th + 1) * 512]), k == 0, k == 7)
                                for k in range(8):
                                    mm(bu_, bu_[:], wu[0], r32(wu[1][:, k, fc * 128:(fc + 1) * 128]), x1T, r32(x1T[:, k, th * 512:(th + 1) * 512]), k == 0, k == 7)
                                sgt = sg[(fc * 2 + th) % 2]
                                act(sgt, sgt[:], bg_, bg_[:], AF.Silu)
                                tt("dve", hb, r32(hb[:, fc, th * 512:(th + 1) * 512]), bu_, bu_[:], sgt, sgt[:], ALU.mult)
                        for a in range(8):
                            for dh in range(2):
                                by = banks[4 + (a * 2 + dh) % 4]
                                for fc in range(2):
                                    mm(by, by[:], hb, r32(hb[:, fc, a * 128:(a + 1) * 128]), wd[0], r32(wd[1][:, fc, dh * 512:(dh + 1) * 512]), fc == 0, fc == 1)
                                stt(yacc, yacc[:, a, dh * 512:(dh + 1) * 512], by, by[:], comb[:, a, ex:ex + 1],
                                    yacc, yacc[:, a, dh * 512:(dh + 1) * 512], ALU.mult, ALU.add, extra=[comb])
                for a in range(8):
                    layer_norm(P, yacc, yacc[:, a, :], lnt, pp, PP_LN2G, PP_LN2B)
                last = (l == L - 1)
                dst_d = out_d if last else xs_d[0]
                tk = P.dma("pool", dst_d[grow0:grow0 + 1024, :].rearrange("(a p) d -> p a d", p=128), yacc[:],
                           reads=[yacc], writes=[DR(("x0", s, 2 * g)), DR(("x0", s, 2 * g + 1))], key="yst_out")
                if last:
                    out_toks.append(tk)
    P.final_wait("sp", out_toks)
    P.emit()
    return st


def layer_norm(P, x_t, x_ap, lnt, pp, gcol, bcol):
    stats = lnt[:, 0:12].rearrange("p (a b) -> p a b", a=2)
    mv = lnt[:, 12:14]
    rstd = lnt[:, 14:15]
    for hf in range(2):
        P.op("dve", lambda e, hf=hf: e.bn_stats(stats[:, hf, :], x_ap[:, hf * 512:(hf + 1) * 512]), reads=[x_t], writes=[lnt])
    P.op("dve", lambda e: e.bn_aggr(mv, lnt[:, 0:12]), reads=[lnt], writes=[lnt])
    P.op("act", lambda e: e.activation(rstd, lnt[:, 13:14], AF.Ln, bias=pp[:, PP_LNEPS:PP_LNEPS + 1]), reads=[lnt, pp], writes=[lnt])
    P.op("act", lambda e: e.activation(rstd, rstd, AF.Exp, scale=-0.5), reads=[lnt], writes=[lnt])
    P.op("dve", lambda e: e.tensor_scalar(x_ap, x_ap, lnt[:, 12:13], rstd, ALU.subtract, ALU.mult), reads=[x_t, lnt], writes=[x_t])
    P.op("pool", lambda e: e.tensor_tensor(x_ap, x_ap, pp[:, gcol:gcol + 1024], ALU.mult), reads=[x_t, pp], writes=[x_t])
    P.op("pool", lambda e: e.tensor_tensor(x_ap, x_ap, pp[:, bcol:bcol + 1024], ALU.add), reads=[x_t, pp], writes=[x_t])


def _pack_inputs(inp):
    L = 4
    pp = np.zeros((L, 128, PP_N), np.float32)
    bd = np.zeros((L, 2, 4, 128, 128), np.float32)
    for l in range(L):
        pp[l, :, PP_CAW:PP_CAW + 16] = inp["conv_a_w"][l].reshape(4, 4, 128).transpose(2, 1, 0).reshape(128, 16)
        pp[l, :, PP_CAB:PP_CAB + 4] = inp["conv_a_b"][l].reshape(4, 128).T
        pp[l, :, PP_RBA:PP_RBA + 4] = inp["rg_b_a"][l].reshape(4, 128).T
        pp[l, :, PP_RBX:PP_RBX + 4] = inp["rg_b_x"][l].reshape(4, 128).T
        pp[l, :, PP_LAM:PP_LAM + 4] = inp["rg_lambda"][l].reshape(4, 128).T
        pp[l, :, PP_GCW:PP_GCW + 48] = inp["gdn_conv_w"][l].reshape(4, 12, 128).transpose(2, 1, 0).reshape(128, 48)
        pp[l, :, PP_GNW] = inp["gdn_norm_w"][l]
        pp[l, :, PP_LNW] = inp["gla_norm_w"][l]
        pp[l, :, PP_DTB:PP_DTB + 4] = inp["gdn_dt_bias"][l][None, :]
        pp[l, :, PP_ALOG:PP_ALOG + 4] = inp["gdn_a_log"][l][None, :]
        pp[l, :, PP_RB:PP_RB + 16] = inp["router_bias"][None, :]
        pp[l, :, PP_GLB:PP_GLB + 512] = inp["gla_b_gate"][l][None, :]
        pp[l, :, PP_LN1G:PP_LN1G + 1024] = inp["ln1_g"][l][None, :]
        pp[l, :, PP_LN1B:PP_LN1B + 1024] = inp["ln1_b"][l][None, :]
        pp[l, :, PP_LN2G:PP_LN2G + 1024] = inp["ln2_g"][l][None, :]
        pp[l, :, PP_LN2B:PP_LN2B + 1024] = inp["ln2_b"][l][None, :]
        pp[l, :, PP_ONE] = 1.0
        pp[l, :, PP_EPS] = NORM_EPS
        pp[l, :, PP_LNEPS] = LN_EPS
        for a, nm in enumerate(("rg_w_a", "rg_w_x")):
            w = inp[nm][l]
            for ch in range(4):
                for gb in range(2):
                    bd[l, a, ch, gb * 64:(gb + 1) * 64, gb * 64:(gb + 1) * 64] = w[ch * 2 + gb]
    return pp, bd


_NC_CACHE = {}


def kernel(**inp):
    inp = {k: np.ascontiguousarray(np.asarray(v)) for k, v in inp.items()}
    n = 8
    nseq = 2
    pp, bd = _pack_inputs(inp)
    consts = host_consts()
    if "nc" not in _NC_CACHE:
        nc = bass.Bass("TRN2", target_bir_lowering=False)
        st = build(nc, L=4, NSEQ=nseq)
        _NC_CACHE["nc"] = (nc, st)
    nc = _NC_CACHE["nc"][0]
    x = inp["x"].reshape(n, nseq * S, D)
    shared = {"w_in": inp["w_in"], "w_branch": inp["w_branch"], "w_out": inp["w_out"], "w_gate": inp["w_gate"],
              "w_up": inp["w_up"], "w_down": inp["w_down"], "w_router": inp["w_router"], "pp": pp, "bd": bd,
              "gla_up": inp["gla_w_gate_up"], "consts": consts}
    in_maps = [dict(shared, x=x[i]) for i in range(n)]
    res = run_bass_kernel_spmd(nc, in_maps, core_ids=list(range(n)))
    out = np.stack([np.asarray(r["out"]) for r in res.results], 0)
    return out.reshape(16, S, D).astype(np.float32)
```

```python
from contextlib import ExitStack
import numpy as np
import concourse.bass as bass
import concourse.mybir as mybir
from concourse.bass_utils import run_bass_kernel_spmd

F32 = mybir.dt.float32
F32R = mybir.dt.float32r
BF16 = mybir.dt.bfloat16
AF = mybir.ActivationFunctionType
ALU = mybir.AluOpType
AX = mybir.AxisListType

STRICT = False
DBG = {}
ENGS = ("pe", "act", "dve", "pool", "sp")
CENG = ("pe", "act", "dve", "pool")


class Res:
    __slots__ = ("name", "w", "rs")

    def __init__(self, name):
        self.name = name
        self.w = None
        self.rs = {}


class Tok:
    __slots__ = ("kind", "eng", "idx", "clock")

    def __init__(self, kind, eng, idx, clock):
        self.kind = kind
        self.eng = eng
        self.idx = idx
        self.clock = clock


class T:
    __slots__ = ("t", "res", "name")

    def __init__(self, t, name):
        self.t = t
        self.res = Res(name)
        self.name = name

    def __getitem__(self, k):
        return self.t[k]


class Prog:
    def __init__(self, nc, stack):
        self.nc = nc
        self.stack = stack
        self.ops = {e: [] for e in ENGS}
        self.known = {e: {} for e in ENGS}
        self.n = {e: 0 for e in ENGS}
        self.last = {}
        self.dcount = {}
        self.needed = {e: set() for e in CENG}

    def sb(self, name, shape, dt=F32):
        t = self.stack.enter_context(self.nc.sbuf_tensor(name, list(shape), dt))
        return T(t, name)

    def ps(self, name, shape, dt=F32):
        t = self.stack.enter_context(self.nc.psum_tensor(name, list(shape), dt))
        return T(t, name)

    def _need(self, eng, known, waits, tok, raw, is_dma):
        if tok is None:
            return
        if tok.kind == "c":
            if tok.eng == eng and not raw and not is_dma and (not STRICT or eng == 'pe'):
                return
            if known.get(tok.eng, 0) >= tok.idx:
                return
            waits.append(("c", tok.eng, tok.idx))
            self.needed[tok.eng].add(tok.idx)
        else:
            if known.get(tok.eng, 0) >= tok.idx:
                return
            waits.append(("d", tok.eng, tok.idx))
        for k, v in tok.clock.items():
            if known.get(k, 0) < v:
                known[k] = v
        known[tok.eng] = tok.idx

    def _wait_list(self, eng, reads, writes, is_dma):
        known = self.known[eng]
        waits = []
        for r in reads:
            self._need(eng, known, waits, r.w, True, is_dma)
        for r in writes:
            self._need(eng, known, waits, r.w, False, is_dma)
            for tk in r.rs.values():
                self._need(eng, known, waits, tk, False, is_dma)
        return waits

    @staticmethod
    def _res(x):
        return x.res if isinstance(x, T) else x

    def op(self, eng, fn, reads=(), writes=()):
        reads = [self._res(r) for r in reads]
        writes = [self._res(r) for r in writes]
        waits = self._wait_list(eng, reads, writes, False)
        self.n[eng] += 1
        idx = self.n[eng]
        clock = {k: v for k, v in self.known[eng].items() if k in CENG}
        tok = Tok("c", eng, idx, clock)
        self.ops[eng].append((waits, fn, ("c", idx)))
        self.last[eng] = tok
        for r in reads:
            r.rs[eng] = tok
        for r in writes:
            r.w = tok
            r.rs = {}
        return tok

    def dma(self, eng, out_ap, in_ap, reads=(), writes=(), key=None):
        reads = [self._res(r) for r in reads]
        writes = [self._res(r) for r in writes]
        dkey = ("d", key)
        waits = self._wait_list(eng, reads, writes, True)
        cnt = self.dcount.get(dkey, 0) + 1
        self.dcount[dkey] = cnt
        clock = {k: v for k, v in self.known[eng].items() if k in CENG}
        tok = Tok("d", dkey, cnt, clock)

        nc = self.nc

        def fn(e, out_ap=out_ap, in_ap=in_ap):
            if out_ap.dtype == F32R:
                nc.dge_precook = False
                r = e.dma_start(out=out_ap, in_=in_ap)
                nc.dge_precook = True
                return r
            return e.dma_start(out=out_ap, in_=in_ap)

        self.ops[eng].append((waits, fn, ("d", dkey)))
        self.last[dkey] = tok
        for r in reads:
            r.rs[dkey] = tok
        for r in writes:
            r.w = tok
            r.rs = {}
        return tok

    def barrier(self, skip=(), skip_keys=()):
        toks = []
        for k, tok in self.last.items():
            if tok.kind == "d" and isinstance(tok.eng[1], tuple) and tok.eng[1][0] in skip_keys:
                continue
            toks.append(tok)
        for eng in ENGS:
            if eng in skip:
                continue
            known = self.known[eng]
            waits = []
            for tok in toks:
                self._need(eng, known, waits, tok, True, True)
            if waits:
                self.ops[eng].append((waits, None, None))

    def final_wait(self, eng, toks):
        known = self.known[eng]
        waits = []
        for tok in toks:
            self._need(eng, known, waits, tok, True, True)
        self.ops[eng].append((waits, None, None))

    def emit(self):
        nc = self.nc
        rank = {}
        for e in CENG:
            s = sorted(self.needed[e])
            rank[e] = {idx: i + 1 for i, idx in enumerate(s)}
            assert len(s) < 60000, (e, len(s))
        sems = {e: self.stack.enter_context(nc.semaphore("sem_" + e)) for e in CENG}
        dsems = {}
        for dkey in self.dcount:
            dsems[dkey] = self.stack.enter_context(nc.semaphore("dsem%d" % len(dsems)))
        block = self.stack.enter_context(nc.Block())

        def run(engname, eng):
            for waits, fn, info in self.ops[engname]:
                for kind, k, idx in waits:
                    if kind == "c":
                        eng.wait_ge(sems[k], rank[k][idx])
                    else:
                        eng.wait_ge(dsems[k], 16 * idx)
                if fn is None:
                    continue
                ins = fn(eng)
                if info[0] == "c":
                    if info[1] in rank[engname]:
                        ins.then_inc(sems[engname], 1)
                else:
                    ins.then_inc(dsems[info[1]], 16)

        @block.tensor
        def _(e):
            run("pe", e)

        @block.scalar
        def _(e):
            run("act", e)

        @block.vector
        def _(e):
            run("dve", e)

        @block.gpsimd
        def _(e):
            run("pool", e)

        @block.sync
        def _(e):
            run("sp", e)


D = 1024
S = 2048
TT = 512
NSUB = 4
D_IN = 10264
C_A = 0
C_BQKV = 512
C_BBETA = 2048
C_BGATE = 2056
C_CQKV = 2568
C_DQKV = 4104
C_DLR = 5640
C_DGATE = 5656
C_MERGE = 6168
ALPHA = 8.0 ** 0.25
LN_EPS = 1e-5
NORM_EPS = 1e-6

PP_CAW = 0
PP_CAB = 16
PP_RBA = 20
PP_RBX = 24
PP_LAM = 28
PP_GCW = 32
PP_GNW = 80
PP_LNW = 81
PP_DTB = 82
PP_ALOG = 86
PP_RB = 90
PP_GLB = 106
PP_LN1G = 618
PP_LN1B = 1642
PP_LN2G = 2666
PP_LN2B = 3690
PP_ONE = 4714
PP_EPS = 4715
PP_LNEPS = 4716
PP_N = 4717

K_ID = 0
K_LE = 128
K_GE = 256
K_GT = 384
K_ONE = 512
K_LE16 = 640
K_GT16 = 768
K_LT = 896
K_SBM = 1024
K_ZL = 1024 + 2048
K_N = 1024 + 2048 + 256


def host_consts():
    p = np.arange(128)[:, None]
    f = np.arange(128)[None, :]
    c = np.zeros((128, K_N), np.float32)
    c[:, K_ID:K_ID + 128] = (p == f)
    c[:, K_LE:K_LE + 128] = (p <= f)
    c[:, K_GE:K_GE + 128] = (p >= f)
    c[:, K_GT:K_GT + 128] = (p > f)
    c[:, K_ONE:K_ONE + 128] = 1.0
    c[:, K_LE16:K_LE16 + 128] = (p <= f) * (-1.0 / 16.0)
    c[:, K_GT16:K_GT16 + 128] = (p > f) * (-1.0 / 16.0)
    c[:, K_LT:K_LT + 128] = (p < f)
    c[:, K_ZL:K_ZL + 128] = (p <= f) * 1e4
    c[:, K_ZL + 128:K_ZL + 256] = (p > f) * 1e4
    f5 = np.arange(512)[None, :]
    for jo in range(4):
        c[:, K_SBM + jo * 512:K_SBM + (jo + 1) * 512] = (jo * 128 + p < f5)
    return c


def build(nc, L=4, NSEQ=2, taps=(), en="ABCD", moe=True):
    NTOK = NSEQ * S
    st = ExitStack()
    P = Prog(nc, st)
    dr = lambda name, shape, kind="ExternalInput": nc.dram_tensor(name, list(shape), F32, kind=kind).ap()
    x_d = dr("x", [NTOK, D])
    w_in_d = dr("w_in", [4, D, D_IN])
    w_br_d = dr("w_branch", [4, 4, 512, D])
    w_out_d = dr("w_out", [4, D, D])
    w_g_d = dr("w_gate", [4, 16, D, 512])
    w_u_d = dr("w_up", [4, 16, D, 512])
    w_d_d = dr("w_down", [4, 16, 512, D])
    w_r_d = dr("w_router", [D, 16])
    pp_d = dr("pp", [4, 128, PP_N])
    bd_d = dr("bd", [4, 2, 4, 128, 128])
    gup_d = dr("gla_up", [4, 16, 512])
    k_d = dr("consts", [128, K_N])
    out_d = dr("out", [NTOK, D], "ExternalOutput")
    xs_d = dr("xs_scr", [2, NTOK, D], "Internal")
    tap_d = {}
    for name, shape in taps:
        tap_d[name] = dr("tap_" + name, shape, "ExternalOutput")
    dres = {}

    def DR(key):
        if key not in dres:
            dres[key] = Res("dram:" + str(key))
        return dres[key]

    out_toks = []

    kc = P.sb("kc", [128, 1280])
    kb = P.sb("kb", [128, 2048], BF16)
    pp = P.sb("pp_sb", [128, PP_N])
    NB = 4
    wring = [P.sb("wr%d" % i, [128, 2048]) for i in range(NB)]
    wri = [0]
    bdw = P.sb("bdw", [128, 8, 128])
    gup = P.sb("gup", [16, 512])
    wrt = P.sb("wrt", [128, 8, 16])
    lam8 = P.sb("lam8", [128, 4])
    aexp = P.sb("aexp", [128, 4])
    nge = P.sb("nge", [128, 128], BF16)
    none_ = P.sb("none", [128, 128], BF16)
    ARENA = 36000 - 12288 - 2048 - 256
    arena = P.sb("arena", [128, ARENA])
    arenaR = P.sb("arenaR", [128, 12288])
    xst = [P.sb("xst0", [128, 1024]), P.sb("xst1", [128, 1024])]
    banks = [P.ps("bank%d" % i, [128, 512]) for i in range(8)]

    aoff = [0]

    live = {"a": [], "r": []}

    def inherit(which, start, end, t_new):
        keep = []
        for (s0, e0, t_old) in live[which]:
            if s0 < end and start < e0:
                toks = list(t_old.res.rs.values())
                if t_old.res.w is not None:
                    toks.append(t_old.res.w)
                for tk in toks:
                    cur = t_new.res.rs.get(tk.eng)
                    if cur is None or cur.idx < tk.idx:
                        t_new.res.rs[tk.eng] = tk
            else:
                keep.append((s0, e0, t_old))
        keep.append((start, end, t_new))
        live[which] = keep

    def carve(name, ncols, shape=None, dt=F32):
        DBG[name] = (aoff[0], ncols)
        ap = arena[:, aoff[0]:aoff[0] + ncols]
        start = aoff[0]
        aoff[0] += ncols
        assert aoff[0] <= ARENA, (name, aoff[0])
        if dt == BF16:
            ap = ap.bitcast(BF16)
        if shape is not None and len(shape) == 3:
            ap = ap.rearrange("p (a b) -> p a b", a=shape[1])
        t_new = T(ap, name)
        inherit("a", start, start + ncols, t_new)
        return t_new

    roff = [0]

    def carveR(name, ncols, shape=None):
        ap = arenaR[:, roff[0]:roff[0] + ncols]
        roff[0] += ncols
        assert roff[0] <= 12288
        if shape is not None and len(shape) == 3:
            ap = ap.rearrange("p (a b) -> p a b", a=shape[1])
        t_new = T(ap, name)
        inherit("r", roff[0] - ncols, roff[0], t_new)
        return t_new

    def r32(ap):
        return ap.bitcast(F32R)

    def v3(ap, a):
        return ap.rearrange("p (a b) -> p a b", a=a)

    def mm(out_t, out_ap, l_t, l_ap, r_t, r_ap, start, stop):
        P.op("pe", lambda e: e.matmul(out_ap, l_ap, r_ap, start=start, stop=stop), reads=[l_t, r_t], writes=[out_t])

    def tr(out_t, out_ap, in_t, in_ap):
        P.op("pe", lambda e: e.transpose(out_ap, in_ap, kc[:, K_ID:K_ID + 128]), reads=[in_t, kc], writes=[out_t])

    def act(out_t, out_ap, in_t, in_ap, func, bias=None, scale=None, extra=()):
        kw = {}
        if bias is not None:
            kw["bias"] = bias
        if scale is not None:
            kw["scale"] = scale
        P.op("act", lambda e: e.activation(out_ap, in_ap, func, **kw), reads=[in_t] + list(extra), writes=[out_t])

    def ts(eng, out_t, out_ap, in_t, in_ap, s1, s2, op0, op1=None, extra=()):
        if op1 is None:
            P.op(eng, lambda e: e.tensor_scalar(out_ap, in_ap, s1, None, op0), reads=[in_t] + list(extra), writes=[out_t])
        else:
            P.op(eng, lambda e: e.tensor_scalar(out_ap, in_ap, s1, s2, op0, op1), reads=[in_t] + list(extra), writes=[out_t])

    def tt(eng, out_t, out_ap, a_t, a_ap, b_t, b_ap, op):
        P.op(eng, lambda e: e.tensor_tensor(out_ap, a_ap, b_ap, op), reads=[a_t, b_t], writes=[out_t])

    def stt(out_t, out_ap, a_t, a_ap, sc, b_t, b_ap, op0, op1, extra=()):
        P.op("dve", lambda e: e.scalar_tensor_tensor(out_ap, a_ap, sc, b_ap, op0, op1),
             reads=[a_t, b_t] + list(extra), writes=[out_t])

    def cp(eng, out_t, out_ap, in_t, in_ap):
        if eng == "act":
            P.op("act", lambda e: e.copy(out_ap, in_ap), reads=[in_t], writes=[out_t])
        else:
            P.op(eng, lambda e: e.tensor_copy(out_ap, in_ap), reads=[in_t], writes=[out_t])

    def memset(eng, out_t, out_ap, val):
        P.op(eng, lambda e: e.memset(out_ap, val), writes=[out_t])

    def wload(src_ap):
        buf = wring[wri[0] % NB]
        wri[0] += 1
        n = 1
        for s_ in src_ap.shape[1:]:
            n *= s_
        dst = buf[:, 0:n]
        if len(src_ap.shape) == 3:
            dst = dst.rearrange("p (a b) -> p a b", a=src_ap.shape[1])
        P.dma("sp", dst.bitcast(F32R), src_ap.bitcast(F32R), writes=[buf], key=("wr", buf.name))
        return buf, dst

    def tap(name, src_t, src_ap, dst_ap_fn):
        if name in tap_d:
            tk = P.dma("pool", dst_ap_fn(tap_d[name]), src_ap, reads=[src_t], key=("tap", name))
            out_toks.append(tk)

    def barrier():
        pass

    P.dma("sp", kc[:, 0:1024], k_d[:, 0:1024], writes=[kc], key="kc")
    P.dma("sp", kc[:, 1024:1280], k_d[:, K_ZL:K_ZL + 256], writes=[kc], key="kc")
    P.dma("sp", arena[:, 0:2048], k_d[:, 1024:3072], writes=[arena], key="kcm")
    cp("dve", kb, kb[:], arena, arena[:, 0:2048])
    P.barrier()
    P.dma("sp", wrt[:], w_r_d.rearrange("(k p) e -> p k e", p=128), writes=[wrt], key="wrt")
    ts("dve", nge, nge[:], kc, kc[:, K_GE:K_GE + 128], -1.0, None, ALU.mult)
    ts("dve", none_, none_[:], kc, kc[:, K_ONE:K_ONE + 128], -1.0, None, ALU.mult)
    ident = kc[:, K_ID:K_ID + 128]
    ones = kc[:, K_ONE:K_ONE + 128]

    def win(l, c0, n):
        return wload(w_in_d[l].rearrange("(k p) n -> p k n", p=128)[:, :, c0:c0 + n])

    for l in range(L):
        barrier()
        P.dma("sp", pp[:], pp_d[l], writes=[pp], key="pp")
        P.dma("sp", bdw[:], bd_d[l].rearrange("a c p j -> p (a c) j"), writes=[bdw], key="bdw")
        P.dma("sp", gup[:], gup_d[l], writes=[gup], key="gup")
        act(lam8, lam8[:], pp, pp[:, PP_LAM:PP_LAM + 4], AF.Exp, scale=-1.0)
        act(lam8, lam8[:], lam8, lam8[:], AF.Ln, bias=pp[:, PP_ONE:PP_ONE + 1], extra=[pp])
        ts("dve", lam8, lam8[:], lam8, lam8[:], -8.0, None, ALU.mult)
        act(aexp, aexp[:], pp, pp[:, PP_ALOG:PP_ALOG + 4], AF.Exp)
        ts("dve", aexp, aexp[:], aexp, aexp[:], -1.0, None, ALU.mult)

        for s in range(NSEQ):
            barrier()
            aoff[0] = 0
            roff[0] = 0
            xT = carveR("xT", 4096, [128, 8, 512])
            merged = carveR("merged", 4096, [128, 8, 512])
            yn = carveR("yn", 2048, [128, 4, 512])
            KT = carve("KT", 4096, [128, 4, 2048], BF16)
            Vc = carve("Vc", 4096, [128, 16, 512], BF16)
            utail = carve("utail", 64, [128, 16, 4])
            hst = carve("hst", 4)
            S_d = [carve("S_d%d" % i_, 128) for i_ in range(4)]
            S_b = [carve("S_b%d" % i_, 128) for i_ in range(4)]
            S_db = [carve("S_db%d" % i_, 64, None, BF16) for i_ in range(4)]
            gsig = carve("gsig", 512)
            tmpm = carve("tmpm", 512)
            OV = aoff[0]
            memset("pool", utail, utail[:], 0.0)
            memset("pool", hst, hst[:], 0.0)
            for i_ in range(4):
                memset("pool", S_d[i_], S_d[i_][:], 0.0)
                memset("pool", S_db[i_], S_db[i_][:], 0.0)
            for i_ in range(4):
                memset("pool", S_b[i_], S_b[i_][:], 0.0)

            for c in range(NSUB):
                row0 = s * S + c * TT

                def proj_fm(bk, wb, wap, j0, ncols):
                    for k in range(8):
                        mm(bk, bk[0:ncols, :], wb, r32(wap[:, k, j0:j0 + ncols]), xT, r32(xT[:, k, :]), k == 0, k == 7)

                def proj_tm(bk, sub, wb, wap, j0, ncols):
                    for k in range(8):
                        mm(bk, bk[:, 0:ncols], xT, r32(xT[:, k, sub * 128:(sub + 1) * 128]), wb, r32(wap[:, k, j0:j0 + ncols]),
                           k == 0, k == 7)

                def branch_merge(n, first):
                    for dq in range(4):
                        wbb, wbap = wload(w_br_d[l, n].rearrange("(k p) n -> p k n", p=128)[:, :, dq * 256:(dq + 1) * 256])
                        wgb, wgap = win(l, C_MERGE + n * 1024 + dq * 256, 256)
                        for h_ in range(2):
                            dch = dq * 2 + h_
                            bp = banks[4 + (dch % 2)]
                            bg = banks[6 + (dch % 2)]
                            for k in range(4):
                                mm(bp, bp[:, :], wbb, r32(wbap[:, k, h_ * 128:(h_ + 1) * 128]), yn, r32(yn[:, k, :]), k == 0, k == 3)
                            proj_fm(bg, wgb, wgap, h_ * 128, 128)
                            act(gsig, gsig[:], bg, bg[:], AF.Sigmoid)
                            if first:
                                tt("dve", merged, r32(merged[:, dch, :]), bp, bp[:], gsig, gsig[:], ALU.mult)
                            else:
                                tt("dve", tmpm, tmpm[:], bp, bp[:], gsig, gsig[:], ALU.mult)
                                tt("pool", merged, r32(merged[:, dch, :]), merged, merged[:, dch, :], tmpm, tmpm[:], ALU.add)

                def gated_norm(o_t, o_ap, h, gate_c0, nw_col, t1, t2, t3, bs=None, bg=None):
                    act(t1, t1[:], o_t, o_ap, AF.Square)
                    bs = banks[6] if bs is None else bs
                    mm(bs, bs[:], kc, ones, t1, t1[:], True, True)
                    act(t2, t2[:], bs, bs[:], AF.Ln, bias=pp[:, PP_EPS:PP_EPS + 1], scale=1.0 / 128.0, extra=[pp])
                    act(t2, t2[:], t2, t2[:], AF.Exp, scale=-0.5)
                    wgb, wgap = win(l, gate_c0 + h * 128, 128)
                    bg = banks[7] if bg is None else bg
                    proj_fm(bg, wgb, wgap, 0, 128)
                    act(t3, t3[:], bg, bg[:], AF.Silu)
                    stt(t1, t1[:], o_t, o_ap, pp[:, nw_col:nw_col + 1], t2, t2[:], ALU.mult, ALU.mult, extra=[pp])
                    tt("dve", yn, r32(yn[:, h, :]), t1, t1[:], t3, t3[:], ALU.mult)

                barrier()
                for a in range(4):
                    xs_ = xst[a % 2]
                    r0 = row0 + a * 128
                    if l == 0:
                        P.dma("sp", r32(xs_[:]), r32(x_d[r0:r0 + 128, :]), writes=[xs_], key=("xst", a % 2))
                    else:
                        P.dma("sp", r32(xs_[:]), r32(xs_d[0, r0:r0 + 128, :]), reads=[DR(("x0", s, c))], writes=[xs_], key=("xst", a % 2))
                    for hf in range(2):
                        bk = banks[hf]
                        for k in range(4):
                            kk = hf * 4 + k
                            tr(bk, bk[:, k * 128:(k + 1) * 128], xs_, xs_[:, kk * 128:(kk + 1) * 128])
                        cp("act" if hf else "dve", xT, r32(xT[:, hf * 4:hf * 4 + 4, a * 128:(a + 1) * 128]), bk, v3(bk[:], 4))

                first = True
                if "A" in en:
                    barrier()
                    aoff[0] = OV
                    ubuf = carve("ubuf", 516)
                    t1 = carve("t1", 512)
                    t2 = carve("t2", 512)
                    t3 = carve("t3", 512)
                    t4 = carve("t4", 512)
                    wa = [win(l, C_A, 256), win(l, C_A + 256, 256)]
                    for ch in range(4):
                        bk = banks[ch % 2]
                        proj_fm(bk, wa[ch // 2][0], wa[ch // 2][1], (ch % 2) * 128, 128)
                        cp("dve", ubuf, ubuf[:, 0:3], utail, utail[:, ch, 0:3])
                        cp("act", ubuf, ubuf[:, 3:515], bk, bk[:])
                        cp("pool", utail, utail[:, ch, 0:3], ubuf, ubuf[:, 512:515])
                        cw = lambda k: pp[:, PP_CAW + ch * 4 + k:PP_CAW + ch * 4 + k + 1]
                        ts("dve", t1, t1[:], ubuf, ubuf[:, 3:515], cw(3), pp[:, PP_CAB + ch:PP_CAB + ch + 1], ALU.mult, ALU.add, extra=[pp])
                        for k in range(3):
                            stt(t1, t1[:], ubuf, ubuf[:, k:k + 512], cw(k), t1, t1[:], ALU.mult, ALU.add, extra=[pp])
                        ba = banks[2]
                        bx = banks[3]
                        mm(ba, ba[:], bdw, bdw[:, ch, :], t1, t1[:], True, True)
                        mm(bx, bx[:], bdw, bdw[:, 4 + ch, :], t1, t1[:], True, True)
                        act(t2, t2[:], ba, ba[:], AF.Sigmoid, bias=pp[:, PP_RBA + ch:PP_RBA + ch + 1], extra=[pp])
                        act(t3, t3[:], bx, bx[:], AF.Sigmoid, bias=pp[:, PP_RBX + ch:PP_RBX + ch + 1], extra=[pp])
                        act(t2, t2[:], t2, t2[:], AF.Exp, scale=lam8[:, ch:ch + 1], extra=[lam8])
                        tt("dve", t4, t4[:], t2, t2[:], t2, t2[:], ALU.mult)
                        act(t4, t4[:], t4, t4[:], AF.Sqrt, bias=pp[:, PP_ONE:PP_ONE + 1], scale=-1.0, extra=[pp])
                        if c == 0:
                            memset("dve", t4, t4[:, 0:1], 1.0)
                        tt("dve", t3, t3[:], t3, t3[:], t1, t1[:], ALU.mult)
                        tt("dve", t3, t3[:], t3, t3[:], t4, t4[:], ALU.mult)
                        P.op("dve", lambda e, o_=r32(yn[:, ch, :]), a_=t2[:], b_=t3[:], i_=hst[:, ch:ch + 1]: e.tensor_tensor_scan(o_, a_, b_, i_, ALU.mult, ALU.add),
                             reads=[t2, t3, hst], writes=[yn])
                        cp("pool", hst, hst[:, ch:ch + 1], yn, yn[:, ch, 511:512])
                    tap("y_a", yn, yn[:], lambda d_: d_[:, :, c * TT:(c + 1) * TT])
                    branch_merge(0, first)
                    first = False

                if "B" in en:
                    barrier()
                    aoff[0] = OV
                    qnT = carve("qnT", 2048, [128, 4, 512])
                    knT = carve("knT", 2048, [128, 4, 512])
                    vT = carve("vT", 2048, [128, 4, 512])
                    ubuf = carve("ubuf", 516)
                    t1 = carve("t1", 512)
                    t2 = carve("t2", 512)
                    t3 = carve("t3", 512)
                    beta_t = carve("beta_t", 16)
                    g_t = carve("g_t", 16)
                    gcc = carve("gcc", 16)
                    bexp = carve("bexp", 16)
                    kdsc = carve("kdsc", 16)
                    gl = carve("gl", 16)
                    tsm = carve("tsm", 16)
                    dests = [qnT, knT, vT]
                    for un in range(6):
                        wq = win(l, C_BQKV + un * 256, 256)
                        for h_ in range(2):
                            ci = un * 2 + h_
                            bk = banks[ci % 2]
                            proj_fm(bk, wq[0], wq[1], h_ * 128, 128)
                            cp("dve", ubuf, ubuf[:, 0:3], utail, utail[:, 4 + ci, 0:3])
                            cp("act", ubuf, ubuf[:, 3:515], bk, bk[:])
                            cp("pool", utail, utail[:, 4 + ci, 0:3], ubuf, ubuf[:, 512:515])
                            cw = lambda k: pp[:, PP_GCW + ci * 4 + k:PP_GCW + ci * 4 + k + 1]
                            ts("dve", t1, t1[:], ubuf, ubuf[:, 3:515], cw(3), None, ALU.mult, extra=[pp])
                            for k in range(3):
                                stt(t1, t1[:], ubuf, ubuf[:, k:k + 512], cw(k), t1, t1[:], ALU.mult, ALU.add, extra=[pp])
                            dst = dests[ci // 4]
                            act(dst, dst[:, ci % 4, :], t1, t1[:], AF.Silu)
                    for qi, dst in enumerate((qnT, knT)):
                        for h in range(4):
                            act(t1, t1[:], dst, dst[:, h, :], AF.Square)
                            bs = banks[2 + h % 2]
                            mm(bs, bs[:], kc, ones, t1, t1[:], True, True)
                            act(t2, t2[:], bs, bs[:], AF.Ln, bias=pp[:, PP_EPS:PP_EPS + 1], extra=[pp])
                            act(t2, t2[:], t2, t2[:], AF.Exp, scale=-0.5)
                            if qi == 0:
                                stt(dst, dst[:, h, :], dst, dst[:, h, :], 128.0 ** -0.5, t2, t2[:], ALU.mult, ALU.mult)
                            else:
                                tt("dve", dst, dst[:, h, :], dst, dst[:, h, :], t2, t2[:], ALU.mult)
                    wbg = win(l, C_BBETA, 8)
                    bsm = banks[4]
                    for sub in range(4):
                        for k in range(8):
                            mm(bsm, bsm[:, sub * 8:(sub + 1) * 8], xT, r32(xT[:, k, sub * 128:(sub + 1) * 128]), wbg[0], r32(wbg[1][:, k, 0:8]), k == 0, k == 7)
                    bsm3 = v3(bsm[:, 0:32], 4)
                    b3 = lambda t_: v3(t_[:], 4)
                    act(beta_t, b3(beta_t), bsm, bsm3[:, :, 0:4], AF.Sigmoid)
                    cp("act", tsm, b3(tsm), bsm, bsm3[:, :, 4:8])
                    tt("dve", tsm, b3(tsm), tsm, b3(tsm), pp, pp[:, PP_DTB:PP_DTB + 4].unsqueeze(1).to_broadcast([128, 4, 4]), ALU.add)
                    act(tsm, tsm[:], tsm, tsm[:], AF.Exp)
                    act(tsm, tsm[:], tsm, tsm[:], AF.Ln, bias=pp[:, PP_ONE:PP_ONE + 1], extra=[pp])
                    tt("dve", g_t, b3(g_t), tsm, b3(tsm), aexp, aexp[:].unsqueeze(1).to_broadcast([128, 4, 4]), ALU.mult)
                    bsm2 = banks[5]
                    for sub in range(4):
                        mm(bsm2, bsm2[:, sub * 4:(sub + 1) * 4], kc, kc[:, K_LE:K_LE + 128], g_t, g_t[:, sub * 4:(sub + 1) * 4], True, True)
                    for sub in range(4):
                        mm(bsm2, bsm2[:, 16 + sub * 4:16 + (sub + 1) * 4], kc, ones, g_t, g_t[:, sub * 4:(sub + 1) * 4], True, True)
                    cp("act", gcc, gcc[:], bsm2, bsm2[:, 0:16])
                    act(bexp, bexp[:], bsm2, bsm2[:, 0:16], AF.Exp)
                    tt("dve", bexp, bexp[:], bexp, bexp[:], beta_t, beta_t[:], ALU.mult)
                    act(gl, gl[:], bsm2, bsm2[:, 16:32], AF.Exp)
                    cp("act", kdsc, kdsc[:], bsm2, bsm2[:, 16:32])
                    tt("dve", kdsc, kdsc[:], kdsc, kdsc[:], gcc, gcc[:], ALU.subtract)
                    act(kdsc, kdsc[:], kdsc, kdsc[:], AF.Exp)
                    TOPB = aoff[0]
                    names = ("tg", "tmp1", "tmp2", "qdec", "PT", "attn_s", "kbg", "kdec", "vb", "u_s", "wT_s", "vn_s")

                    def mk_tmp(tagc):
                        d = {n_: carve(n_ + tagc, 128) for n_ in names}
                        d["AAT"] = [carve("AAT0" + tagc, 256), carve("AAT1" + tagc, 256)]
                        return d

                    def chain(h, sub, tm, bA, bB, bC, bo):
                        cs = slice(sub * 128, (sub + 1) * 128)
                        si = sub * 4 + h
                        tg, tmp1, tmp2, qdec, PT, attn_s = tm["tg"], tm["tmp1"], tm["tmp2"], tm["qdec"], tm["PT"], tm["attn_s"]
                        kbg, kdec, vb, u_s, wT_s, vn_s, AAT = tm["kbg"], tm["kdec"], tm["vb"], tm["u_s"], tm["wT_s"], tm["vn_s"], tm["AAT"]
                        ts("dve", tg, tg[:], kc, kc[:, K_LE:K_LE + 128], g_t[:, si:si + 1], None, ALU.mult, extra=[g_t])
                        mm(bA, bA[:, 0:128], kc, ones, tg, tg[:], True, True)
                        mm(bA, bA[:, 128:256], knT, knT[:, h, cs], knT, knT[:, h, cs], True, True)
                        mm(bA, bA[:, 256:384], knT, knT[:, h, cs], qnT, qnT[:, h, cs], True, True)
                        mm(bA, bA[:, 384:512], knT, knT[:, h, cs], kc, ident, True, True)
                        mm(bB, bB[:, 0:128], vT, vT[:, h, cs], kc, ident, True, True)
                        yield
                        stt(tmp1, tmp1[:], bA, bA[:, 0:128], gcc[:, si:si + 1], kc, kc[:, 1152:1280], ALU.subtract, ALU.subtract, extra=[gcc])
                        stt(tmp2, tmp2[:], bA, bA[:, 0:128], gcc[:, si:si + 1], kc, kc[:, 1024:1152], ALU.subtract, ALU.add, extra=[gcc])
                        act(tmp1, tmp1[:], tmp1, tmp1[:], AF.Exp)
                        act(tmp2, tmp2[:], tmp2, tmp2[:], AF.Exp, scale=-1.0)
                        act(tg, tg[:], bA, bA[:, 0:128], AF.Exp)
                        yield
                        tt("dve", qdec, qdec[:], qnT, qnT[:, h, cs], tg, tg[:], ALU.mult)
                        stt(AAT[0], AAT[0][:, 0:128], bA, bA[:, 128:256], beta_t[:, si:si + 1], tmp2, tmp2[:], ALU.mult, ALU.mult, extra=[beta_t])
                        tt("dve", attn_s, attn_s[:], bA, bA[:, 256:384], tmp1, tmp1[:], ALU.mult)
                        ts("dve", kbg, kbg[:], bA, bA[:, 384:512], bexp[:, si:si + 1], None, ALU.mult, extra=[bexp])
                        ts("dve", kdec, kdec[:], bA, bA[:, 384:512], kdsc[:, si:si + 1], None, ALU.mult, extra=[kdsc])
                        ts("dve", vb, vb[:], bB, bB[:, 0:128], beta_t[:, si:si + 1], None, ALU.mult, extra=[beta_t])
                        mm(bB, bB[:, 128:256], AAT[0], AAT[0][:, 0:128], kc, ident, True, True)
                        yield
                        cp("act", AAT[0], AAT[0][:, 128:256], bB, bB[:, 128:256])
                        act(PT, PT[:], bB, bB[:, 128:256], AF.Copy, scale=-1.0)
                        tt("dve", PT, PT[:], PT, PT[:], kc, ident, ALU.add)
                        cur = 0
                        for m in range(1, 7):
                            A_c = AAT[cur]
                            A_n = AAT[1 - cur]
                            mm(bC, bC[:, 0:128], A_c, A_c[:, 128:256], A_c, A_c[:, 0:128], True, True)
                            if m < 6:
                                mm(bC, bC[:, 128:256], A_c, A_c[:, 0:128], A_c, A_c[:, 128:256], True, True)
                            yield
                            if m < 6:
                                cp("act", A_n, A_n[:, 0:256], bC, bC[:, 0:256])
                            else:
                                cp("act", A_n, A_n[:, 0:128], bC, bC[:, 0:128])
                            mm(bC, bC[:, 256:384], A_n, A_n[:, 0:128], PT, PT[:], True, True)
                            yield
                            tt("dve", PT, PT[:], PT, PT[:], bC, bC[:, 256:384], ALU.add)
                            cur = 1 - cur
                        mm(bB, bB[:, 256:384], PT, PT[:], vb, vb[:], True, True)
                        mm(bB, bB[:, 384:512], kbg, kbg[:], PT, PT[:], True, True)
                        yield
                        cp("act", u_s, u_s[:], bB, bB[:, 256:384])
                        cp("act", wT_s, wT_s[:], bB, bB[:, 384:512])
                        mm(bC, bC[:, 384:512], wT_s, wT_s[:], S_b[h], S_b[h][:], True, True)
                        yield
                        tt("dve", vn_s, vn_s[:], u_s, u_s[:], bC, bC[:, 384:512], ALU.subtract)
                        mm(bo, bo[:, cs], S_b[h], S_b[h][:], qdec, qdec[:], True, False)
                        mm(bo, bo[:, cs], vn_s, vn_s[:], attn_s, attn_s[:], False, True)
                        mm(bC, bC[:, 0:128], kdec, kdec[:], vn_s, vn_s[:], True, True)
                        yield
                        stt(S_b[h], S_b[h][:], S_b[h], S_b[h][:], gl[:, si:si + 1], bC, bC[:, 0:128], ALU.mult, ALU.add, extra=[gl])
                        yield

                    for hp in range(2):
                        aoff[0] = OV + 6144
                        tm1 = mk_tmp("_b")
                        aoff[0] = max(aoff[0], TOPB)
                        tm0 = mk_tmp("_a")
                        tms = (tm0, tm1)
                        for sub in range(4):
                            gens = []
                            for j in range(2):
                                h = 2 * hp + j
                                gens.append(chain(h, sub, tms[j], banks[4 * j], banks[4 * j + 1], banks[4 * j + 2], banks[4 * j + 3]))
                            alive = list(gens)
                            while alive:
                                nxt = []
                                for g_ in alive:
                                    try:
                                        next(g_)
                                        nxt.append(g_)
                                    except StopIteration:
                                        pass
                                alive = nxt
                        aoff[0] = OV + 6144
                        ubuf = carve("ubuf", 516)
                        t1 = carve("t1", 512)
                        t2 = carve("t2", 512)
                        t3 = carve("t3", 512)
                        for j in range(2):
                            h = 2 * hp + j
                            gated_norm(banks[4 * j + 3], banks[4 * j + 3][:], h, C_BGATE, PP_GNW, t1, t2, t3, bs=banks[4 * j], bg=banks[4 * j + 1])
                    tap("y_b", yn, yn[:], lambda d_: d_[:, :, c * TT:(c + 1) * TT])
                    branch_merge(1, first)
                    first = False

                if "C" in en:
                    barrier()
                    aoff[0] = OV
                    qTb = carve("qTb", 2048, [128, 8, 512], BF16)
                    memset("pool", qTb, qTb[:], 0.0)
                    e_sb = [carve("e_sb%d" % i_, 512) for i_ in range(4)]
                    sp_sb = [carve("sp_sb%d" % i_, 256, None, BF16) for i_ in range(4)]
                    spsum = [carve("spsum%d" % i_, 512) for i_ in range(4)]
                    spsum_b = [carve("spsum_b%d" % i_, 256, None, BF16) for i_ in range(4)]
                    w_sb = [carve("w_sb%d" % i_, 256, None, BF16) for i_ in range(4)]
                    qb0 = c * 4
                    for u in range(2):
                        wq = win(l, C_CQKV + u * 256, 256)
                        wk = win(l, C_CQKV + 512 + u * 256, 256)
                        for h_ in range(2):
                            pr = u * 2 + h_
                            bq = banks[0 + h_]
                            proj_fm(bq, wq[0], wq[1], h_ * 128, 128)
                            ts("dve", qTb, qTb[0:64, 2 * pr, :], bq, bq[0:64, :], 0.125, None, ALU.mult)
                            ts("dve", qTb, qTb[64:128, 2 * pr + 1, :], bq, bq[64:128, :], 0.125, None, ALU.mult)
                            bk_ = banks[2 + h_]
                            proj_fm(bk_, wk[0], wk[1], h_ * 128, 128)
                            cp("act", KT, KT[:, pr, c * TT:(c + 1) * TT], bk_, bk_[:])
                    wv0 = win(l, C_CQKV + 1024, 256)
                    wv1 = win(l, C_CQKV + 1280, 256)
                    for sub in range(4):
                        bv = banks[sub % 2]
                        proj_tm(bv, sub, wv0[0], wv0[1], 0, 256)
                        cp("act", Vc, Vc[:, qb0 + sub, 0:256], bv, bv[:, 0:256])
                        bv2 = banks[2 + sub % 2]
                        proj_tm(bv2, sub, wv1[0], wv1[1], 0, 256)
                        cp("dve", Vc, Vc[:, qb0 + sub, 256:512], bv2, bv2[:, 0:256])
                    nkb = qb0 + 4
                    for pg in range(2):
                        for j in range(4):
                            memset("pool", spsum[j], spsum[j][:], 0.0)
                            memset("pool", spsum_b[j], spsum_b[j][:], 0.0)
                        for step, J in enumerate(range(nkb - 1, -1, -1)):
                            jo = J - qb0
                            q0 = max(jo, 0) * 128
                            qs = slice(q0, 512)
                            dg = slice(q0, q0 + 128)
                            for j in range(4):
                                h = 4 * pg + j
                                bzd = banks[j]
                                mm(bzd, bzd[:, qs], KT, KT[:, h // 2, J * 128:(J + 1) * 128], qTb, qTb[:, h, qs], True, False)
                            for j in range(4):
                                bzd = banks[j]
                                e_h, sp_h = e_sb[j], sp_sb[j]
                                act(e_h, e_h[:, qs], bzd, bzd[:, qs], AF.Exp)
                                act(sp_h, sp_h[:, qs], e_h, e_h[:, qs], AF.Ln, bias=pp[:, PP_ONE:PP_ONE + 1], extra=[pp])
                                if jo >= 0:
                                    tt("dve", sp_h, sp_h[:, dg], sp_h, sp_h[:, dg], kb, kb[:, 0:128], ALU.mult)
                            for j in range(4):
                                bzd = banks[j]
                                sp_h, sub_h = sp_sb[j], spsum_b[j]
                                mm(bzd, bzd[:, qs], nge, nge[:], sp_h, sp_h[:, qs], False, step == 0)
                                if step > 0:
                                    mm(bzd, bzd[:, qs], none_, none_[:], sub_h, sub_h[:, qs], False, True)
                            for j in range(4):
                                bzd = banks[j]
                                sp_h, su_h, sub_h, w_h = sp_sb[j], spsum[j], spsum_b[j], w_sb[j]
                                act(w_h, w_h[:, qs], bzd, bzd[:, qs], AF.Exp)
                                if jo >= 0:
                                    tt("dve", w_h, w_h[:, dg], w_h, w_h[:, dg], kb, kb[:, 0:128], ALU.mult)
                                if J > 0:
                                    tt("dve", su_h, su_h[:, qs], su_h, su_h[:, qs], sp_h, sp_h[:, qs], ALU.add)
                                    cp("dve", sub_h, sub_h[:, qs], su_h, su_h[:, qs])
                            for j in range(4):
                                h = 4 * pg + j
                                bo = banks[4 + j]
                                w_h = w_sb[j]
                                mm(bo, bo[:, qs], Vc, Vc[:, J, (h // 2) * 128:(h // 2 + 1) * 128], w_h, w_h[:, qs], step == 0, J == 0)
                        for j in range(4):
                            h = 4 * pg + j
                            pr = h // 2
                            hh = h % 2
                            ps_ = slice(hh * 64, hh * 64 + 64)
                            cp("act", yn, r32(yn[ps_, pr, :]), banks[4 + j], banks[4 + j][ps_, :])
                    tap("y_c", yn, yn[:], lambda d_: d_[:, :, c * TT:(c + 1) * TT])
                    branch_merge(2, first)
                    first = False

                if "D" in en:
                    barrier()
                    aoff[0] = OV
                    t1 = carve("t1", 512)
                    t2 = carve("t2", 512)
                    t3 = carve("t3", 512)
                    sp_tok = carve("sp_tok", 2048, [128, 4, 512])
                    qd = carve("qd", 1024, [128, 4, 512], BF16)
                    ki = carve("ki", 1024, [128, 4, 512], BF16)
                    kdt = carve("kdt", 1024, [128, 4, 512], BF16)
                    vtk = carve("vtk", 1024, [128, 4, 512], BF16)
                    lrT = carve("lrT", 512)
                    glast = carve("glast", 16)
                    attn = [carve("attn%d" % i_, 64, None, BF16) for i_ in range(4)]
                    wl = win(l, C_DLR, 16)
                    b0 = banks[0]
                    proj_fm(b0, wl[0], wl[1], 0, 16)
                    cp("act", lrT, lrT[0:16, :], b0, b0[0:16, :])
                    for sub in range(4):
                        bg_ = banks[1 + sub % 2]
                        mm(bg_, bg_[:], lrT, lrT[0:16, sub * 128:(sub + 1) * 128], gup, gup[:], True, True)
                        tt("dve", t1, t1[:], bg_, bg_[:], pp, pp[:, PP_GLB:PP_GLB + 512], ALU.add)
                        act(t1, t1[:], t1, t1[:], AF.Exp, scale=-1.0)
                        act(sp_tok, sp_tok[:, sub, :], t1, t1[:], AF.Ln, bias=pp[:, PP_ONE:PP_ONE + 1], extra=[pp])
                    wk0 = win(l, C_DQKV + 512, 256)
                    wk1 = win(l, C_DQKV + 768, 256)
                    for sub in range(4):
                        br = banks[3]
                        mm(br, br[:], kc, kc[:, K_GT16:K_GT16 + 128], sp_tok, sp_tok[:, sub, :], True, True)
                        act(t2, t2[:], br, br[:], AF.Exp)
                        for hf, wk_ in enumerate((wk0, wk1)):
                            bkk = banks[4 + hf]
                            proj_tm(bkk, sub, wk_[0], wk_[1], 0, 256)
                            tt("dve", kdt, kdt[:, sub, hf * 256:(hf + 1) * 256], bkk, bkk[:, 0:256], t2, t2[:, hf * 256:(hf + 1) * 256], ALU.mult)
                    wv0 = win(l, C_DQKV + 1024, 256)
                    wv1 = win(l, C_DQKV + 1280, 256)
                    for sub in range(4):
                        for hf, wvv in enumerate((wv0, wv1)):
                            bvv = banks[6 + hf]
                            proj_tm(bvv, sub, wvv[0], wvv[1], 0, 256)
                            cp("act", vtk, vtk[:, sub, hf * 256:(hf + 1) * 256], bvv, bvv[:, 0:256])
                    for h in range(4):
                        bb = banks[0]
                        for sub in range(4):
                            mm(bb, bb[:, sub * 128:(sub + 1) * 128], sp_tok, sp_tok[:, sub, h * 128:(h + 1) * 128],
                               kc, kc[:, K_LE16:K_LE16 + 128], True, True)
                        act(t1, t1[:], bb, bb[:], AF.Exp)
                        act(t2, t2[:], bb, bb[:], AF.Exp, scale=-1.0)
                        for sub in range(4):
                            cp("pool", glast, glast[:, h * 4 + sub:h * 4 + sub + 1], t1, t1[:, sub * 128 + 127:sub * 128 + 128])
                        wq = win(l, C_DQKV + h * 128, 128)
                        bq = banks[1]
                        proj_fm(bq, wq[0], wq[1], 0, 128)
                        stt(qd, qd[:, h, :], bq, bq[:], 128.0 ** -0.5, t1, t1[:], ALU.mult, ALU.mult)
                        wkf = win(l, C_DQKV + 512 + h * 128, 128)
                        bk2 = banks[2]
                        proj_fm(bk2, wkf[0], wkf[1], 0, 128)
                        tt("dve", ki, ki[:, h, :], bk2, bk2[:], t2, t2[:], ALU.mult)
                    for sub in range(4):
                        cs = slice(sub * 128, (sub + 1) * 128)
                        for h in range(4):
                            ba_ = banks[4 + h]
                            mm(ba_, ba_[:, 0:128], ki, ki[:, h, cs], qd, qd[:, h, cs], True, True)
                        for h in range(4):
                            ba_ = banks[4 + h]
                            tt("dve", attn[h], attn[h][:], ba_, ba_[:, 0:128], kc, kc[:, K_LE:K_LE + 128], ALU.mult)
                        for h in range(4):
                            bo = banks[h]
                            hs = slice(h * 128, (h + 1) * 128)
                            mm(bo, bo[:, cs], vtk, vtk[:, sub, hs], attn[h], attn[h][:], True, False)
                            mm(bo, bo[:, cs], S_db[h], S_db[h][:], qd, qd[:, h, cs], False, True)
                            bs_ = banks[4 + h]
                            mm(bs_, bs_[:, 128:256], kdt, kdt[:, sub, hs], vtk, vtk[:, sub, hs], True, True)
                        for h in range(4):
                            bs_ = banks[4 + h]
                            stt(S_d[h], S_d[h][:], S_d[h], S_d[h][:], glast[:, h * 4 + sub:h * 4 + sub + 1], bs_, bs_[:, 128:256],
                                ALU.mult, ALU.add, extra=[glast])
                            cp("pool", S_db[h], S_db[h][:], S_d[h], S_d[h][:])
                    for h in range(4):
                        gated_norm(banks[h], banks[h][:], h, C_DGATE, PP_LNW, t1, t2, t3)
                    tap("y_d", yn, yn[:], lambda d_: d_[:, :, c * TT:(c + 1) * TT])
                    branch_merge(3, first)
                    first = False

                barrier()
                aoff[0] = OV
                x_tok = carve("x_tok", 4096, [128, 4, 1024])
                x_sub = [T(x_tok[:, i_, :], "x_sub%d" % i_) for i_ in range(4)]
                lnts = [carve("lnt%d" % i_, 64) for i_ in range(4)]
                src = (x_d if l == 0 else xs_d[0])[row0:row0 + TT, :]
                P.dma("pool", x_tok[:], src.rearrange("(a p) d -> p a d", p=128),
                      reads=([DR(("x0", s, c))] if l > 0 else []), writes=[x_tok] + x_sub, key="xtok")
                for dq in range(4):
                    wo = wload(w_out_d[l].rearrange("(k p) n -> p k n", p=128)[:, :, dq * 256:(dq + 1) * 256])
                    for sub in range(4):
                        bo = banks[sub % 4]
                        for k in range(8):
                            mm(bo, bo[:, 0:256], merged, r32(merged[:, k, sub * 128:(sub + 1) * 128]), wo[0], r32(wo[1][:, k, :]), k == 0, k == 7)
                        stt(x_sub[sub], x_tok[:, sub, dq * 256:(dq + 1) * 256], x_sub[sub], x_tok[:, sub, dq * 256:(dq + 1) * 256], ALPHA,
                            bo, bo[:, 0:256], ALU.mult, ALU.add)
                for sub in range(4):
                    layer_norm(P, x_sub[sub], x_tok[:, sub, :], lnts[sub], pp, PP_LN1G, PP_LN1B)
                if "x1" in tap_d:
                    tk = P.dma("pool", tap_d["x1"][c * TT:(c + 1) * TT, :].rearrange("(a p) d -> p a d", p=128), x_tok[:], reads=[x_tok] + x_sub, key=("tap", "x1"))
                    out_toks.append(tk)
                dst_d = xs_d[1] if moe else (out_d if l == L - 1 else xs_d[0])
                tk = P.dma("pool", dst_d[row0:row0 + TT, :].rearrange("(a p) d -> p a d", p=128), x_tok[:],
                           reads=[x_tok] + x_sub, writes=[DR((("x1" if moe else "x0"), s, c))], key="xst_out")
                if not moe and l == L - 1:
                    out_toks.append(tk)

            if not moe:
                continue
            for g in range(2):
                barrier()
                aoff[0] = 0
                roff[0] = 0
                x1T = carveR("x1T", 8192, [128, 8, 1024])
                hT = [carveR("hT0", 2048, [128, 2, 1024]), carveR("hT1", 2048, [128, 2, 1024])]
                yacc = carve("yacc", 8192, [128, 8, 1024])
                y_sub = [T(yacc[:, i_, :], "y_sub%d" % i_) for i_ in range(8)]
                sg = [carve("sg0", 512), carve("sg1", 512)]
                sc = carve("sc", 128, [128, 8, 16])
                bi = carve("bi", 128, [128, 8, 16])
                mb = carve("mb", 128, [128, 8, 16])
                eq = carve("eq", 128, [128, 8, 16])
                sel = carve("sel", 128, [128, 8, 16])
                comb = carve("comb", 128, [128, 8, 16])
                m1 = carve("m1", 32)
                m2 = carve("m2", 32)
                gsel = carve("gsel", 32)
                gm = carve("gm", 8)
                lnts = [carve("lnt%d" % i_, 64) for i_ in range(8)]
                grow0 = s * S + g * 1024
                P.dma("pool", yacc[:], xs_d[1, grow0:grow0 + 1024, :].rearrange("(a p) d -> p a d", p=128),
                      reads=[DR(("x1", s, 2 * g)), DR(("x1", s, 2 * g + 1))], writes=[yacc] + y_sub, key="yacc")
                for a in range(8):
                    xs_ = xst[a % 2]
                    P.dma("sp", r32(xs_[:]), r32(xs_d[1, grow0 + a * 128:grow0 + (a + 1) * 128, :]),
                          reads=[DR(("x1", s, 2 * g)), DR(("x1", s, 2 * g + 1))], writes=[xs_], key=("xst", a % 2))
                    for hf in range(2):
                        bk = banks[hf]
                        for k in range(4):
                            kk = hf * 4 + k
                            tr(bk, bk[:, k * 128:(k + 1) * 128], xs_, xs_[:, kk * 128:(kk + 1) * 128])
                        cp("act" if hf else "dve", x1T, r32(x1T[:, hf * 4:hf * 4 + 4, a * 128:(a + 1) * 128]), bk, v3(bk[:], 4))
                brt = banks[2]
                for a in range(8):
                    for k in range(8):
                        mm(brt, brt[:, a * 16:(a + 1) * 16], x1T, x1T[:, k, a * 128:(a + 1) * 128], wrt, wrt[:, k, :], k == 0, k == 7)
                act(sc, sc[:], brt, v3(brt[:, 0:128], 8), AF.Sigmoid)
                rb_b = pp[:, PP_RB:PP_RB + 16].unsqueeze(1).to_broadcast([128, 8, 16])
                tt("dve", bi, bi[:], sc, sc[:], pp, rb_b, ALU.add)
                bi4 = bi[:].rearrange("p a (g e) -> p (a g) e", g=4)
                mb4 = mb[:].rearrange("p a (g e) -> p (a g) e", g=4)
                eq4 = eq[:].rearrange("p a (g e) -> p (a g) e", g=4)
                P.op("dve", lambda e, o_=m1[:], i_=bi4: e.tensor_reduce(o_, i_, AX.X, ALU.max), reads=[bi], writes=[m1])
                tt("dve", eq, eq4, bi, bi4, m1, m1[:].unsqueeze(2).to_broadcast([128, 32, 4]), ALU.is_equal)
                stt(mb, mb4, eq, eq4, -1e30, bi, bi4, ALU.mult, ALU.add)
                P.op("dve", lambda e, o_=m2[:], i_=mb4: e.tensor_reduce(o_, i_, AX.X, ALU.max), reads=[mb], writes=[m2])
                tt("dve", m1, m1[:], m1, m1[:], m2, m2[:], ALU.add)
                m1g = m1[:].rearrange("p (a g) -> p a g", g=4)
                P.op("dve", lambda e, o_=gm[:], i_=m1g: e.tensor_reduce(o_, i_, AX.X, ALU.max), reads=[m1], writes=[gm])
                tt("dve", gsel, gsel[:].rearrange("p (a g) -> p a g", g=4), m1, m1g, gm, gm[:].unsqueeze(2).to_broadcast([128, 8, 4]), ALU.is_equal)
                ts("dve", gsel, gsel[:], gsel, gsel[:], -1.0, 1e30, ALU.add, ALU.mult)
                tt("dve", mb, mb4, bi, bi4, gsel, gsel[:].unsqueeze(2).to_broadcast([128, 32, 4]), ALU.add)
                P.op("dve", lambda e, o_=gm[:], i_=mb[:]: e.tensor_reduce(o_, i_, AX.X, ALU.max), reads=[mb], writes=[gm])
                tt("dve", sel, sel[:], mb, mb[:], gm, gm[:].unsqueeze(2).to_broadcast([128, 8, 16]), ALU.is_equal)
                stt(mb, mb[:], sel, sel[:], -1e30, mb, mb[:], ALU.mult, ALU.add)
                P.op("dve", lambda e, o_=gm[:], i_=mb[:]: e.tensor_reduce(o_, i_, AX.X, ALU.max), reads=[mb], writes=[gm])
                tt("dve", eq, eq[:], mb, mb[:], gm, gm[:].unsqueeze(2).to_broadcast([128, 8, 16]), ALU.is_equal)
                tt("dve", sel, sel[:], sel, sel[:], eq, eq[:], ALU.add)
                tt("dve", comb, comb[:], sel, sel[:], sc, sc[:], ALU.mult)
                P.op("dve", lambda e, o_=gm[:], i_=comb[:]: e.tensor_reduce(o_, i_, AX.X, ALU.add), reads=[comb], writes=[gm])
                P.op("dve", lambda e, o_=gm[:]: e.reciprocal(o_, o_), reads=[gm], writes=[gm])
                tt("dve", comb, comb[:], comb, comb[:], gm, gm[:].unsqueeze(2).to_broadcast([128, 8, 16]), ALU.mult)
                tap("comb", comb, comb[:], lambda d_: d_[g])
                for a in range(8):
                    ts("pool", y_sub[a], yacc[:, a, :], y_sub[a], yacc[:, a, :], ALPHA, None, ALU.mult)
                for ex in range(16):
                    wgs, wus = [], []
                    for fh in range(2):
                        wgs.append(wload(w_g_d[l, ex].rearrange("(k p) f -> p k f", p=128)[:, :, fh * 256:(fh + 1) * 256]))
                        wus.append(wload(w_u_d[l, ex].rearrange("(k p) f -> p k f", p=128)[:, :, fh * 256:(fh + 1) * 256]))
                    for fh in range(2):
                        wg, wu = wgs[fh], wus[fh]
                        hb = hT[fh]
                        for fc in range(2):
                            for th in range(2):
                                bg_ = banks[(fc * 2 + th) % 2]
                                bu_ = banks[2 + (fc * 2 + th) % 2]
                                for k in range(8):
                                    mm(bg_, bg_[:], wg[0], r32(wg[1][:, k, fc * 128:(fc + 1) * 128]), x1T, r32(x1T[:, k, th * 512:(th + 1) * 512]), k == 0, k == 7)
                                for k in range(8):
                                    mm(bu_, bu_[:], wu[0], r32(wu[1][:, k, fc * 128:(fc + 1) * 128]), x1T, r32(x1T[:, k, th * 512:(th + 1) * 512]), k == 0, k == 7)
                                sgt = sg[(fc * 2 + th) % 2]
                                act(sgt, sgt[:], bg_, bg_[:], AF.Silu)
                                tt("dve", hb, r32(hb[:, fc, th * 512:(th + 1) * 512]), bu_, bu_[:], sgt, sgt[:], ALU.mult)
                    wds = [wload(w_d_d[l, ex, fh * 256:(fh + 1) * 256, :].rearrange("(a p) d -> p a d", p=128)) for fh in range(2)]
                    for a in range(8):
                        for dh in range(2):
                            by = banks[4 + (a * 2 + dh) % 4]
                            i_ = 0
                            for fh in range(2):
                                for fc in range(2):
                                    mm(by, by[:], hT[fh], r32(hT[fh][:, fc, a * 128:(a + 1) * 128]), wds[fh][0], r32(wds[fh][1][:, fc, dh * 512:(dh + 1) * 512]), i_ == 0, i_ == 3)
                                    i_ += 1
                            stt(y_sub[a], yacc[:, a, dh * 512:(dh + 1) * 512], by, by[:], comb[:, a, ex:ex + 1],
                                y_sub[a], yacc[:, a, dh * 512:(dh + 1) * 512], ALU.mult, ALU.add, extra=[comb])
                for a in range(8):
                    layer_norm(P, y_sub[a], yacc[:, a, :], lnts[a], pp, PP_LN2G, PP_LN2B)
                last = (l == L - 1)
                dst_d = out_d if last else xs_d[0]
                tk = P.dma("pool", dst_d[grow0:grow0 + 1024, :].rearrange("(a p) d -> p a d", p=128), yacc[:],
                           reads=[yacc] + y_sub, writes=[DR(("x0", s, 2 * g)), DR(("x0", s, 2 * g + 1))], key="yst_out")
                if last:
                    out_toks.append(tk)
    P.final_wait("sp", out_toks)
    P.emit()
    return st


def layer_norm(P, x_t, x_ap, lnt, pp, gcol, bcol):
    stats = lnt[:, 0:12].rearrange("p (a b) -> p a b", a=2)
    mv = lnt[:, 12:14]
    rstd = lnt[:, 14:15]
    for hf in range(2):
        P.op("dve", lambda e, hf=hf: e.bn_stats(stats[:, hf, :], x_ap[:, hf * 512:(hf + 1) * 512]), reads=[x_t], writes=[lnt])
    P.op("dve", lambda e: e.bn_aggr(mv, lnt[:, 0:12]), reads=[lnt], writes=[lnt])
    P.op("act", lambda e: e.activation(rstd, lnt[:, 13:14], AF.Ln, bias=pp[:, PP_LNEPS:PP_LNEPS + 1]), reads=[lnt, pp], writes=[lnt])
    P.op("act", lambda e: e.activation(rstd, rstd, AF.Exp, scale=-0.5), reads=[lnt], writes=[lnt])
    P.op("dve", lambda e: e.tensor_scalar(x_ap, x_ap, lnt[:, 12:13], rstd, ALU.subtract, ALU.mult), reads=[x_t, lnt], writes=[x_t])
    P.op("dve", lambda e: e.tensor_tensor(x_ap, x_ap, pp[:, gcol:gcol + 1024], ALU.mult), reads=[x_t, pp], writes=[x_t])
    P.op("pool", lambda e: e.tensor_tensor(x_ap, x_ap, pp[:, bcol:bcol + 1024], ALU.add), reads=[x_t, pp], writes=[x_t])


def _pack_inputs(inp):
    L = 4
    pp = np.zeros((L, 128, PP_N), np.float32)
    bd = np.zeros((L, 2, 4, 128, 128), np.float32)
    for l in range(L):
        pp[l, :, PP_CAW:PP_CAW + 16] = inp["conv_a_w"][l].reshape(4, 4, 128).transpose(2, 1, 0).reshape(128, 16)
        pp[l, :, PP_CAB:PP_CAB + 4] = inp["conv_a_b"][l].reshape(4, 128).T
        pp[l, :, PP_RBA:PP_RBA + 4] = inp["rg_b_a"][l].reshape(4, 128).T
        pp[l, :, PP_RBX:PP_RBX + 4] = inp["rg_b_x"][l].reshape(4, 128).T
        pp[l, :, PP_LAM:PP_LAM + 4] = inp["rg_lambda"][l].reshape(4, 128).T
        pp[l, :, PP_GCW:PP_GCW + 48] = inp["gdn_conv_w"][l].reshape(4, 12, 128).transpose(2, 1, 0).reshape(128, 48)
        pp[l, :, PP_GNW] = inp["gdn_norm_w"][l]
        pp[l, :, PP_LNW] = inp["gla_norm_w"][l]
        pp[l, :, PP_DTB:PP_DTB + 4] = inp["gdn_dt_bias"][l][None, :]
        pp[l, :, PP_ALOG:PP_ALOG + 4] = inp["gdn_a_log"][l][None, :]
        pp[l, :, PP_RB:PP_RB + 16] = inp["router_bias"][None, :]
        pp[l, :, PP_GLB:PP_GLB + 512] = inp["gla_b_gate"][l][None, :]
        pp[l, :, PP_LN1G:PP_LN1G + 1024] = inp["ln1_g"][l][None, :]
        pp[l, :, PP_LN1B:PP_LN1B + 1024] = inp["ln1_b"][l][None, :]
        pp[l, :, PP_LN2G:PP_LN2G + 1024] = inp["ln2_g"][l][None, :]
        pp[l, :, PP_LN2B:PP_LN2B + 1024] = inp["ln2_b"][l][None, :]
        pp[l, :, PP_ONE] = 1.0
        pp[l, :, PP_EPS] = NORM_EPS
        pp[l, :, PP_LNEPS] = LN_EPS
        for a, nm in enumerate(("rg_w_a", "rg_w_x")):
            w = inp[nm][l]
            for ch in range(4):
                for gb in range(2):
                    bd[l, a, ch, gb * 64:(gb + 1) * 64, gb * 64:(gb + 1) * 64] = w[ch * 2 + gb]
    return pp, bd


_NC_CACHE = {}


def kernel(**inp):
    inp = {k: np.ascontiguousarray(np.asarray(v)) for k, v in inp.items()}
    n = 8
    nseq = 2
    pp, bd = _pack_inputs(inp)
    consts = host_consts()
    if "nc" not in _NC_CACHE:
        nc = bass.Bass("TRN2", target_bir_lowering=False)
        st = build(nc, L=4, NSEQ=nseq)
        _NC_CACHE["nc"] = (nc, st)
    nc = _NC_CACHE["nc"][0]
    x = inp["x"].reshape(n, nseq * S, D)
    shared = {"w_in": inp["w_in"], "w_branch": inp["w_branch"], "w_out": inp["w_out"], "w_gate": inp["w_gate"],
              "w_up": inp["w_up"], "w_down": inp["w_down"], "w_router": inp["w_router"], "pp": pp, "bd": bd,
              "gla_up": inp["gla_w_gate_up"], "consts": consts}
    in_maps = [dict(shared, x=x[i]) for i in range(n)]
    res = run_bass_kernel_spmd(nc, in_maps, core_ids=list(range(n)))
    out = np.stack([np.asarray(r["out"]) for r in res.results], 0)
    return out.reshape(16, S, D).astype(np.float32)
```

```python
from contextlib import ExitStack
import numpy as np
import concourse.bass as bass
import concourse.mybir as mybir
from concourse.bass_utils import run_bass_kernel_spmd

F32 = mybir.dt.float32
F32R = mybir.dt.float32r
BF16 = mybir.dt.bfloat16
AF = mybir.ActivationFunctionType
ALU = mybir.AluOpType
AX = mybir.AxisListType

STRICT = False
DBG = {}
ENGS = ("pe", "act", "dve", "pool", "sp")
CENG = ("pe", "act", "dve", "pool")


class Res:
    __slots__ = ("name", "w", "rs")

    def __init__(self, name):
        self.name = name
        self.w = None
        self.rs = {}


class Tok:
    __slots__ = ("kind", "eng", "idx", "clock")

    def __init__(self, kind, eng, idx, clock):
        self.kind = kind
        self.eng = eng
        self.idx = idx
        self.clock = clock


class T:
    __slots__ = ("t", "res", "name")

    def __init__(self, t, name):
        self.t = t
        self.res = Res(name)
        self.name = name

    def __getitem__(self, k):
        return self.t[k]


class Prog:
    def __init__(self, nc, stack):
        self.nc = nc
        self.stack = stack
        self.ops = {e: [] for e in ENGS}
        self.known = {e: {} for e in ENGS}
        self.n = {e: 0 for e in ENGS}
        self.last = {}
        self.dcount = {}
        self.needed = {e: set() for e in CENG}

    def sb(self, name, shape, dt=F32):
        t = self.stack.enter_context(self.nc.sbuf_tensor(name, list(shape), dt))
        return T(t, name)

    def ps(self, name, shape, dt=F32):
        t = self.stack.enter_context(self.nc.psum_tensor(name, list(shape), dt))
        return T(t, name)

    def _need(self, eng, known, waits, tok, raw, is_dma):
        if tok is None:
            return
        if tok.kind == "c":
            if tok.eng == eng and not raw and not is_dma and (not STRICT or eng == 'pe'):
                return
            if known.get(tok.eng, 0) >= tok.idx:
                return
            waits.append(("c", tok.eng, tok.idx))
            self.needed[tok.eng].add(tok.idx)
        else:
            if known.get(tok.eng, 0) >= tok.idx:
                return
            waits.append(("d", tok.eng, tok.idx))
        for k, v in tok.clock.items():
            if known.get(k, 0) < v:
                known[k] = v
        known[tok.eng] = tok.idx

    def _wait_list(self, eng, reads, writes, is_dma):
        known = self.known[eng]
        waits = []
        for r in reads:
            self._need(eng, known, waits, r.w, True, is_dma)
        for r in writes:
            self._need(eng, known, waits, r.w, False, is_dma)
            for tk in r.rs.values():
                self._need(eng, known, waits, tk, False, is_dma)
        return waits

    @staticmethod
    def _res(x):
        return x.res if isinstance(x, T) else x

    def op(self, eng, fn, reads=(), writes=()):
        reads = [self._res(r) for r in reads]
        writes = [self._res(r) for r in writes]
        waits = self._wait_list(eng, reads, writes, False)
        self.n[eng] += 1
        idx = self.n[eng]
        clock = {k: v for k, v in self.known[eng].items() if k in CENG}
        tok = Tok("c", eng, idx, clock)
        self.ops[eng].append((waits, fn, ("c", idx)))
        self.last[eng] = tok
        for r in reads:
            r.rs[eng] = tok
        for r in writes:
            r.w = tok
            r.rs = {}
        return tok

    def dma(self, eng, out_ap, in_ap, reads=(), writes=(), key=None):
        reads = [self._res(r) for r in reads]
        writes = [self._res(r) for r in writes]
        dkey = ("d", key)
        waits = self._wait_list(eng, reads, writes, True)
        cnt = self.dcount.get(dkey, 0) + 1
        self.dcount[dkey] = cnt
        clock = {k: v for k, v in self.known[eng].items() if k in CENG}
        tok = Tok("d", dkey, cnt, clock)

        nc = self.nc

        def fn(e, out_ap=out_ap, in_ap=in_ap):
            if out_ap.dtype == F32R:
                nc.dge_precook = False
                r = e.dma_start(out=out_ap, in_=in_ap)
                nc.dge_precook = True
                return r
            return e.dma_start(out=out_ap, in_=in_ap)

        self.ops[eng].append((waits, fn, ("d", dkey)))
        self.last[dkey] = tok
        for r in reads:
            r.rs[dkey] = tok
        for r in writes:
            r.w = tok
            r.rs = {}
        return tok

    def barrier(self, skip=(), skip_keys=()):
        toks = []
        for k, tok in self.last.items():
            if tok.kind == "d" and isinstance(tok.eng[1], tuple) and tok.eng[1][0] in skip_keys:
                continue
            toks.append(tok)
        for eng in ENGS:
            if eng in skip:
                continue
            known = self.known[eng]
            waits = []
            for tok in toks:
                self._need(eng, known, waits, tok, True, True)
            if waits:
                self.ops[eng].append((waits, None, None))

    def final_wait(self, eng, toks):
        known = self.known[eng]
        waits = []
        for tok in toks:
            self._need(eng, known, waits, tok, True, True)
        self.ops[eng].append((waits, None, None))

    def emit(self):
        nc = self.nc
        rank = {}
        for e in CENG:
            s = sorted(self.needed[e])
            rank[e] = {idx: i + 1 for i, idx in enumerate(s)}
            assert len(s) < 60000, (e, len(s))
        sems = {e: self.stack.enter_context(nc.semaphore("sem_" + e)) for e in CENG}
        dsems = {}
        for dkey in self.dcount:
            dsems[dkey] = self.stack.enter_context(nc.semaphore("dsem%d" % len(dsems)))
        block = self.stack.enter_context(nc.Block())

        def run(engname, eng):
            for waits, fn, info in self.ops[engname]:
                for kind, k, idx in waits:
                    if kind == "c":
                        eng.wait_ge(sems[k], rank[k][idx])
                    else:
                        eng.wait_ge(dsems[k], 16 * idx)
                if fn is None:
                    continue
                ins = fn(eng)
                if info[0] == "c":
                    if info[1] in rank[engname]:
                        ins.then_inc(sems[engname], 1)
                else:
                    ins.then_inc(dsems[info[1]], 16)

        @block.tensor
        def _(e):
            run("pe", e)

        @block.scalar
        def _(e):
            run("act", e)

        @block.vector
        def _(e):
            run("dve", e)

        @block.gpsimd
        def _(e):
            run("pool", e)

        @block.sync
        def _(e):
            run("sp", e)


D = 1024
S = 2048
TT = 512
NSUB = 4
D_IN = 10264
C_A = 0
C_BQKV = 512
C_BBETA = 2048
C_BGATE = 2056
C_CQKV = 2568
C_DQKV = 4104
C_DLR = 5640
C_DGATE = 5656
C_MERGE = 6168
ALPHA = 8.0 ** 0.25
LN_EPS = 1e-5
NORM_EPS = 1e-6

PP_CAW = 0
PP_CAB = 16
PP_RBA = 20
PP_RBX = 24
PP_LAM = 28
PP_GCW = 32
PP_GNW = 80
PP_LNW = 81
PP_DTB = 82
PP_ALOG = 86
PP_RB = 90
PP_GLB = 106
PP_LN1G = 618
PP_LN1B = 1642
PP_LN2G = 2666
PP_LN2B = 3690
PP_ONE = 4714
PP_EPS = 4715
PP_LNEPS = 4716
PP_N = 4717

K_ID = 0
K_LE = 128
K_GE = 256
K_GT = 384
K_ONE = 512
K_LE16 = 640
K_GT16 = 768
K_LT = 896
K_SBM = 1024
K_ZL = 1024 + 2048
K_N = 1024 + 2048 + 256


def host_consts():
    p = np.arange(128)[:, None]
    f = np.arange(128)[None, :]
    c = np.zeros((128, K_N), np.float32)
    c[:, K_ID:K_ID + 128] = (p == f)
    c[:, K_LE:K_LE + 128] = (p <= f)
    c[:, K_GE:K_GE + 128] = (p >= f)
    c[:, K_GT:K_GT + 128] = (p > f)
    c[:, K_ONE:K_ONE + 128] = 1.0
    c[:, K_LE16:K_LE16 + 128] = (p <= f) * (-1.0 / 16.0)
    c[:, K_GT16:K_GT16 + 128] = (p > f) * (-1.0 / 16.0)
    c[:, K_LT:K_LT + 128] = (p < f)
    c[:, K_ZL:K_ZL + 128] = (p <= f) * 1e4
    c[:, K_ZL + 128:K_ZL + 256] = (p > f) * 1e4
    f5 = np.arange(512)[None, :]
    for jo in range(4):
        c[:, K_SBM + jo * 512:K_SBM + (jo + 1) * 512] = (jo * 128 + p < f5)
    return c


def build(nc, L=4, NSEQ=2, taps=(), en="ABCD", moe=True):
    NTOK = NSEQ * S
    st = ExitStack()
    P = Prog(nc, st)
    dr = lambda name, shape, kind="ExternalInput": nc.dram_tensor(name, list(shape), F32, kind=kind).ap()
    x_d = dr("x", [NTOK, D])
    w_in_d = dr("w_in", [4, D, D_IN])
    w_br_d = dr("w_branch", [4, 4, 512, D])
    w_out_d = dr("w_out", [4, D, D])
    w_g_d = dr("w_gate", [4, 16, D, 512])
    w_u_d = dr("w_up", [4, 16, D, 512])
    w_d_d = dr("w_down", [4, 16, 512, D])
    w_r_d = dr("w_router", [D, 16])
    pp_d = dr("pp", [4, 128, PP_N])
    bd_d = dr("bd", [4, 2, 4, 128, 128])
    gup_d = dr("gla_up", [4, 16, 512])
    k_d = dr("consts", [128, K_N])
    out_d = dr("out", [NTOK, D], "ExternalOutput")
    xs_d = dr("xs_scr", [2, NTOK, D], "Internal")
    tap_d = {}
    for name, shape in taps:
        tap_d[name] = dr("tap_" + name, shape, "ExternalOutput")
    dres = {}

    def DR(key):
        if key not in dres:
            dres[key] = Res("dram:" + str(key))
        return dres[key]

    out_toks = []

    kc = P.sb("kc", [128, 1280])
    kb = P.sb("kb", [128, 2048], BF16)
    pp = P.sb("pp_sb", [128, PP_N])
    NB = 4
    wring = [P.sb("wr%d" % i, [128, 2048]) for i in range(NB)]
    wri = [0]
    bdw = P.sb("bdw", [128, 8, 128])
    gup = P.sb("gup", [16, 512])
    wrt = P.sb("wrt", [128, 8, 16])
    lam8 = P.sb("lam8", [128, 4])
    aexp = P.sb("aexp", [128, 4])
    nge = P.sb("nge", [128, 128], BF16)
    none_ = P.sb("none", [128, 128], BF16)
    ARENA = 36000 - 12288 - 2048 - 256
    arena = P.sb("arena", [128, ARENA])
    arenaR = P.sb("arenaR", [128, 12288])
    xst = [P.sb("xst0", [128, 1024]), P.sb("xst1", [128, 1024])]
    banks = [P.ps("bank%d" % i, [128, 512]) for i in range(8)]

    aoff = [0]

    live = {"a": [], "r": []}

    def inherit(which, start, end, t_new):
        keep = []
        for (s0, e0, t_old) in live[which]:
            if s0 < end and start < e0:
                toks = list(t_old.res.rs.values())
                if t_old.res.w is not None:
                    toks.append(t_old.res.w)
                for tk in toks:
                    cur = t_new.res.rs.get(tk.eng)
                    if cur is None or cur.idx < tk.idx:
                        t_new.res.rs[tk.eng] = tk
            else:
                keep.append((s0, e0, t_old))
        keep.append((start, end, t_new))
        live[which] = keep

    def carve(name, ncols, shape=None, dt=F32):
        DBG[name] = (aoff[0], ncols)
        ap = arena[:, aoff[0]:aoff[0] + ncols]
        start = aoff[0]
        aoff[0] += ncols
        assert aoff[0] <= ARENA, (name, aoff[0])
        if dt == BF16:
            ap = ap.bitcast(BF16)
        if shape is not None and len(shape) == 3:
            ap = ap.rearrange("p (a b) -> p a b", a=shape[1])
        t_new = T(ap, name)
        inherit("a", start, start + ncols, t_new)
        return t_new

    roff = [0]

    def carveR(name, ncols, shape=None):
        ap = arenaR[:, roff[0]:roff[0] + ncols]
        roff[0] += ncols
        assert roff[0] <= 12288
        if shape is not None and len(shape) == 3:
            ap = ap.rearrange("p (a b) -> p a b", a=shape[1])
        t_new = T(ap, name)
        inherit("r", roff[0] - ncols, roff[0], t_new)
        return t_new

    def r32(ap):
        return ap.bitcast(F32R)

    def v3(ap, a):
        return ap.rearrange("p (a b) -> p a b", a=a)

    def mm(out_t, out_ap, l_t, l_ap, r_t, r_ap, start, stop):
        P.op("pe", lambda e: e.matmul(out_ap, l_ap, r_ap, start=start, stop=stop), reads=[l_t, r_t], writes=[out_t])

    def tr(out_t, out_ap, in_t, in_ap):
        P.op("pe", lambda e: e.transpose(out_ap, in_ap, kc[:, K_ID:K_ID + 128]), reads=[in_t, kc], writes=[out_t])

    def act(out_t, out_ap, in_t, in_ap, func, bias=None, scale=None, extra=()):
        kw = {}
        if bias is not None:
            kw["bias"] = bias
        if scale is not None:
            kw["scale"] = scale
        P.op("act", lambda e: e.activation(out_ap, in_ap, func, **kw), reads=[in_t] + list(extra), writes=[out_t])

    def ts(eng, out_t, out_ap, in_t, in_ap, s1, s2, op0, op1=None, extra=()):
        if op1 is None:
            P.op(eng, lambda e: e.tensor_scalar(out_ap, in_ap, s1, None, op0), reads=[in_t] + list(extra), writes=[out_t])
        else:
            P.op(eng, lambda e: e.tensor_scalar(out_ap, in_ap, s1, s2, op0, op1), reads=[in_t] + list(extra), writes=[out_t])

    def tt(eng, out_t, out_ap, a_t, a_ap, b_t, b_ap, op):
        P.op(eng, lambda e: e.tensor_tensor(out_ap, a_ap, b_ap, op), reads=[a_t, b_t], writes=[out_t])

    def stt(out_t, out_ap, a_t, a_ap, sc, b_t, b_ap, op0, op1, extra=()):
        P.op("dve", lambda e: e.scalar_tensor_tensor(out_ap, a_ap, sc, b_ap, op0, op1),
             reads=[a_t, b_t] + list(extra), writes=[out_t])

    def cp(eng, out_t, out_ap, in_t, in_ap):
        if eng == "act":
            P.op("act", lambda e: e.copy(out_ap, in_ap), reads=[in_t], writes=[out_t])
        else:
            P.op(eng, lambda e: e.tensor_copy(out_ap, in_ap), reads=[in_t], writes=[out_t])

    def memset(eng, out_t, out_ap, val):
        P.op(eng, lambda e: e.memset(out_ap, val), writes=[out_t])

    def wload(src_ap):
        buf = wring[wri[0] % NB]
        wri[0] += 1
        n = 1
        for s_ in src_ap.shape[1:]:
            n *= s_
        dst = buf[:, 0:n]
        if len(src_ap.shape) == 3:
            dst = dst.rearrange("p (a b) -> p a b", a=src_ap.shape[1])
        P.dma("sp", dst.bitcast(F32R), src_ap.bitcast(F32R), writes=[buf], key=("wr", buf.name))
        return buf, dst

    def tap(name, src_t, src_ap, dst_ap_fn):
        if name in tap_d:
            tk = P.dma("pool", dst_ap_fn(tap_d[name]), src_ap, reads=[src_t], key=("tap", name))
            out_toks.append(tk)

    def barrier():
        pass

    P.dma("sp", kc[:, 0:1024], k_d[:, 0:1024], writes=[kc], key="kc")
    P.dma("sp", kc[:, 1024:1280], k_d[:, K_ZL:K_ZL + 256], writes=[kc], key="kc")
    P.dma("sp", arena[:, 0:2048], k_d[:, 1024:3072], writes=[arena], key="kcm")
    cp("dve", kb, kb[:], arena, arena[:, 0:2048])
    P.barrier()
    P.dma("sp", wrt[:], w_r_d.rearrange("(k p) e -> p k e", p=128), writes=[wrt], key="wrt")
    ts("dve", nge, nge[:], kc, kc[:, K_GE:K_GE + 128], -1.0, None, ALU.mult)
    ts("dve", none_, none_[:], kc, kc[:, K_ONE:K_ONE + 128], -1.0, None, ALU.mult)
    ident = kc[:, K_ID:K_ID + 128]
    ones = kc[:, K_ONE:K_ONE + 128]

    def win(l, c0, n):
        return wload(w_in_d[l].rearrange("(k p) n -> p k n", p=128)[:, :, c0:c0 + n])

    for l in range(L):
        barrier()
        P.dma("sp", pp[:], pp_d[l], writes=[pp], key="pp")
        P.dma("sp", bdw[:], bd_d[l].rearrange("a c p j -> p (a c) j"), writes=[bdw], key="bdw")
        P.dma("sp", gup[:], gup_d[l], writes=[gup], key="gup")
        act(lam8, lam8[:], pp, pp[:, PP_LAM:PP_LAM + 4], AF.Exp, scale=-1.0)
        act(lam8, lam8[:], lam8, lam8[:], AF.Ln, bias=pp[:, PP_ONE:PP_ONE + 1], extra=[pp])
        ts("dve", lam8, lam8[:], lam8, lam8[:], -8.0, None, ALU.mult)
        act(aexp, aexp[:], pp, pp[:, PP_ALOG:PP_ALOG + 4], AF.Exp)
        ts("dve", aexp, aexp[:], aexp, aexp[:], -1.0, None, ALU.mult)

        for s in range(NSEQ):
            barrier()
            aoff[0] = 0
            roff[0] = 0
            xT = carveR("xT", 4096, [128, 8, 512])
            merged = carveR("merged", 4096, [128, 8, 512])
            yn = carveR("yn", 2048, [128, 4, 512])
            KT = carve("KT", 4096, [128, 4, 2048], BF16)
            Vc = carve("Vc", 4096, [128, 16, 512], BF16)
            utail = carve("utail", 64, [128, 16, 4])
            hst = carve("hst", 4)
            S_d = [carve("S_d%d" % i_, 128) for i_ in range(4)]
            S_b = [carve("S_b%d" % i_, 128) for i_ in range(4)]
            S_db = [carve("S_db%d" % i_, 64, None, BF16) for i_ in range(4)]
            gsig = carve("gsig", 512)
            tmpm = carve("tmpm", 512)
            OV = aoff[0]
            memset("pool", utail, utail[:], 0.0)
            memset("pool", hst, hst[:], 0.0)
            for i_ in range(4):
                memset("pool", S_d[i_], S_d[i_][:], 0.0)
                memset("pool", S_db[i_], S_db[i_][:], 0.0)
            for i_ in range(4):
                memset("pool", S_b[i_], S_b[i_][:], 0.0)

            for c in range(NSUB):
                row0 = s * S + c * TT

                def proj_fm(bk, wb, wap, j0, ncols):
                    for k in range(8):
                        mm(bk, bk[0:ncols, :], wb, r32(wap[:, k, j0:j0 + ncols]), xT, r32(xT[:, k, :]), k == 0, k == 7)

                def proj_tm(bk, sub, wb, wap, j0, ncols):
                    for k in range(8):
                        mm(bk, bk[:, 0:ncols], xT, r32(xT[:, k, sub * 128:(sub + 1) * 128]), wb, r32(wap[:, k, j0:j0 + ncols]),
                           k == 0, k == 7)

                def branch_merge(n, first):
                    for dq in range(4):
                        wbb, wbap = wload(w_br_d[l, n].rearrange("(k p) n -> p k n", p=128)[:, :, dq * 256:(dq + 1) * 256])
                        wgb, wgap = win(l, C_MERGE + n * 1024 + dq * 256, 256)
                        for h_ in range(2):
                            dch = dq * 2 + h_
                            bp = banks[4 + (dch % 2)]
                            bg = banks[6 + (dch % 2)]
                            for k in range(4):
                                mm(bp, bp[:, :], wbb, r32(wbap[:, k, h_ * 128:(h_ + 1) * 128]), yn, r32(yn[:, k, :]), k == 0, k == 3)
                            proj_fm(bg, wgb, wgap, h_ * 128, 128)
                            act(gsig, gsig[:], bg, bg[:], AF.Sigmoid)
                            if first:
                                tt("dve", merged, r32(merged[:, dch, :]), bp, bp[:], gsig, gsig[:], ALU.mult)
                            else:
                                tt("dve", tmpm, tmpm[:], bp, bp[:], gsig, gsig[:], ALU.mult)
                                tt("pool", merged, r32(merged[:, dch, :]), merged, merged[:, dch, :], tmpm, tmpm[:], ALU.add)

                def gated_norm(o_t, o_ap, h, gate_c0, nw_col, t1, t2, t3, bs=None, bg=None):
                    act(t1, t1[:], o_t, o_ap, AF.Square)
                    bs = banks[6] if bs is None else bs
                    mm(bs, bs[:], kc, ones, t1, t1[:], True, True)
                    act(t2, t2[:], bs, bs[:], AF.Ln, bias=pp[:, PP_EPS:PP_EPS + 1], scale=1.0 / 128.0, extra=[pp])
                    act(t2, t2[:], t2, t2[:], AF.Exp, scale=-0.5)
                    wgb, wgap = win(l, gate_c0 + h * 128, 128)
                    bg = banks[7] if bg is None else bg
                    proj_fm(bg, wgb, wgap, 0, 128)
                    act(t3, t3[:], bg, bg[:], AF.Silu)
                    stt(t1, t1[:], o_t, o_ap, pp[:, nw_col:nw_col + 1], t2, t2[:], ALU.mult, ALU.mult, extra=[pp])
                    tt("dve", yn, r32(yn[:, h, :]), t1, t1[:], t3, t3[:], ALU.mult)

                barrier()
                for a in range(4):
                    xs_ = xst[a % 2]
                    r0 = row0 + a * 128
                    if l == 0:
                        P.dma("sp", r32(xs_[:]), r32(x_d[r0:r0 + 128, :]), writes=[xs_], key=("xst", a % 2))
                    else:
                        P.dma("sp", r32(xs_[:]), r32(xs_d[0, r0:r0 + 128, :]), reads=[DR(("x0", s, c))], writes=[xs_], key=("xst", a % 2))
                    for hf in range(2):
                        bk = banks[hf]
                        for k in range(4):
                            kk = hf * 4 + k
                            tr(bk, bk[:, k * 128:(k + 1) * 128], xs_, xs_[:, kk * 128:(kk + 1) * 128])
                        cp("act" if hf else "dve", xT, r32(xT[:, hf * 4:hf * 4 + 4, a * 128:(a + 1) * 128]), bk, v3(bk[:], 4))

                first = True
                if "A" in en:
                    barrier()
                    aoff[0] = OV
                    UB = [carve("ubuf%d" % i_, 516) for i_ in range(4)]
                    T1 = [carve("t1_%d" % i_, 512) for i_ in range(4)]
                    T2 = [carve("t2_%d" % i_, 512) for i_ in range(4)]
                    T3 = [carve("t3_%d" % i_, 512) for i_ in range(4)]
                    T4 = [carve("t4_%d" % i_, 512) for i_ in range(4)]
                    wa = [win(l, C_A, 256), win(l, C_A + 256, 256)]
                    for ch in range(4):
                        proj_fm(banks[ch], wa[ch // 2][0], wa[ch // 2][1], (ch % 2) * 128, 128)
                    for ch in range(4):
                        ubuf, bk = UB[ch], banks[ch]
                        cp("dve", ubuf, ubuf[:, 0:3], utail, utail[:, ch, 0:3])
                        cp("act", ubuf, ubuf[:, 3:515], bk, bk[:])
                    for ch in range(4):
                        ubuf, t1 = UB[ch], T1[ch]
                        cp("pool", utail, utail[:, ch, 0:3], ubuf, ubuf[:, 512:515])
                        cw = lambda k: pp[:, PP_CAW + ch * 4 + k:PP_CAW + ch * 4 + k + 1]
                        ts("dve", t1, t1[:], ubuf, ubuf[:, 3:515], cw(3), pp[:, PP_CAB + ch:PP_CAB + ch + 1], ALU.mult, ALU.add, extra=[pp])
                        for k in range(3):
                            stt(t1, t1[:], ubuf, ubuf[:, k:k + 512], cw(k), t1, t1[:], ALU.mult, ALU.add, extra=[pp])
                    for ch in range(4):
                        t1 = T1[ch]
                        ba, bx = banks[4 + ch], banks[ch]
                        mm(ba, ba[:], bdw, bdw[:, ch, :], t1, t1[:], True, True)
                        mm(bx, bx[:], bdw, bdw[:, 4 + ch, :], t1, t1[:], True, True)
                    for ch in range(4):
                        act(T2[ch], T2[ch][:], banks[4 + ch], banks[4 + ch][:], AF.Sigmoid, bias=pp[:, PP_RBA + ch:PP_RBA + ch + 1], extra=[pp])
                        act(T3[ch], T3[ch][:], banks[ch], banks[ch][:], AF.Sigmoid, bias=pp[:, PP_RBX + ch:PP_RBX + ch + 1], extra=[pp])
                    for ch in range(4):
                        act(T2[ch], T2[ch][:], T2[ch], T2[ch][:], AF.Exp, scale=lam8[:, ch:ch + 1], extra=[lam8])
                    for ch in range(4):
                        tt("dve", T4[ch], T4[ch][:], T2[ch], T2[ch][:], T2[ch], T2[ch][:], ALU.mult)
                        tt("dve", T3[ch], T3[ch][:], T3[ch], T3[ch][:], T1[ch], T1[ch][:], ALU.mult)
                    for ch in range(4):
                        act(T4[ch], T4[ch][:], T4[ch], T4[ch][:], AF.Sqrt, bias=pp[:, PP_ONE:PP_ONE + 1], scale=-1.0, extra=[pp])
                    for ch in range(4):
                        t2, t3, t4 = T2[ch], T3[ch], T4[ch]
                        if c == 0:
                            memset("dve", t4, t4[:, 0:1], 1.0)
                        tt("dve", t3, t3[:], t3, t3[:], t4, t4[:], ALU.mult)
                        P.op("dve", lambda e, o_=r32(yn[:, ch, :]), a_=t2[:], b_=t3[:], i_=hst[:, ch:ch + 1]: e.tensor_tensor_scan(o_, a_, b_, i_, ALU.mult, ALU.add),
                             reads=[t2, t3, hst], writes=[yn])
                        cp("pool", hst, hst[:, ch:ch + 1], yn, yn[:, ch, 511:512])
                    tap("y_a", yn, yn[:], lambda d_: d_[:, :, c * TT:(c + 1) * TT])
                    branch_merge(0, first)
                    first = False

                if "B" in en:
                    barrier()
                    aoff[0] = OV
                    qnT = carve("qnT", 2048, [128, 4, 512])
                    knT = carve("knT", 2048, [128, 4, 512])
                    vT = carve("vT", 2048, [128, 4, 512])
                    ubuf = carve("ubuf", 516)
                    t1 = carve("t1", 512)
                    t2 = carve("t2", 512)
                    t3 = carve("t3", 512)
                    beta_t = carve("beta_t", 16)
                    g_t = carve("g_t", 16)
                    gcc = carve("gcc", 16)
                    bexp = carve("bexp", 16)
                    kdsc = carve("kdsc", 16)
                    gl = carve("gl", 16)
                    tsm = carve("tsm", 16)
                    dests = [qnT, knT, vT]
                    for un in range(6):
                        wq = win(l, C_BQKV + un * 256, 256)
                        for h_ in range(2):
                            ci = un * 2 + h_
                            bk = banks[ci % 2]
                            proj_fm(bk, wq[0], wq[1], h_ * 128, 128)
                            cp("dve", ubuf, ubuf[:, 0:3], utail, utail[:, 4 + ci, 0:3])
                            cp("act", ubuf, ubuf[:, 3:515], bk, bk[:])
                            cp("pool", utail, utail[:, 4 + ci, 0:3], ubuf, ubuf[:, 512:515])
                            cw = lambda k: pp[:, PP_GCW + ci * 4 + k:PP_GCW + ci * 4 + k + 1]
                            ts("dve", t1, t1[:], ubuf, ubuf[:, 3:515], cw(3), None, ALU.mult, extra=[pp])
                            for k in range(3):
                                stt(t1, t1[:], ubuf, ubuf[:, k:k + 512], cw(k), t1, t1[:], ALU.mult, ALU.add, extra=[pp])
                            dst = dests[ci // 4]
                            act(dst, dst[:, ci % 4, :], t1, t1[:], AF.Silu)
                    for qi, dst in enumerate((qnT, knT)):
                        for h in range(4):
                            act(t1, t1[:], dst, dst[:, h, :], AF.Square)
                            bs = banks[2 + h % 2]
                            mm(bs, bs[:], kc, ones, t1, t1[:], True, True)
                            act(t2, t2[:], bs, bs[:], AF.Ln, bias=pp[:, PP_EPS:PP_EPS + 1], extra=[pp])
                            act(t2, t2[:], t2, t2[:], AF.Exp, scale=-0.5)
                            if qi == 0:
                                stt(dst, dst[:, h, :], dst, dst[:, h, :], 128.0 ** -0.5, t2, t2[:], ALU.mult, ALU.mult)
                            else:
                                tt("dve", dst, dst[:, h, :], dst, dst[:, h, :], t2, t2[:], ALU.mult)
                    wbg = win(l, C_BBETA, 8)
                    bsm = banks[4]
                    for sub in range(4):
                        for k in range(8):
                            mm(bsm, bsm[:, sub * 8:(sub + 1) * 8], xT, r32(xT[:, k, sub * 128:(sub + 1) * 128]), wbg[0], r32(wbg[1][:, k, 0:8]), k == 0, k == 7)
                    bsm3 = v3(bsm[:, 0:32], 4)
                    b3 = lambda t_: v3(t_[:], 4)
                    act(beta_t, b3(beta_t), bsm, bsm3[:, :, 0:4], AF.Sigmoid)
                    cp("act", tsm, b3(tsm), bsm, bsm3[:, :, 4:8])
                    tt("dve", tsm, b3(tsm), tsm, b3(tsm), pp, pp[:, PP_DTB:PP_DTB + 4].unsqueeze(1).to_broadcast([128, 4, 4]), ALU.add)
                    act(tsm, tsm[:], tsm, tsm[:], AF.Exp)
                    act(tsm, tsm[:], tsm, tsm[:], AF.Ln, bias=pp[:, PP_ONE:PP_ONE + 1], extra=[pp])
                    tt("dve", g_t, b3(g_t), tsm, b3(tsm), aexp, aexp[:].unsqueeze(1).to_broadcast([128, 4, 4]), ALU.mult)
                    bsm2 = banks[5]
                    for sub in range(4):
                        mm(bsm2, bsm2[:, sub * 4:(sub + 1) * 4], kc, kc[:, K_LE:K_LE + 128], g_t, g_t[:, sub * 4:(sub + 1) * 4], True, True)
                    for sub in range(4):
                        mm(bsm2, bsm2[:, 16 + sub * 4:16 + (sub + 1) * 4], kc, ones, g_t, g_t[:, sub * 4:(sub + 1) * 4], True, True)
                    cp("act", gcc, gcc[:], bsm2, bsm2[:, 0:16])
                    act(bexp, bexp[:], bsm2, bsm2[:, 0:16], AF.Exp)
                    tt("dve", bexp, bexp[:], bexp, bexp[:], beta_t, beta_t[:], ALU.mult)
                    act(gl, gl[:], bsm2, bsm2[:, 16:32], AF.Exp)
                    cp("act", kdsc, kdsc[:], bsm2, bsm2[:, 16:32])
                    tt("dve", kdsc, kdsc[:], kdsc, kdsc[:], gcc, gcc[:], ALU.subtract)
                    act(kdsc, kdsc[:], kdsc, kdsc[:], AF.Exp)
                    TOPB = aoff[0]
                    names = ("tg", "tmp1", "tmp2", "qdec", "PT", "attn_s", "kbg", "kdec", "vb", "u_s", "wT_s", "vn_s")

                    def mk_tmp(tagc):
                        d = {n_: carve(n_ + tagc, 128) for n_ in names}
                        d["AAT"] = [carve("AAT0" + tagc, 256), carve("AAT1" + tagc, 256)]
                        return d

                    def chain(h, sub, tm, bA, bB, bC, bo):
                        cs = slice(sub * 128, (sub + 1) * 128)
                        si = sub * 4 + h
                        tg, tmp1, tmp2, qdec, PT, attn_s = tm["tg"], tm["tmp1"], tm["tmp2"], tm["qdec"], tm["PT"], tm["attn_s"]
                        kbg, kdec, vb, u_s, wT_s, vn_s, AAT = tm["kbg"], tm["kdec"], tm["vb"], tm["u_s"], tm["wT_s"], tm["vn_s"], tm["AAT"]
                        ts("dve", tg, tg[:], kc, kc[:, K_LE:K_LE + 128], g_t[:, si:si + 1], None, ALU.mult, extra=[g_t])
                        mm(bA, bA[:, 0:128], kc, ones, tg, tg[:], True, True)
                        mm(bA, bA[:, 128:256], knT, knT[:, h, cs], knT, knT[:, h, cs], True, True)
                        mm(bA, bA[:, 256:384], knT, knT[:, h, cs], qnT, qnT[:, h, cs], True, True)
                        mm(bA, bA[:, 384:512], knT, knT[:, h, cs], kc, ident, True, True)
                        mm(bB, bB[:, 0:128], vT, vT[:, h, cs], kc, ident, True, True)
                        yield
                        stt(tmp1, tmp1[:], bA, bA[:, 0:128], gcc[:, si:si + 1], kc, kc[:, 1152:1280], ALU.subtract, ALU.subtract, extra=[gcc])
                        stt(tmp2, tmp2[:], bA, bA[:, 0:128], gcc[:, si:si + 1], kc, kc[:, 1024:1152], ALU.subtract, ALU.add, extra=[gcc])
                        act(tmp1, tmp1[:], tmp1, tmp1[:], AF.Exp)
                        act(tmp2, tmp2[:], tmp2, tmp2[:], AF.Exp, scale=-1.0)
                        act(tg, tg[:], bA, bA[:, 0:128], AF.Exp)
                        yield
                        tt("dve", qdec, qdec[:], qnT, qnT[:, h, cs], tg, tg[:], ALU.mult)
                        stt(AAT[0], AAT[0][:, 0:128], bA, bA[:, 128:256], beta_t[:, si:si + 1], tmp2, tmp2[:], ALU.mult, ALU.mult, extra=[beta_t])
                        tt("dve", attn_s, attn_s[:], bA, bA[:, 256:384], tmp1, tmp1[:], ALU.mult)
                        ts("dve", kbg, kbg[:], bA, bA[:, 384:512], bexp[:, si:si + 1], None, ALU.mult, extra=[bexp])
                        ts("dve", kdec, kdec[:], bA, bA[:, 384:512], kdsc[:, si:si + 1], None, ALU.mult, extra=[kdsc])
                        ts("dve", vb, vb[:], bB, bB[:, 0:128], beta_t[:, si:si + 1], None, ALU.mult, extra=[beta_t])
                        mm(bB, bB[:, 128:256], AAT[0], AAT[0][:, 0:128], kc, ident, True, True)
                        yield
                        cp("act", AAT[0], AAT[0][:, 128:256], bB, bB[:, 128:256])
                        act(PT, PT[:], bB, bB[:, 128:256], AF.Copy, scale=-1.0)
                        tt("dve", PT, PT[:], PT, PT[:], kc, ident, ALU.add)
                        cur = 0
                        for m in range(1, 7):
                            A_c = AAT[cur]
                            A_n = AAT[1 - cur]
                            mm(bC, bC[:, 0:128], A_c, A_c[:, 128:256], A_c, A_c[:, 0:128], True, True)
                            if m < 6:
                                mm(bC, bC[:, 128:256], A_c, A_c[:, 0:128], A_c, A_c[:, 128:256], True, True)
                            yield
                            if m < 6:
                                cp("act", A_n, A_n[:, 0:256], bC, bC[:, 0:256])
                            else:
                                cp("act", A_n, A_n[:, 0:128], bC, bC[:, 0:128])
                            mm(bC, bC[:, 256:384], A_n, A_n[:, 0:128], PT, PT[:], True, True)
                            yield
                            tt("dve", PT, PT[:], PT, PT[:], bC, bC[:, 256:384], ALU.add)
                            cur = 1 - cur
                        mm(bB, bB[:, 256:384], PT, PT[:], vb, vb[:], True, True)
                        mm(bB, bB[:, 384:512], kbg, kbg[:], PT, PT[:], True, True)
                        yield
                        cp("act", u_s, u_s[:], bB, bB[:, 256:384])
                        cp("act", wT_s, wT_s[:], bB, bB[:, 384:512])
                        mm(bC, bC[:, 384:512], wT_s, wT_s[:], S_b[h], S_b[h][:], True, True)
                        yield
                        tt("dve", vn_s, vn_s[:], u_s, u_s[:], bC, bC[:, 384:512], ALU.subtract)
                        mm(bo, bo[:, cs], S_b[h], S_b[h][:], qdec, qdec[:], True, False)
                        mm(bo, bo[:, cs], vn_s, vn_s[:], attn_s, attn_s[:], False, True)
                        mm(bC, bC[:, 0:128], kdec, kdec[:], vn_s, vn_s[:], True, True)
                        yield
                        stt(S_b[h], S_b[h][:], S_b[h], S_b[h][:], gl[:, si:si + 1], bC, bC[:, 0:128], ALU.mult, ALU.add, extra=[gl])
                        yield

                    for hp in range(2):
                        aoff[0] = OV + 6144
                        tm1 = mk_tmp("_b")
                        aoff[0] = max(aoff[0], TOPB)
                        tm0 = mk_tmp("_a")
                        tms = (tm0, tm1)
                        for sub in range(4):
                            gens = []
                            for j in range(2):
                                h = 2 * hp + j
                                gens.append(chain(h, sub, tms[j], banks[4 * j], banks[4 * j + 1], banks[4 * j + 2], banks[4 * j + 3]))
                            alive = list(gens)
                            while alive:
                                nxt = []
                                for g_ in alive:
                                    try:
                                        next(g_)
                                        nxt.append(g_)
                                    except StopIteration:
                                        pass
                                alive = nxt
                        aoff[0] = OV + 6144
                        ubuf = carve("ubuf", 516)
                        t1 = carve("t1", 512)
                        t2 = carve("t2", 512)
                        t3 = carve("t3", 512)
                        for j in range(2):
                            h = 2 * hp + j
                            gated_norm(banks[4 * j + 3], banks[4 * j + 3][:], h, C_BGATE, PP_GNW, t1, t2, t3, bs=banks[4 * j], bg=banks[4 * j + 1])
                    tap("y_b", yn, yn[:], lambda d_: d_[:, :, c * TT:(c + 1) * TT])
                    branch_merge(1, first)
                    first = False

                if "C" in en:
                    barrier()
                    aoff[0] = OV
                    qTb = carve("qTb", 2048, [128, 8, 512], BF16)
                    memset("pool", qTb, qTb[:], 0.0)
                    e_sb = [carve("e_sb%d" % i_, 512) for i_ in range(4)]
                    sp_sb = [carve("sp_sb%d" % i_, 256, None, BF16) for i_ in range(4)]
                    spsum = [carve("spsum%d" % i_, 512) for i_ in range(4)]
                    spsum_b = [carve("spsum_b%d" % i_, 256, None, BF16) for i_ in range(4)]
                    w_sb = [carve("w_sb%d" % i_, 256, None, BF16) for i_ in range(4)]
                    qb0 = c * 4
                    for u in range(2):
                        wq = win(l, C_CQKV + u * 256, 256)
                        wk = win(l, C_CQKV + 512 + u * 256, 256)
                        for h_ in range(2):
                            pr = u * 2 + h_
                            bq = banks[0 + h_]
                            proj_fm(bq, wq[0], wq[1], h_ * 128, 128)
                            ts("dve", qTb, qTb[0:64, 2 * pr, :], bq, bq[0:64, :], 0.125, None, ALU.mult)
                            ts("dve", qTb, qTb[64:128, 2 * pr + 1, :], bq, bq[64:128, :], 0.125, None, ALU.mult)
                            bk_ = banks[2 + h_]
                            proj_fm(bk_, wk[0], wk[1], h_ * 128, 128)
                            cp("act", KT, KT[:, pr, c * TT:(c + 1) * TT], bk_, bk_[:])
                    wv0 = win(l, C_CQKV + 1024, 256)
                    wv1 = win(l, C_CQKV + 1280, 256)
                    for sub in range(4):
                        bv = banks[sub % 2]
                        proj_tm(bv, sub, wv0[0], wv0[1], 0, 256)
                        cp("act", Vc, Vc[:, qb0 + sub, 0:256], bv, bv[:, 0:256])
                        bv2 = banks[2 + sub % 2]
                        proj_tm(bv2, sub, wv1[0], wv1[1], 0, 256)
                        cp("dve", Vc, Vc[:, qb0 + sub, 256:512], bv2, bv2[:, 0:256])
                    nkb = qb0 + 4
                    for pg in range(2):
                        for j in range(4):
                            memset("pool", spsum[j], spsum[j][:], 0.0)
                            memset("pool", spsum_b[j], spsum_b[j][:], 0.0)
                        for step, J in enumerate(range(nkb - 1, -1, -1)):
                            jo = J - qb0
                            q0 = max(jo, 0) * 128
                            qs = slice(q0, 512)
                            dg = slice(q0, q0 + 128)
                            for j in range(4):
                                h = 4 * pg + j
                                bzd = banks[j]
                                mm(bzd, bzd[:, qs], KT, KT[:, h // 2, J * 128:(J + 1) * 128], qTb, qTb[:, h, qs], True, False)
                            for j in range(4):
                                bzd = banks[j]
                                e_h, sp_h = e_sb[j], sp_sb[j]
                                act(e_h, e_h[:, qs], bzd, bzd[:, qs], AF.Exp)
                                act(sp_h, sp_h[:, qs], e_h, e_h[:, qs], AF.Ln, bias=pp[:, PP_ONE:PP_ONE + 1], extra=[pp])
                                if jo >= 0:
                                    tt("dve", sp_h, sp_h[:, dg], sp_h, sp_h[:, dg], kb, kb[:, 0:128], ALU.mult)
                            for j in range(4):
                                bzd = banks[j]
                                sp_h, sub_h = sp_sb[j], spsum_b[j]
                                mm(bzd, bzd[:, qs], nge, nge[:], sp_h, sp_h[:, qs], False, step == 0)
                                if step > 0:
                                    mm(bzd, bzd[:, qs], none_, none_[:], sub_h, sub_h[:, qs], False, True)
                            for j in range(4):
                                bzd = banks[j]
                                sp_h, su_h, sub_h, w_h = sp_sb[j], spsum[j], spsum_b[j], w_sb[j]
                                act(w_h, w_h[:, qs], bzd, bzd[:, qs], AF.Exp)
                                if jo >= 0:
                                    tt("dve", w_h, w_h[:, dg], w_h, w_h[:, dg], kb, kb[:, 0:128], ALU.mult)
                                if J > 0:
                                    tt("dve", su_h, su_h[:, qs], su_h, su_h[:, qs], sp_h, sp_h[:, qs], ALU.add)
                                    cp("dve", sub_h, sub_h[:, qs], su_h, su_h[:, qs])
                            for j in range(4):
                                h = 4 * pg + j
                                bo = banks[4 + j]
                                w_h = w_sb[j]
                                mm(bo, bo[:, qs], Vc, Vc[:, J, (h // 2) * 128:(h // 2 + 1) * 128], w_h, w_h[:, qs], step == 0, J == 0)
                        for j in range(4):
                            h = 4 * pg + j
                            pr = h // 2
                            hh = h % 2
                            ps_ = slice(hh * 64, hh * 64 + 64)
                            cp("act", yn, r32(yn[ps_, pr, :]), banks[4 + j], banks[4 + j][ps_, :])
                    tap("y_c", yn, yn[:], lambda d_: d_[:, :, c * TT:(c + 1) * TT])
                    branch_merge(2, first)
                    first = False

                if "D" in en:
                    barrier()
                    aoff[0] = OV
                    t1 = carve("t1", 512)
                    t2 = carve("t2", 512)
                    t3 = carve("t3", 512)
                    sp_tok = carve("sp_tok", 2048, [128, 4, 512])
                    qd = carve("qd", 1024, [128, 4, 512], BF16)
                    ki = carve("ki", 1024, [128, 4, 512], BF16)
                    kdt = carve("kdt", 1024, [128, 4, 512], BF16)
                    vtk = carve("vtk", 1024, [128, 4, 512], BF16)
                    lrT = carve("lrT", 512)
                    glast = carve("glast", 16)
                    attn = [carve("attn%d" % i_, 64, None, BF16) for i_ in range(4)]
                    wl = win(l, C_DLR, 16)
                    b0 = banks[0]
                    proj_fm(b0, wl[0], wl[1], 0, 16)
                    cp("act", lrT, lrT[0:16, :], b0, b0[0:16, :])
                    for sub in range(4):
                        bg_ = banks[1 + sub % 2]
                        mm(bg_, bg_[:], lrT, lrT[0:16, sub * 128:(sub + 1) * 128], gup, gup[:], True, True)
                        tt("dve", t1, t1[:], bg_, bg_[:], pp, pp[:, PP_GLB:PP_GLB + 512], ALU.add)
                        act(t1, t1[:], t1, t1[:], AF.Exp, scale=-1.0)
                        act(sp_tok, sp_tok[:, sub, :], t1, t1[:], AF.Ln, bias=pp[:, PP_ONE:PP_ONE + 1], extra=[pp])
                    wk0 = win(l, C_DQKV + 512, 256)
                    wk1 = win(l, C_DQKV + 768, 256)
                    for sub in range(4):
                        br = banks[3]
                        mm(br, br[:], kc, kc[:, K_GT16:K_GT16 + 128], sp_tok, sp_tok[:, sub, :], True, True)
                        act(t2, t2[:], br, br[:], AF.Exp)
                        for hf, wk_ in enumerate((wk0, wk1)):
                            bkk = banks[4 + hf]
                            proj_tm(bkk, sub, wk_[0], wk_[1], 0, 256)
                            tt("dve", kdt, kdt[:, sub, hf * 256:(hf + 1) * 256], bkk, bkk[:, 0:256], t2, t2[:, hf * 256:(hf + 1) * 256], ALU.mult)
                    wv0 = win(l, C_DQKV + 1024, 256)
                    wv1 = win(l, C_DQKV + 1280, 256)
                    for sub in range(4):
                        for hf, wvv in enumerate((wv0, wv1)):
                            bvv = banks[6 + hf]
                            proj_tm(bvv, sub, wvv[0], wvv[1], 0, 256)
                            cp("act", vtk, vtk[:, sub, hf * 256:(hf + 1) * 256], bvv, bvv[:, 0:256])
                    for h in range(4):
                        bb = banks[0]
                        for sub in range(4):
                            mm(bb, bb[:, sub * 128:(sub + 1) * 128], sp_tok, sp_tok[:, sub, h * 128:(h + 1) * 128],
                               kc, kc[:, K_LE16:K_LE16 + 128], True, True)
                        act(t1, t1[:], bb, bb[:], AF.Exp)
                        act(t2, t2[:], bb, bb[:], AF.Exp, scale=-1.0)
                        for sub in range(4):
                            cp("pool", glast, glast[:, h * 4 + sub:h * 4 + sub + 1], t1, t1[:, sub * 128 + 127:sub * 128 + 128])
                        wq = win(l, C_DQKV + h * 128, 128)
                        bq = banks[1]
                        proj_fm(bq, wq[0], wq[1], 0, 128)
                        stt(qd, qd[:, h, :], bq, bq[:], 128.0 ** -0.5, t1, t1[:], ALU.mult, ALU.mult)
                        wkf = win(l, C_DQKV + 512 + h * 128, 128)
                        bk2 = banks[2]
                        proj_fm(bk2, wkf[0], wkf[1], 0, 128)
                        tt("dve", ki, ki[:, h, :], bk2, bk2[:], t2, t2[:], ALU.mult)
                    for sub in range(4):
                        cs = slice(sub * 128, (sub + 1) * 128)
                        for h in range(4):
                            ba_ = banks[4 + h]
                            mm(ba_, ba_[:, 0:128], ki, ki[:, h, cs], qd, qd[:, h, cs], True, True)
                        for h in range(4):
                            ba_ = banks[4 + h]
                            tt("dve", attn[h], attn[h][:], ba_, ba_[:, 0:128], kc, kc[:, K_LE:K_LE + 128], ALU.mult)
                        for h in range(4):
                            bo = banks[h]
                            hs = slice(h * 128, (h + 1) * 128)
                            mm(bo, bo[:, cs], vtk, vtk[:, sub, hs], attn[h], attn[h][:], True, False)
                            mm(bo, bo[:, cs], S_db[h], S_db[h][:], qd, qd[:, h, cs], False, True)
                            bs_ = banks[4 + h]
                            mm(bs_, bs_[:, 128:256], kdt, kdt[:, sub, hs], vtk, vtk[:, sub, hs], True, True)
                        for h in range(4):
                            bs_ = banks[4 + h]
                            stt(S_d[h], S_d[h][:], S_d[h], S_d[h][:], glast[:, h * 4 + sub:h * 4 + sub + 1], bs_, bs_[:, 128:256],
                                ALU.mult, ALU.add, extra=[glast])
                            cp("pool", S_db[h], S_db[h][:], S_d[h], S_d[h][:])
                    for h in range(4):
                        gated_norm(banks[h], banks[h][:], h, C_DGATE, PP_LNW, t1, t2, t3)
                    tap("y_d", yn, yn[:], lambda d_: d_[:, :, c * TT:(c + 1) * TT])
                    branch_merge(3, first)
                    first = False

                barrier()
                aoff[0] = OV
                x_tok = carve("x_tok", 4096, [128, 4, 1024])
                x_sub = [T(x_tok[:, i_, :], "x_sub%d" % i_) for i_ in range(4)]
                lnts = [carve("lnt%d" % i_, 64) for i_ in range(4)]
                src = (x_d if l == 0 else xs_d[0])[row0:row0 + TT, :]
                P.dma("pool", x_tok[:], src.rearrange("(a p) d -> p a d", p=128),
                      reads=([DR(("x0", s, c))] if l > 0 else []), writes=[x_tok] + x_sub, key="xtok")
                for dq in range(4):
                    wo = wload(w_out_d[l].rearrange("(k p) n -> p k n", p=128)[:, :, dq * 256:(dq + 1) * 256])
                    for sub in range(4):
                        bo = banks[sub % 4]
                        for k in range(8):
                            mm(bo, bo[:, 0:256], merged, r32(merged[:, k, sub * 128:(sub + 1) * 128]), wo[0], r32(wo[1][:, k, :]), k == 0, k == 7)
                        stt(x_sub[sub], x_tok[:, sub, dq * 256:(dq + 1) * 256], x_sub[sub], x_tok[:, sub, dq * 256:(dq + 1) * 256], ALPHA,
                            bo, bo[:, 0:256], ALU.mult, ALU.add)
                layer_norm_multi(P, [(x_sub[sub], x_tok[:, sub, :], lnts[sub]) for sub in range(4)], pp, PP_LN1G, PP_LN1B)
                if "x1" in tap_d:
                    tk = P.dma("pool", tap_d["x1"][c * TT:(c + 1) * TT, :].rearrange("(a p) d -> p a d", p=128), x_tok[:], reads=[x_tok] + x_sub, key=("tap", "x1"))
                    out_toks.append(tk)
                dst_d = xs_d[1] if moe else (out_d if l == L - 1 else xs_d[0])
                tk = P.dma("pool", dst_d[row0:row0 + TT, :].rearrange("(a p) d -> p a d", p=128), x_tok[:],
                           reads=[x_tok] + x_sub, writes=[DR((("x1" if moe else "x0"), s, c))], key="xst_out")
                if not moe and l == L - 1:
                    out_toks.append(tk)

            if not moe:
                continue
            for g in range(2):
                barrier()
                aoff[0] = 0
                roff[0] = 0
                x1T = carveR("x1T", 8192, [128, 8, 1024])
                hT = [carveR("hT0", 2048, [128, 2, 1024]), carveR("hT1", 2048, [128, 2, 1024])]
                yacc = carve("yacc", 8192, [128, 8, 1024])
                y_sub = [T(yacc[:, i_, :], "y_sub%d" % i_) for i_ in range(8)]
                sg = [carve("sg0", 512), carve("sg1", 512)]
                sc = carve("sc", 128, [128, 8, 16])
                bi = carve("bi", 128, [128, 8, 16])
                mb = carve("mb", 128, [128, 8, 16])
                eq = carve("eq", 128, [128, 8, 16])
                sel = carve("sel", 128, [128, 8, 16])
                comb = carve("comb", 128, [128, 8, 16])
                m1 = carve("m1", 32)
                m2 = carve("m2", 32)
                gsel = carve("gsel", 32)
                gm = carve("gm", 8)
                lnts = [carve("lnt%d" % i_, 64) for i_ in range(8)]
                grow0 = s * S + g * 1024
                P.dma("pool", yacc[:], xs_d[1, grow0:grow0 + 1024, :].rearrange("(a p) d -> p a d", p=128),
                      reads=[DR(("x1", s, 2 * g)), DR(("x1", s, 2 * g + 1))], writes=[yacc] + y_sub, key="yacc")
                for a in range(8):
                    xs_ = xst[a % 2]
                    P.dma("sp", r32(xs_[:]), r32(xs_d[1, grow0 + a * 128:grow0 + (a + 1) * 128, :]),
                          reads=[DR(("x1", s, 2 * g)), DR(("x1", s, 2 * g + 1))], writes=[xs_], key=("xst", a % 2))
                    for hf in range(2):
                        bk = banks[hf]
                        for k in range(4):
                            kk = hf * 4 + k
                            tr(bk, bk[:, k * 128:(k + 1) * 128], xs_, xs_[:, kk * 128:(kk + 1) * 128])
                        cp("act" if hf else "dve", x1T, r32(x1T[:, hf * 4:hf * 4 + 4, a * 128:(a + 1) * 128]), bk, v3(bk[:], 4))
                brt = banks[2]
                for a in range(8):
                    for k in range(8):
                        mm(brt, brt[:, a * 16:(a + 1) * 16], x1T, x1T[:, k, a * 128:(a + 1) * 128], wrt, wrt[:, k, :], k == 0, k == 7)
                act(sc, sc[:], brt, v3(brt[:, 0:128], 8), AF.Sigmoid)
                rb_b = pp[:, PP_RB:PP_RB + 16].unsqueeze(1).to_broadcast([128, 8, 16])
                tt("dve", bi, bi[:], sc, sc[:], pp, rb_b, ALU.add)
                bi4 = bi[:].rearrange("p a (g e) -> p (a g) e", g=4)
                mb4 = mb[:].rearrange("p a (g e) -> p (a g) e", g=4)
                eq4 = eq[:].rearrange("p a (g e) -> p (a g) e", g=4)
                P.op("dve", lambda e, o_=m1[:], i_=bi4: e.tensor_reduce(o_, i_, AX.X, ALU.max), reads=[bi], writes=[m1])
                tt("dve", eq, eq4, bi, bi4, m1, m1[:].unsqueeze(2).to_broadcast([128, 32, 4]), ALU.is_equal)
                stt(mb, mb4, eq, eq4, -1e30, bi, bi4, ALU.mult, ALU.add)
                P.op("dve", lambda e, o_=m2[:], i_=mb4: e.tensor_reduce(o_, i_, AX.X, ALU.max), reads=[mb], writes=[m2])
                tt("dve", m1, m1[:], m1, m1[:], m2, m2[:], ALU.add)
                m1g = m1[:].rearrange("p (a g) -> p a g", g=4)
                P.op("dve", lambda e, o_=gm[:], i_=m1g: e.tensor_reduce(o_, i_, AX.X, ALU.max), reads=[m1], writes=[gm])
                tt("dve", gsel, gsel[:].rearrange("p (a g) -> p a g", g=4), m1, m1g, gm, gm[:].unsqueeze(2).to_broadcast([128, 8, 4]), ALU.is_equal)
                ts("dve", gsel, gsel[:], gsel, gsel[:], -1.0, 1e30, ALU.add, ALU.mult)
                tt("dve", mb, mb4, bi, bi4, gsel, gsel[:].unsqueeze(2).to_broadcast([128, 32, 4]), ALU.add)
                P.op("dve", lambda e, o_=gm[:], i_=mb[:]: e.tensor_reduce(o_, i_, AX.X, ALU.max), reads=[mb], writes=[gm])
                tt("dve", sel, sel[:], mb, mb[:], gm, gm[:].unsqueeze(2).to_broadcast([128, 8, 16]), ALU.is_equal)
                stt(mb, mb[:], sel, sel[:], -1e30, mb, mb[:], ALU.mult, ALU.add)
                P.op("dve", lambda e, o_=gm[:], i_=mb[:]: e.tensor_reduce(o_, i_, AX.X, ALU.max), reads=[mb], writes=[gm])
                tt("dve", eq, eq[:], mb, mb[:], gm, gm[:].unsqueeze(2).to_broadcast([128, 8, 16]), ALU.is_equal)
                tt("dve", sel, sel[:], sel, sel[:], eq, eq[:], ALU.add)
                tt("dve", comb, comb[:], sel, sel[:], sc, sc[:], ALU.mult)
                P.op("dve", lambda e, o_=gm[:], i_=comb[:]: e.tensor_reduce(o_, i_, AX.X, ALU.add), reads=[comb], writes=[gm])
                P.op("dve", lambda e, o_=gm[:]: e.reciprocal(o_, o_), reads=[gm], writes=[gm])
                tt("dve", comb, comb[:], comb, comb[:], gm, gm[:].unsqueeze(2).to_broadcast([128, 8, 16]), ALU.mult)
                tap("comb", comb, comb[:], lambda d_: d_[g])
                for a in range(8):
                    ts("pool", y_sub[a], yacc[:, a, :], y_sub[a], yacc[:, a, :], ALPHA, None, ALU.mult)
                for ex in range(16):
                    wgs, wus = [], []
                    for fh in range(2):
                        wgs.append(wload(w_g_d[l, ex].rearrange("(k p) f -> p k f", p=128)[:, :, fh * 256:(fh + 1) * 256]))
                        wus.append(wload(w_u_d[l, ex].rearrange("(k p) f -> p k f", p=128)[:, :, fh * 256:(fh + 1) * 256]))
                    for fh in range(2):
                        wg, wu = wgs[fh], wus[fh]
                        hb = hT[fh]
                        for fc in range(2):
                            for th in range(2):
                                bg_ = banks[(fc * 2 + th) % 2]
                                bu_ = banks[2 + (fc * 2 + th) % 2]
                                for k in range(8):
                                    mm(bg_, bg_[:], wg[0], r32(wg[1][:, k, fc * 128:(fc + 1) * 128]), x1T, r32(x1T[:, k, th * 512:(th + 1) * 512]), k == 0, k == 7)
                                for k in range(8):
                                    mm(bu_, bu_[:], wu[0], r32(wu[1][:, k, fc * 128:(fc + 1) * 128]), x1T, r32(x1T[:, k, th * 512:(th + 1) * 512]), k == 0, k == 7)
                                sgt = sg[(fc * 2 + th) % 2]
                                act(sgt, sgt[:], bg_, bg_[:], AF.Silu)
                                tt("dve", hb, r32(hb[:, fc, th * 512:(th + 1) * 512]), bu_, bu_[:], sgt, sgt[:], ALU.mult)
                    wds = [wload(w_d_d[l, ex, fh * 256:(fh + 1) * 256, :].rearrange("(a p) d -> p a d", p=128)) for fh in range(2)]
                    for a in range(8):
                        for dh in range(2):
                            by = banks[4 + (a * 2 + dh) % 4]
                            i_ = 0
                            for fh in range(2):
                                for fc in range(2):
                                    mm(by, by[:], hT[fh], r32(hT[fh][:, fc, a * 128:(a + 1) * 128]), wds[fh][0], r32(wds[fh][1][:, fc, dh * 512:(dh + 1) * 512]), i_ == 0, i_ == 3)
                                    i_ += 1
                            stt(y_sub[a], yacc[:, a, dh * 512:(dh + 1) * 512], by, by[:], comb[:, a, ex:ex + 1],
                                y_sub[a], yacc[:, a, dh * 512:(dh + 1) * 512], ALU.mult, ALU.add, extra=[comb])
                layer_norm_multi(P, [(y_sub[a], yacc[:, a, :], lnts[a]) for a in range(8)], pp, PP_LN2G, PP_LN2B)
                last = (l == L - 1)
                dst_d = out_d if last else xs_d[0]
                tk = P.dma("pool", dst_d[grow0:grow0 + 1024, :].rearrange("(a p) d -> p a d", p=128), yacc[:],
                           reads=[yacc] + y_sub, writes=[DR(("x0", s, 2 * g)), DR(("x0", s, 2 * g + 1))], key="yst_out")
                if last:
                    out_toks.append(tk)
    P.final_wait("sp", out_toks)
    P.emit()
    return st


def layer_norm_multi(P, items, pp, gcol, bcol):
    for x_t, x_ap, lnt in items:
        stats = lnt[:, 0:12].rearrange("p (a b) -> p a b", a=2)
        for hf in range(2):
            P.op("dve", lambda e, o_=stats[:, hf, :], i_=x_ap[:, hf * 512:(hf + 1) * 512]: e.bn_stats(o_, i_), reads=[x_t], writes=[lnt])
        P.op("dve", lambda e, o_=lnt[:, 12:14], i_=lnt[:, 0:12]: e.bn_aggr(o_, i_), reads=[lnt], writes=[lnt])
    for x_t, x_ap, lnt in items:
        P.op("act", lambda e, o_=lnt[:, 14:15], i_=lnt[:, 13:14]: e.activation(o_, i_, AF.Ln, bias=pp[:, PP_LNEPS:PP_LNEPS + 1]), reads=[lnt, pp], writes=[lnt])
    for x_t, x_ap, lnt in items:
        P.op("act", lambda e, o_=lnt[:, 14:15]: e.activation(o_, o_, AF.Exp, scale=-0.5), reads=[lnt], writes=[lnt])
    for x_t, x_ap, lnt in items:
        P.op("dve", lambda e, x_=x_ap, m_=lnt[:, 12:13], r_=lnt[:, 14:15]: e.tensor_scalar(x_, x_, m_, r_, ALU.subtract, ALU.mult), reads=[x_t, lnt], writes=[x_t])
    for x_t, x_ap, lnt in items:
        P.op("dve", lambda e, x_=x_ap: e.tensor_tensor(x_, x_, pp[:, gcol:gcol + 1024], ALU.mult), reads=[x_t, pp], writes=[x_t])
        P.op("pool", lambda e, x_=x_ap: e.tensor_tensor(x_, x_, pp[:, bcol:bcol + 1024], ALU.add), reads=[x_t, pp], writes=[x_t])


def layer_norm(P, x_t, x_ap, lnt, pp, gcol, bcol):
    stats = lnt[:, 0:12].rearrange("p (a b) -> p a b", a=2)
    mv = lnt[:, 12:14]
    rstd = lnt[:, 14:15]
    for hf in range(2):
        P.op("dve", lambda e, hf=hf: e.bn_stats(stats[:, hf, :], x_ap[:, hf * 512:(hf + 1) * 512]), reads=[x_t], writes=[lnt])
    P.op("dve", lambda e: e.bn_aggr(mv, lnt[:, 0:12]), reads=[lnt], writes=[lnt])
    P.op("act", lambda e: e.activation(rstd, lnt[:, 13:14], AF.Ln, bias=pp[:, PP_LNEPS:PP_LNEPS + 1]), reads=[lnt, pp], writes=[lnt])
    P.op("act", lambda e: e.activation(rstd, rstd, AF.Exp, scale=-0.5), reads=[lnt], writes=[lnt])
    P.op("dve", lambda e: e.tensor_scalar(x_ap, x_ap, lnt[:, 12:13], rstd, ALU.subtract, ALU.mult), reads=[x_t, lnt], writes=[x_t])
    P.op("dve", lambda e: e.tensor_tensor(x_ap, x_ap, pp[:, gcol:gcol + 1024], ALU.mult), reads=[x_t, pp], writes=[x_t])
    P.op("pool", lambda e: e.tensor_tensor(x_ap, x_ap, pp[:, bcol:bcol + 1024], ALU.add), reads=[x_t, pp], writes=[x_t])


def _pack_inputs(inp):
    L = 4
    pp = np.zeros((L, 128, PP_N), np.float32)
    bd = np.zeros((L, 2, 4, 128, 128), np.float32)
    for l in range(L):
        pp[l, :, PP_CAW:PP_CAW + 16] = inp["conv_a_w"][l].reshape(4, 4, 128).transpose(2, 1, 0).reshape(128, 16)
        pp[l, :, PP_CAB:PP_CAB + 4] = inp["conv_a_b"][l].reshape(4, 128).T
        pp[l, :, PP_RBA:PP_RBA + 4] = inp["rg_b_a"][l].reshape(4, 128).T
        pp[l, :, PP_RBX:PP_RBX + 4] = inp["rg_b_x"][l].reshape(4, 128).T
        pp[l, :, PP_LAM:PP_LAM + 4] = inp["rg_lambda"][l].reshape(4, 128).T
        pp[l, :, PP_GCW:PP_GCW + 48] = inp["gdn_conv_w"][l].reshape(4, 12, 128).transpose(2, 1, 0).reshape(128, 48)
        pp[l, :, PP_GNW] = inp["gdn_norm_w"][l]
        pp[l, :, PP_LNW] = inp["gla_norm_w"][l]
        pp[l, :, PP_DTB:PP_DTB + 4] = inp["gdn_dt_bias"][l][None, :]
        pp[l, :, PP_ALOG:PP_ALOG + 4] = inp["gdn_a_log"][l][None, :]
        pp[l, :, PP_RB:PP_RB + 16] = inp["router_bias"][None, :]
        pp[l, :, PP_GLB:PP_GLB + 512] = inp["gla_b_gate"][l][None, :]
        pp[l, :, PP_LN1G:PP_LN1G + 1024] = inp["ln1_g"][l][None, :]
        pp[l, :, PP_LN1B:PP_LN1B + 1024] = inp["ln1_b"][l][None, :]
        pp[l, :, PP_LN2G:PP_LN2G + 1024] = inp["ln2_g"][l][None, :]
        pp[l, :, PP_LN2B:PP_LN2B + 1024] = inp["ln2_b"][l][None, :]
        pp[l, :, PP_ONE] = 1.0
        pp[l, :, PP_EPS] = NORM_EPS
        pp[l, :, PP_LNEPS] = LN_EPS
        for a, nm in enumerate(("rg_w_a", "rg_w_x")):
            w = inp[nm][l]
            for ch in range(4):
                for gb in range(2):
                    bd[l, a, ch, gb * 64:(gb + 1) * 64, gb * 64:(gb + 1) * 64] = w[ch * 2 + gb]
    return pp, bd


_NC_CACHE = {}


def kernel(**inp):
    inp = {k: np.ascontiguousarray(np.asarray(v)) for k, v in inp.items()}
    n = 8
    nseq = 2
    pp, bd = _pack_inputs(inp)
    consts = host_consts()
    if "nc" not in _NC_CACHE:
        nc = bass.Bass("TRN2", target_bir_lowering=False)
        st = build(nc, L=4, NSEQ=nseq)
        _NC_CACHE["nc"] = (nc, st)
    nc = _NC_CACHE["nc"][0]
    x = inp["x"].reshape(n, nseq * S, D)
    shared = {"w_in": inp["w_in"], "w_branch": inp["w_branch"], "w_out": inp["w_out"], "w_gate": inp["w_gate"],
              "w_up": inp["w_up"], "w_down": inp["w_down"], "w_router": inp["w_router"], "pp": pp, "bd": bd,
              "gla_up": inp["gla_w_gate_up"], "consts": consts}
    in_maps = [dict(shared, x=x[i]) for i in range(n)]
    res = run_bass_kernel_spmd(nc, in_maps, core_ids=list(range(n)))
    out = np.stack([np.asarray(r["out"]) for r in res.results], 0)
    return out.reshape(16, S, D).astype(np.float32)
```
